# Optimizing a Trainium2 kernel written in Bass

```python
import math
import jax, jax.numpy as jnp
from jax import lax
import numpy as np

D_MODEL = 1024
BATCH = 4
SEQ = 4096
DEPTH = 4

D_MIX = D_MODEL
N_MIXERS = 4
GROUP_W = D_MIX // N_MIXERS
HEAD_DIM = 64
N_HEADS_G = GROUP_W // HEAD_DIM
SSD_STATE = 128
SSD_CONV = 4
SSD_CHUNK = 128
RWKV_DECAY_LORA = 32
RWKV_A_LORA = 32
RWKV_GATE_LORA = 64
RWKV_GN_EPS = 64e-5
GLA_DK = GROUP_W // 2
GLA_HEAD_K = GLA_DK // N_HEADS_G
GLA_GATE_LORA = 16
GLA_GATE_TAU = 16.0
GLA_CHUNK = 64
MLSTM_CONV = 4
MLSTM_CHUNK = 64
N_EXPERTS = 32
TOP_K = 4
D_EXPERT = D_MODEL
SWIGLU_LIMIT = 7.0
SWIGLU_ALPHA = 1.702
EXPERT_BLOCK = 128
PLE_DIM = 256
DEEPNORM_ALPHA = (2 * DEPTH) ** 0.25
DEEPNORM_BETA = (8 * DEPTH) ** -0.25
LN_EPS = 1e-5
NORM_EPS = 1e-5

SSD_SPLITS = (GROUP_W, GROUP_W, SSD_STATE, SSD_STATE, N_HEADS_G)
RWKV_SPLITS = (GROUP_W, GROUP_W, GROUP_W, RWKV_DECAY_LORA, RWKV_A_LORA, RWKV_GATE_LORA)
GLA_SPLITS = (GLA_DK, GLA_DK, GROUP_W, GLA_GATE_LORA, GROUP_W)
MLSTM_SPLITS = (GROUP_W, GROUP_W, GROUP_W, N_HEADS_G, N_HEADS_G, GROUP_W)
SSD_W = sum(SSD_SPLITS)
RWKV_W = sum(RWKV_SPLITS)
GLA_W = sum(GLA_SPLITS)
MLSTM_W = sum(MLSTM_SPLITS)
D_IN_PROJ = SSD_W + RWKV_W + GLA_W + MLSTM_W

kernel_name = "hybrid_ssd_rwkv7_gla_mlstm_moe_deepnorm"


def _split(u, sizes):
    idx = np.cumsum(sizes)[:-1].tolist()
    return jnp.split(u, idx, axis=-1)


def _causal_mask(n):
    return jnp.tril(jnp.ones((n, n), dtype=bool))


def _causal_dwconv(u, w, b):
    k, c = w.shape
    y = lax.conv_general_dilated(u, w[:, None, :].astype(u.dtype), window_strides=(1,),
                                 padding=[(k - 1, 0)], dimension_numbers=("NWC", "WIO", "NWC"),
                                 feature_group_count=c)
    return y + b


def _token_shift(u):
    return jnp.pad(u, ((0, 0), (1, 0), (0, 0)))[:, :-1]


def _heads(u, n_heads):
    return u.reshape(*u.shape[:-1], n_heads, u.shape[-1] // n_heads)


def _to_chunks(u, size):
    b, s, h = u.shape[:3]
    u = u.reshape(b, s // size, size, h, *u.shape[3:])
    return jnp.moveaxis(u, 3, 1)


def _from_chunks(y):
    y = jnp.moveaxis(y, 1, 3)
    return y.reshape(y.shape[0], y.shape[1] * y.shape[2], *y.shape[3:])


def _chunk_states(decay, contrib):
    d = jnp.moveaxis(decay, 2, 0)
    u = jnp.moveaxis(contrib, 2, 0)

    def step(s, inp):
        dc, uc = inp
        return dc * s + uc, s

    _, prev = lax.scan(step, jnp.zeros_like(u[0]), (d, u))
    return jnp.moveaxis(prev, 0, 2)


def _layer_norm(x, g, b):
    xf = x.astype(jnp.float32)
    mu = jnp.mean(xf, -1, keepdims=True)
    xc = xf - mu
    var = jnp.mean(xc * xc, -1, keepdims=True)
    return (xc * lax.rsqrt(var + LN_EPS) * g + b).astype(x.dtype)


def _group_norm(y, n_heads, gain, bias, eps, center):
    yf = _heads(y.astype(jnp.float32), n_heads)
    if center:
        yf = yf - jnp.mean(yf, -1, keepdims=True)
    yf = yf * lax.rsqrt(jnp.mean(yf * yf, -1, keepdims=True) + eps)
    yf = yf.reshape(y.shape) * gain
    if bias is not None:
        yf = yf + bias
    return yf


def _ssd_mixer(u, conv_w, conv_b, a_log, dt_bias, d_skip, norm_g):
    bsz, seq, _ = u.shape
    z, xr, bm, cm, dt = _split(u, SSD_SPLITS)
    xbc = jax.nn.silu(_causal_dwconv(jnp.concatenate([xr, bm, cm], -1), conv_w, conv_b))
    xs, bm, cm = _split(xbc, (GROUP_W, SSD_STATE, SSD_STATE))
    xs = _heads(xs, N_HEADS_G)
    dt = jax.nn.softplus(dt + dt_bias)
    nc = seq // SSD_CHUNK
    xc = _to_chunks(xs * dt[..., None], SSD_CHUNK)
    adt = _to_chunks(dt * (-jnp.exp(a_log)), SSD_CHUNK)
    bc = bm.reshape(bsz, nc, SSD_CHUNK, SSD_STATE)
    cc = cm.reshape(bsz, nc, SSD_CHUNK, SSD_STATE)
    acum = jnp.cumsum(adt, axis=-1)
    lmat = jnp.exp(jnp.where(_causal_mask(SSD_CHUNK), acum[..., :, None] - acum[..., None, :], -jnp.inf))
    y_diag = jnp.einsum("bcln,bcsn,bhcls,bhcsp->bhclp", cc, bc, lmat, xc)
    decay_states = jnp.exp(acum[..., -1:] - acum)
    states = jnp.einsum("bcsn,bhcs,bhcsp->bhcpn", bc, decay_states, xc)
    prev = _chunk_states(jnp.exp(acum[..., -1])[..., None, None], states)
    y_off = jnp.einsum("bcln,bhcpn,bhcl->bhclp", cc, prev, jnp.exp(acum))
    y = _from_chunks(y_diag + y_off) + xs * d_skip[:, None]
    y = y.reshape(bsz, seq, GROUP_W) * jax.nn.silu(z)
    return _group_norm(y, 1, norm_g, None, NORM_EPS, center=False)


def _rwkv7_mixer(u, mu, w0, w_up, a0, a_up, g_up, k_k, k_a, r_k, ln_g, ln_b):
    bsz, seq, _ = u.shape
    u = u + (_token_shift(u) - u) * mu
    r, k, v, wd, ad, gd = _split(u, RWKV_SPLITS)
    w = -jax.nn.softplus(-(w0 + jnp.tanh(wd) @ w_up)) - 0.5
    decay = jnp.exp(-jnp.exp(w))
    a = jax.nn.sigmoid(a0 + ad @ a_up)
    g = jax.nn.sigmoid(gd) @ g_up
    kk = _heads(k * k_k, N_HEADS_G)
    kk = kk / jnp.maximum(jnp.linalg.norm(kk, axis=-1, keepdims=True), 1e-12)
    k = k * (1.0 + (a - 1.0) * k_a)
    rh, wh, kh, vh, ah = (_heads(t, N_HEADS_G) for t in (r, decay, k, v, a))
    seq_major = [jnp.moveaxis(t, 1, 0) for t in (rh, wh, kh, vh, -kk, kk * ah)]

    def step(state, inp):
        r_t, w_t, k_t, v_t, a_t, b_t = inp
        sa = jnp.einsum("bhvk,bhk->bhv", state, a_t)
        state = (state * w_t[:, :, None, :] + sa[..., None] * b_t[:, :, None, :]
                 + v_t[..., None] * k_t[:, :, None, :])
        return state, jnp.einsum("bhvk,bhk->bhv", state, r_t)

    s0 = jnp.zeros((bsz, N_HEADS_G, HEAD_DIM, HEAD_DIM), rh.dtype)
    _, o = lax.scan(step, s0, tuple(seq_major))
    o = jnp.moveaxis(o, 0, 1).reshape(bsz, seq, GROUP_W)
    o = _group_norm(o, N_HEADS_G, ln_g, ln_b, RWKV_GN_EPS, center=True)
    bonus = jnp.sum(rh * kh * r_k, -1, keepdims=True) * vh
    return (o + bonus.reshape(bsz, seq, GROUP_W)) * g


def _gla_mixer(u, gate_up, gate_b, norm_g):
    bsz, seq, _ = u.shape
    q, k, v, gd, og = _split(u, GLA_SPLITS)
    log_a = jax.nn.log_sigmoid(gd @ gate_up + gate_b) / GLA_GATE_TAU
    qc = _to_chunks(_heads(q, N_HEADS_G), GLA_CHUNK) * GLA_HEAD_K ** -0.5
    kc = _to_chunks(_heads(k, N_HEADS_G), GLA_CHUNK)
    vc = _to_chunks(_heads(v, N_HEADS_G), GLA_CHUNK)
    bcum = jnp.cumsum(_to_chunks(_heads(log_a, N_HEADS_G), GLA_CHUNK), axis=3)
    q_dec = qc * jnp.exp(bcum)
    k_inv = kc * jnp.exp(-bcum)
    attn = jnp.where(_causal_mask(GLA_CHUNK), jnp.einsum("bhctd,bhcsd->bhcts", q_dec, k_inv), 0.0)
    o_intra = jnp.einsum("bhcts,bhcsv->bhctv", attn, vc)
    b_last = bcum[..., -1, :]
    contrib = jnp.einsum("bhcsd,bhcsv->bhcdv", kc * jnp.exp(b_last[..., None, :] - bcum), vc)
    prev = _chunk_states(jnp.exp(b_last)[..., None], contrib)
    o_inter = jnp.einsum("bhctd,bhcdv->bhctv", q_dec, prev)
    o = _from_chunks(o_intra + o_inter).reshape(bsz, seq, GROUP_W)
    return _group_norm(o, N_HEADS_G, norm_g, None, NORM_EPS, center=False) * jax.nn.silu(og)


def _mlstm_mixer(u, conv_w, conv_b, i_b, f_b, norm_g):
    bsz, seq, _ = u.shape
    q, k, v, ig, fg, og = _split(u, MLSTM_SPLITS)
    q, k = _split(jax.nn.silu(_causal_dwconv(jnp.concatenate([q, k], -1), conv_w, conv_b)), (GROUP_W, GROUP_W))
    qc = _to_chunks(_heads(q, N_HEADS_G), MLSTM_CHUNK) * HEAD_DIM ** -0.5
    kc = _to_chunks(_heads(k, N_HEADS_G), MLSTM_CHUNK)
    vc = _to_chunks(_heads(v, N_HEADS_G), MLSTM_CHUNK)
    i_pre = _to_chunks(ig + i_b, MLSTM_CHUNK)
    b = jnp.cumsum(_to_chunks(jax.nn.log_sigmoid(fg + f_b), MLSTM_CHUNK), axis=-1)
    b_last = b[..., -1]
    dmat = jnp.where(_causal_mask(MLSTM_CHUNK), b[..., :, None] - b[..., None, :] + i_pre[..., None, :], -jnp.inf)
    a_st = b_last[..., None] - b + i_pre
    m_loc = jnp.max(a_st, -1)
    w_st = jnp.exp(a_st - m_loc[..., None])
    c_contrib = jnp.einsum("bhcs,bhcsv,bhcsk->bhcvk", w_st, vc, kc)
    n_contrib = jnp.einsum("bhcs,bhcsk->bhck", w_st, kc)

    def step(carry, inp):
        c_s, n_s, m_s = carry
        bl, ml, cu, nu = inp
        m_new = jnp.maximum(bl + m_s, ml)
        s_old = jnp.exp(bl + m_s - m_new)
        s_new = jnp.exp(ml - m_new)
        c_new = s_old[..., None, None] * c_s + s_new[..., None, None] * cu
        n_new = s_old[..., None] * n_s + s_new[..., None] * nu
        return (c_new, n_new, m_new), (c_s, n_s, m_s)

    init = (jnp.zeros_like(c_contrib[:, :, 0]), jnp.zeros_like(n_contrib[:, :, 0]), jnp.zeros_like(b_last[:, :, 0]))
    xs = tuple(jnp.moveaxis(t, 2, 0) for t in (b_last, m_loc, c_contrib, n_contrib))
    _, (c_prev, n_prev, m_prev) = lax.scan(step, init, xs)
    c_prev, n_prev, m_prev = (jnp.moveaxis(t, 0, 2) for t in (c_prev, n_prev, m_prev))
    m_inter = b + m_prev[..., None]
    m_row = jnp.maximum(m_inter, jnp.max(dmat, -1))
    w_inter = jnp.exp(m_inter - m_row)
    scores = jnp.einsum("bhctk,bhcsk->bhcts", qc, kc) * jnp.exp(dmat - m_row[..., None])
    num = (w_inter[..., None] * jnp.einsum("bhcvk,bhctk->bhctv", c_prev, qc)
           + jnp.einsum("bhcts,bhcsv->bhctv", scores, vc))
    den = w_inter * jnp.einsum("bhck,bhctk->bhct", n_prev, qc) + jnp.sum(scores, -1)
    h = num / jnp.maximum(jnp.abs(den), jnp.exp(-m_row))[..., None]
    h = _from_chunks(h).reshape(bsz, seq, GROUP_W) * jax.nn.sigmoid(og)
    return _group_norm(h, N_HEADS_G, norm_g, None, NORM_EPS, center=True)


def _moe_ffn(x2d, router_w, router_b, w_gu, b_gu, w_down, b_down):
    t, d = x2d.shape
    n_assign = t * TOP_K
    logits = (x2d @ router_w + router_b).astype(jnp.float32)
    top_val, top_idx = lax.top_k(logits, TOP_K)
    gate = jax.nn.softmax(top_val, axis=-1)
    flat_e = top_idx.reshape(-1)
    flat_tok = jnp.arange(n_assign, dtype=jnp.int32) // TOP_K
    order = jnp.argsort(flat_e)
    e_s, tok_s, g_s = flat_e[order], flat_tok[order], gate.reshape(-1)[order]
    counts = jnp.zeros((N_EXPERTS,), jnp.int32).at[flat_e].add(1)
    padded = (counts + EXPERT_BLOCK - 1) // EXPERT_BLOCK * EXPERT_BLOCK
    start = jnp.cumsum(counts) - counts
    pend = jnp.cumsum(padded)
    pstart = pend - padded
    dest = pstart[e_s] + (jnp.arange(n_assign, dtype=jnp.int32) - start[e_s])
    n_blocks = (n_assign + N_EXPERTS * (EXPERT_BLOCK - 1) + EXPERT_BLOCK - 1) // EXPERT_BLOCK
    n_rows = n_blocks * EXPERT_BLOCK
    row_tok = jnp.full((n_rows,), t, jnp.int32).at[dest].set(tok_s)
    row_gate = jnp.zeros((n_rows,), jnp.float32).at[dest].set(g_s)
    block_expert = jnp.minimum(jnp.searchsorted(pend, jnp.arange(n_blocks) * EXPERT_BLOCK, side="right"),
                               N_EXPERTS - 1)
    x_pad = jnp.concatenate([x2d, jnp.zeros((1, d), x2d.dtype)], 0)
    xb = x_pad[row_tok].reshape(n_blocks, EXPERT_BLOCK, d)

    def expert_block(args):
        xblk, e = args
        hg, hl = jnp.split(xblk @ w_gu[e] + b_gu[e], 2, axis=-1)
        hg = jnp.minimum(hg, SWIGLU_LIMIT)
        hl = jnp.clip(hl, -SWIGLU_LIMIT, SWIGLU_LIMIT)
        glu = hg * jax.nn.sigmoid(hg * SWIGLU_ALPHA)
        return ((hl + 1.0) * glu) @ w_down[e] + b_down[e]

    yb = lax.map(expert_block, (xb, block_expert)).reshape(n_rows, d)
    y = (yb * row_gate[:, None]).astype(x2d.dtype)
    return jnp.zeros((t + 1, d), x2d.dtype).at[row_tok].add(y)[:t]


def setup_inputs(seed: int = 0) -> dict:
    key = jax.random.key(seed)
    ks = iter(jax.random.split(key, 64))
    f32 = jnp.float32
    L, W, H = DEPTH, GROUP_W, N_HEADS_G

    def nrm(shape, scale):
        return scale * jax.random.normal(next(ks), shape, f32)

    def uni(shape, lo, hi):
        return jax.random.uniform(next(ks), shape, f32, lo, hi)

    dt0 = jnp.exp(uni((L, H), math.log(1e-3), math.log(1e-1)))
    return {
        "x": nrm((BATCH, SEQ, D_MODEL), 1.0),
        "p": nrm((DEPTH, BATCH, SEQ, PLE_DIM), 1.0),
        "w_in": nrm((L, D_MODEL, D_IN_PROJ), D_MODEL ** -0.5),
        "w_out": nrm((L, D_MIX, D_MODEL), DEEPNORM_BETA * D_MIX ** -0.5),
        "ln1_g": 1.0 + nrm((L, D_MODEL), 0.02),
        "ln1_b": nrm((L, D_MODEL), 0.02),
        "ssd_conv_w": nrm((L, SSD_CONV, W + 2 * SSD_STATE), SSD_CONV ** -0.5),
        "ssd_conv_b": nrm((L, W + 2 * SSD_STATE), 0.02),
        "ssd_a_log": jnp.log(uni((L, H), 1.0, 16.0)),
        "ssd_dt_bias": dt0 + jnp.log(-jnp.expm1(-dt0)),
        "ssd_d": 1.0 + nrm((L, H), 0.1),
        "ssd_norm_g": 1.0 + nrm((L, W), 0.02),
        "rwkv_mu": uni((L, RWKV_W), 0.0, 1.0),
        "rwkv_w0": uni((L, W), -6.0, -1.0),
        "rwkv_w_up": nrm((L, RWKV_DECAY_LORA, W), 0.5 * RWKV_DECAY_LORA ** -0.5),
        "rwkv_a0": nrm((L, W), 0.1),
        "rwkv_a_up": nrm((L, RWKV_A_LORA, W), RWKV_A_LORA ** -0.5),
        "rwkv_g_up": nrm((L, RWKV_GATE_LORA, W), RWKV_GATE_LORA ** -0.5),
        "rwkv_k_k": 0.85 + nrm((L, W), 0.02),
        "rwkv_k_a": 1.0 + nrm((L, W), 0.02),
        "rwkv_r_k": nrm((L, H, HEAD_DIM), 0.1),
        "rwkv_ln_g": 1.0 + nrm((L, W), 0.02),
        "rwkv_ln_b": nrm((L, W), 0.02),
        "gla_gate_up": nrm((L, GLA_GATE_LORA, GLA_DK), GLA_GATE_LORA ** -0.5),
        "gla_gate_b": nrm((L, GLA_DK), 0.1),
        "gla_norm_g": 1.0 + nrm((L, W), 0.02),
        "mlstm_conv_w": nrm((L, MLSTM_CONV, 2 * W), MLSTM_CONV ** -0.5),
        "mlstm_conv_b": nrm((L, 2 * W), 0.02),
        "mlstm_i_b": nrm((L, H), 0.1),
        "mlstm_f_b": uni((L, H), 3.0, 6.0),
        "mlstm_norm_g": 1.0 + nrm((L, W), 0.02),
        "router_w": nrm((L, D_MODEL, N_EXPERTS), D_MODEL ** -0.5),
        "router_b": nrm((L, N_EXPERTS), 0.01),
        "exp_w_gu": nrm((L, N_EXPERTS, D_MODEL, 2 * D_EXPERT), D_MODEL ** -0.5),
        "exp_b_gu": nrm((L, N_EXPERTS, 2 * D_EXPERT), 0.01),
        "exp_w_down": nrm((L, N_EXPERTS, D_EXPERT, D_MODEL), DEEPNORM_BETA * D_EXPERT ** -0.5),
        "exp_b_down": nrm((L, N_EXPERTS, D_MODEL), 0.01),
        "ple_gate_w": nrm((L, D_MODEL, D_MODEL), D_MODEL ** -0.5),
        "ple_proj": nrm((L, PLE_DIM, D_MODEL), DEEPNORM_BETA * PLE_DIM ** -0.5),
        "ln2_g": 1.0 + nrm((L, D_MODEL), 0.02),
        "ln2_b": nrm((L, D_MODEL), 0.02),
    }


def reference(x, p, w_in, w_out, ln1_g, ln1_b,
              ssd_conv_w, ssd_conv_b, ssd_a_log, ssd_dt_bias, ssd_d, ssd_norm_g,
              rwkv_mu, rwkv_w0, rwkv_w_up, rwkv_a0, rwkv_a_up, rwkv_g_up, rwkv_k_k, rwkv_k_a,
              rwkv_r_k, rwkv_ln_g, rwkv_ln_b,
              gla_gate_up, gla_gate_b, gla_norm_g,
              mlstm_conv_w, mlstm_conv_b, mlstm_i_b, mlstm_f_b, mlstm_norm_g,
              router_w, router_b, exp_w_gu, exp_b_gu, exp_w_down, exp_b_down,
              ple_gate_w, ple_proj, ln2_g, ln2_b):
    bsz, seq, d = x.shape
    for i in range(DEPTH):
        u = (x @ w_in[i]).astype(jnp.float32)
        u_ssd, u_rwkv, u_gla, u_mlstm = _split(u, (SSD_W, RWKV_W, GLA_W, MLSTM_W))
        y_ssd = _ssd_mixer(u_ssd, ssd_conv_w[i], ssd_conv_b[i], ssd_a_log[i], ssd_dt_bias[i],
                           ssd_d[i], ssd_norm_g[i])
        y_rwkv = _rwkv7_mixer(u_rwkv, rwkv_mu[i], rwkv_w0[i], rwkv_w_up[i], rwkv_a0[i], rwkv_a_up[i],
                              rwkv_g_up[i], rwkv_k_k[i], rwkv_k_a[i], rwkv_r_k[i], rwkv_ln_g[i], rwkv_ln_b[i])
        y_gla = _gla_mixer(u_gla, gla_gate_up[i], gla_gate_b[i], gla_norm_g[i])
        y_mlstm = _mlstm_mixer(u_mlstm, mlstm_conv_w[i], mlstm_conv_b[i], mlstm_i_b[i], mlstm_f_b[i],
                               mlstm_norm_g[i])
        mix = jnp.concatenate([y_ssd, y_rwkv, y_gla, y_mlstm], -1).astype(x.dtype) @ w_out[i]
        x = _layer_norm(DEEPNORM_ALPHA * x + mix, ln1_g[i], ln1_b[i])
        ffn = _moe_ffn(x.reshape(bsz * seq, d), router_w[i], router_b[i], exp_w_gu[i], exp_b_gu[i],
                       exp_w_down[i], exp_b_down[i]).reshape(bsz, seq, d)
        h = DEEPNORM_ALPHA * x + ffn
        h = h + jax.nn.sigmoid(h @ ple_gate_w[i]) * (p[i] @ ple_proj[i])
        x = _layer_norm(h, ln2_g[i], ln2_b[i])
    return x
```

```python
import math
from contextlib import ExitStack
import numpy as np
import concourse.bass as bass
import concourse.mybir as mybir
from concourse.bass_utils import run_bass_kernel_spmd

F32 = mybir.dt.float32
BF16 = mybir.dt.bfloat16
I32 = mybir.dt.int32
U32 = mybir.dt.uint32
AF = mybir.ActivationFunctionType
ALU = mybir.AluOpType
AX = mybir.AxisListType

D = 1024
T = 4096
NT = T // 128
DEPTH = 4
NCOL = 3484
NE = 32
CAP = 768
ALPHA = (2 * DEPTH) ** 0.25
LN_EPS = 1e-5
SSD0, RW0, GL0, ML0 = 0, 772, 1668, 2452


def _isap(a):
    return hasattr(a, 'tensor')


def K(*aps):
    return list({a.tensor.name for a in aps if _isap(a)})


class Prog:
    ENGS = ['pe', 'act', 'dve', 'pool', 'sp']
    NROT = 4
    CH = 1500
    CHC = 4000

    def __init__(self, nc):
        self.nc = nc
        self.ops = {e: [] for e in self.ENGS}
        self.cnt = {}
        self.last_w = {}
        self.readers = {}
        self.waited = {e: {} for e in self.ENGS}
        self.ndma = {e: 0 for e in self.ENGS}
        self.sems = {}
        self.cwr = {}
        self.pe_bank = {}
        self._bound_reg = None
        self.nops = 0
        self.maxops = None

    def op(self, eng, fn, reads=(), writes=(), dma=False, cwrites=(), selfwait=()):
        self.nops += 1
        if self.maxops is not None and self.nops > self.maxops:
            return
        pr = [r for r in reads if r.startswith('ps')]
        if pr:
            reads = [r for r in reads if not r.startswith('ps')]
            writes = list(writes) + [r for r in pr if r not in writes]
        if dma:
            j = self.ndma[eng]
            self.ndma[eng] += 1
            counter = 'dma_%s_%d' % (eng, j % self.NROT)
        else:
            counter = eng
        idx = self.cnt.get(counter, 0)
        self.cnt[counter] = idx + 1
        deps = {}

        def need(c, j):
            if deps.get(c, -1) < j:
                deps[c] = j
        for r in reads:
            if r in self.last_w:
                need(*self.last_w[r])
            for c, j in self.cwr.get(r, {}).items():
                need(c, j)
        for w in writes:
            if w in self.last_w:
                need(*self.last_w[w])
            for c, j in self.readers.get(w, {}).items():
                need(c, j)
            for c, j in self.cwr.get(w, {}).items():
                need(c, j)
        for w in cwrites:
            if w in self.last_w:
                need(*self.last_w[w])
            for c, j in self.readers.get(w, {}).items():
                need(c, j)
        waits = []
        for j in selfwait:
            if self.waited[eng].get(counter, -1) < j:
                self.waited[eng][counter] = j
                waits.append((counter, j))
        for c, j in deps.items():
            if c == counter and eng == 'pe' and not dma:
                continue
            if self.waited[eng].get(c, -1) >= j:
                continue
            self.waited[eng][c] = j
            waits.append((c, j))
        self.ops[eng].append((waits, fn, counter, idx))
        for w in writes:
            self.last_w[w] = (counter, idx)
            self.readers[w] = {}
            self.cwr[w] = {}
        for w in cwrites:
            d = self.cwr.setdefault(w, {})
            if d.get(counter, -1) < idx:
                d[counter] = idx
        for r in reads:
            if r in writes:
                continue
            d = self.readers.setdefault(r, {})
            if d.get(counter, -1) < idx:
                d[counter] = idx

    @staticmethod
    def _prange(ap):
        st, n = ap.ap[0]
        p0 = (ap.offset // st) if st else 0
        return (p0, p0 + n)

    def mm(self, out, lhsT, rhs, start=True, stop=True):
        bank = out.tensor.name
        r0, r1 = self._prange(lhsT)
        extra = []
        last = self.pe_bank.get(bank)
        if last is not None:
            (l0, l1), lidx = last
            if r1 <= l0 or l1 <= r0:
                extra = [lidx]
        idx = self.cnt.get('pe', 0)
        self.pe_bank[bank] = ((r0, r1), idx)
        self.op('pe', lambda e: e.matmul(out, lhsT=lhsT, rhs=rhs, start=start, stop=stop),
                reads=K(lhsT, rhs), writes=K(out), selfwait=extra)

    def tr(self, out, in_, ident):
        self.op('pe', lambda e: e.transpose(out, in_, ident), reads=K(in_, ident), writes=K(out))

    def act(self, out, in_, func, bias=None, scale=None, accum_out=None, eng='act'):
        kw = {}
        if bias is not None:
            kw['bias'] = bias
        if scale is not None:
            kw['scale'] = scale
        if accum_out is not None:
            kw['accum_out'] = accum_out
        self.op('act', lambda e: e.activation(out=out, in_=in_, func=func, **kw),
                reads=K(in_, bias, scale), writes=K(out, accum_out))

    def cp(self, eng, out, in_):
        if eng == 'act':
            self.op('act', lambda e: e.copy(out=out, in_=in_), reads=K(in_), writes=K(out))
        else:
            self.op(eng, lambda e: e.tensor_copy(out=out, in_=in_), reads=K(in_), writes=K(out))

    def tt(self, eng, out, in0, in1, op):
        self.op(eng, lambda e: e.tensor_tensor(out=out, in0=in0, in1=in1, op=op),
                reads=K(in0, in1), writes=K(out))

    def ts(self, eng, out, in0, s1, op0, s2=None, op1=None, accum_out=None):
        kw = {}
        if op1 is not None:
            kw['op1'] = op1
        if accum_out is not None:
            kw['accum_out'] = accum_out
        self.op(eng, lambda e: e.tensor_scalar(out=out, in0=in0, scalar1=s1, scalar2=s2, op0=op0, **kw),
                reads=K(in0, s1, s2), writes=K(out, accum_out))

    def stt(self, eng, out, in0, scalar, in1, op0, op1):
        self.op(eng, lambda e: e.scalar_tensor_tensor(out=out, in0=in0, scalar=scalar, in1=in1, op0=op0, op1=op1),
                reads=K(in0, scalar, in1), writes=K(out))

    def rsum(self, eng, out, in_):
        self.op(eng, lambda e: e.reduce_sum(out=out, in_=in_, axis=AX.X), reads=K(in_), writes=K(out))

    def memset(self, eng, out, val):
        self.op(eng, lambda e: e.memset(out, val), writes=K(out))

    def dma(self, out, in_, eng='sp', comm=False):
        if comm:
            self.op(eng, lambda e: e.dma_start(out=out, in_=in_), reads=K(in_), cwrites=K(out), dma=True)
        else:
            self.op(eng, lambda e: e.dma_start(out=out, in_=in_), reads=K(in_), writes=K(out), dma=True)


    def dmac(self, out, in_):
        self.op('pool', lambda e: e.dma_start(out=out, in_=in_), reads=K(in_), writes=K(out), dma=True)

    def _breg(self, e, bound):
        if self._bound_reg is None:
            self._bound_reg = (bound, e.to_reg(bound))
        assert self._bound_reg[0] == bound
        return self._bound_reg[1]

    def scatter(self, out_dram, idx_ap, in_sb, bound):
        self.op('pool', lambda e: e.indirect_dma_start(
            out=out_dram, out_offset=bass.IndirectOffsetOnAxis(ap=idx_ap, axis=0),
            in_=in_sb, in_offset=None, bounds_check=self._breg(e, bound), oob_is_err=False),
            reads=K(in_sb, idx_ap), cwrites=K(out_dram), dma=True)

    def gather(self, out_sb, in_dram, idx_ap, bound):
        self.op('pool', lambda e: e.indirect_dma_start(
            out=out_sb, out_offset=None, in_=in_dram,
            in_offset=bass.IndirectOffsetOnAxis(ap=idx_ap, axis=0), bounds_check=self._breg(e, bound), oob_is_err=False),
            reads=K(in_dram, idx_ap), writes=K(out_sb), dma=True)

    def treduce(self, eng, out, in_, op=None):
        op = ALU.add if op is None else op
        self.op(eng, lambda e: e.tensor_reduce(out=out, in_=in_, axis=AX.X, op=op), reads=K(in_), writes=K(out))

    def recip(self, out, in_):
        self.op('dve', lambda e: e.reciprocal(out=out, in_=in_), reads=K(in_), writes=K(out))

    def _ch(self, counter):
        return self.CH if counter.startswith('dma_') else self.CHC

    def _sem(self, counter, idx):
        return self.sems[(counter, idx // self._ch(counter))]

    def _val(self, counter, idx):
        inc = 16 if counter.startswith('dma_') else 1
        return (idx % self._ch(counter) + 1) * inc

    def emit(self):
        nc = self.nc
        with ExitStack() as st:
            for counter, n in self.cnt.items():
                ch = self._ch(counter)
                for k in range((n + ch - 1) // ch):
                    self.sems[(counter, k)] = st.enter_context(nc.semaphore('s_%s_%d' % (counter, k)))
            block = st.enter_context(nc.Block())

            def mk(engname):
                def body(e):
                    for waits, fn, counter, idx in self.ops[engname]:
                        for c, j in waits:
                            e.wait_ge(self._sem(c, j), self._val(c, j))
                        ins = fn(e)
                        ins.then_inc(self._sem(counter, idx), 16 if counter.startswith('dma_') else 1)
                    if engname == 'sp':
                        for c, n in self.cnt.items():
                            if n > 0:
                                e.wait_ge(self._sem(c, n - 1), self._val(c, n - 1))
                return body
            block.tensor(mk('pe'))
            block.scalar(mk('act'))
            block.vector(mk('dve'))
            block.gpsimd(mk('pool'))
            block.sync(mk('sp'))


TM_FIELDS = [
    ('dt_bias', 4), ('a_log', 4), ('ssd_d', 4), ('ssd_ng', 256),
    ('rw_mu', 768), ('rw_w0', 256), ('rw_a0', 256), ('rw_kk', 256), ('rw_ka', 256), ('rw_rk', 256),
    ('rw_lng', 256), ('rw_lnb', 256),
    ('gl_gb', 128), ('gl_ng', 256),
    ('ml_ib', 4), ('ml_fb', 4), ('ml_ng', 256),
    ('ln1_g', 1024), ('ln1_b', 1024), ('ln2_g', 1024), ('ln2_b', 1024), ('rt_b', 32),
]
TM_OFF = {}
_o = 0
for _n, _w in TM_FIELDS:
    TM_OFF[_n] = (_o, _w)
    _o += _w
TM_W = _o
CM_W = 41


def h4(ap, h=4):
    return ap.rearrange("p (h d) -> p h d", h=h)


def b4(ap, w, h=4):
    return ap.rearrange("p (h o) -> p h o", o=1).to_broadcast([128, h, w])


import os
CONVPS = int(os.environ.get("CONVPS", "0"))


def build(n_layers=DEPTH, debug=False, stop_after=None, ntiles=NT, skip=(), maxops=None):
    nc = bass.Bass("TRN2", target_bir_lowering=False)
    P = Prog(nc)
    P.maxops = maxops
    LD = n_layers
    NED = 1 if stop_after in ('mixers', 'phaseA') else NE

    def din(name, shape, dt=F32):
        return nc.dram_tensor(name, list(shape), dt, kind="ExternalInput").ap()
    x_in = din('x', [T, D])
    p_in = din('p', [LD, T, 256])
    w_in = din('w_in', [LD, D, NCOL])
    w_out = din('w_out', [LD, D, D])
    tmrow = din('tmrow', [LD, 1, TM_W])
    cmcol = din('cmcol', [LD, 128, CM_W])
    lora_up = din('lora_up', [LD, 128, 256])
    gla_up = din('gla_up', [LD, 16, 128])
    router_w = din('router_w', [LD, D, NE])
    w_gu = din('w_gu', [LD, NED, D, 2 * D])
    b_gu = din('b_gu', [LD, NE, 128, 16])
    w_dn = din('w_dn', [LD, NED, D, D])
    b_dn = din('b_dn', [LD, NE, 1, D])
    ple_g = din('ple_g', [LD, D, D])
    ple_p = din('ple_p', [LD, 256, D])
    consts = din('consts', [128, 6, 128])
    consts2 = din('consts2', [128, 2, 256])
    out = nc.dram_tensor('out', [T, D], F32, kind="ExternalOutput").ap()
    xbuf = nc.dram_tensor('xbuf', [T, D], F32).ap()
    x1buf = nc.dram_tensor('x1buf', [T, D], F32).ap()
    xg = nc.dram_tensor('xg', [NE * CAP, D], BF16).ap()
    yg = nc.dram_tensor('yg', [NE * CAP, D], F32).ap()
    dbg = {}
    if debug:
        for nm, w in [('d_ssd', 256), ('d_rwkv', 256), ('d_gla', 256), ('d_mlstm', 256), ('d_x1', 1024),
                      ('d_ffn', 1024), ('d_logits', 32), ('d_dest', 4), ('d_gate', 4)]:
            dbg[nm] = nc.dram_tensor(nm, [T, w], F32, kind="ExternalOutput").ap()

    with ExitStack() as st:
        def sb(name, shape, dt=F32):
            return st.enter_context(nc.sbuf_tensor(name, list(shape), dt))

        def psb(name, shape, dt=F32):
            return st.enter_context(nc.psum_tensor(name, list(shape), dt))

        R0 = sb('R0', [128, 8 * NCOL], BF16)
        R1 = sb('R1', [128, 24576], BF16)
        tmc = sb('tmc', [128, TM_W])
        cmc = sb('cmc', [128, CM_W])
        cst = sb('cst', [128, 6, 128])
        cst2 = sb('cst2', [128, 2, 256])
        identb = sb('identb', [128, 128], BF16)
        lup = sb('lup', [128, 256])
        gup = sb('gup', [16, 128])
        rtw = sb('rtw', [128, 8, NE])
        ones = sb('ones', [128, 128])
        G = [sb('G%d' % i, [128, 1024]) for i in range(7)]
        H = [sb('H%d' % i, [128, 1024], BF16) for i in range(5)]
        xTe = [sb('xTe%d' % i, [128, 8, 129], BF16) for i in range(2)]
        cbuf = [sb('cbuf%d' % i, [128, 131]) for i in range(8)]
        small = sb('small', [128, 128])
        small2 = sb('small2', [128, 32])
        TTt = sb('TTt', [128, 128])
        ssd_S = sb('ssd_S', [128, 256]); ssd_Sb = sb('ssd_Sb', [128, 256], BF16)
        rw_S = [sb('rw_S%d' % i, [128, 64]) for i in range(2)]
        rw_Sb = [sb('rw_Sb%d' % i, [128, 64], BF16) for i in range(2)]
        gl_S = sb('gl_S', [128, 256]); gl_Sb = sb('gl_Sb', [128, 256], BF16)
        ml_S = [sb('ml_S%d' % i, [128, 65]) for i in range(2)]
        ml_Sb = [sb('ml_Sb%d' % i, [128, 65], BF16) for i in range(2)]
        dest_all = sb('dest_all', [128, NT, 4], I32)
        gate_all = sb('gate_all', [128, NT, 4])
        cntb = sb('cntb', [128, NE])
        rsc = sb('rsc', [128, 256])
        idx8 = sb('idx8', [128, 8], U32)
        bgu = [sb('bgu%d' % i, [128, 16]) for i in range(2)]
        PS = [psb('ps%d' % i, [128, 512]) for i in range(7)]
        PSB = psb('psb', [128, 1024], BF16)

        ident = cst[:, 0, :]
        TRI = cst[:, 1, :]
        TRIS = cst[:, 2, :]
        BTRI = cst[:, 3, :]
        BTRIS = cst[:, 4, :]
        BLOW = cst[:, 5, :]
        GLMASK = cst2[:, 0, :]
        IOTA32 = cst2[:, 1, 0:32]

        def tmf(name):
            o, w = TM_OFF[name]
            return tmc[:, o:o + w]

        P.dma(cst[:], consts)
        P.dma(cst2[:], consts2)
        P.cp('dve', identb[:], cst[:, 0, :])
        P.memset('dve', ones[:], 1.0)

        W3 = R0[:, 0:8 * NCOL].rearrange("p (k n) -> p k n", k=8)
        WO = R1[:, 0:8192].rearrange("p (k n) -> p k n", k=8)
        PG = R1[:, 8192:16384].rearrange("p (k n) -> p k n", k=8)
        PP = R1[:, 16384:18432].rearrange("p (k n) -> p k n", k=2)

        def layernorm(dst, src, gname, bname, sc):
            P.rsum('dve', sc[:, 0:1], src)
            P.ts('dve', sc[:, 1:2], sc[:, 0:1], -1.0 / D, ALU.mult)
            P.act(G[6][:], src, AF.Square, bias=sc[:, 1:2], accum_out=sc[:, 2:3])
            P.act(sc[:, 3:4], sc[:, 2:3], AF.Ln, bias=LN_EPS, scale=1.0 / D)
            P.act(sc[:, 3:4], sc[:, 3:4], AF.Exp, scale=-0.5)
            P.ts('dve', dst, src, sc[:, 1:2], ALU.add, sc[:, 3:4], ALU.mult)
            P.tt('dve', dst, dst, tmf(gname), ALU.mult)
            P.tt('pool', dst, dst, tmf(bname), ALU.add)

        def groupnorm(y, nh, gain, bias, eps, center, sc, tmp):
            hd = 256 // nh
            y3 = h4(y, nh)
            t3 = h4(tmp, nh)
            if center:
                P.treduce('dve', sc[:, 0:nh], y3)
                P.ts('dve', sc[:, 0:nh], sc[:, 0:nh], -1.0 / hd, ALU.mult)
                P.tt('dve', y3, y3, b4(sc[:, 0:nh], hd, nh), ALU.add)
            P.tt('pool', tmp, y, y, ALU.mult)
            P.treduce('dve', sc[:, 4:4 + nh], t3)
            P.act(sc[:, 4:4 + nh], sc[:, 4:4 + nh], AF.Ln, bias=eps, scale=1.0 / hd)
            P.act(sc[:, 4:4 + nh], sc[:, 4:4 + nh], AF.Exp, scale=-0.5)
            P.tt('dve', y3, y3, b4(sc[:, 4:4 + nh], hd, nh), ALU.mult)
            P.tt('dve', y, y, gain, ALU.mult)
            if bias is not None:
                P.tt('dve', y, y, bias, ALU.add)

        def load_r1(layer):
            for k in range(8):
                P.dmac(WO[:, k, :], w_out[layer, k * 128:(k + 1) * 128, :])
            for k in range(8):
                P.dmac(PG[:, k, :], ple_g[layer, k * 128:(k + 1) * 128, :])
            for k in range(2):
                P.dmac(PP[:, k, :], ple_p[layer, k * 128:(k + 1) * 128, :])

        for layer in range(n_layers):
            xsrc = x_in if layer == 0 else xbuf
            xdst = out if layer == n_layers - 1 else xbuf
            for k in range(8):
                P.dmac(W3[:, k, :], w_in[layer, k * 128:(k + 1) * 128, :])
            load_r1(layer)
            P.dma(tmc[:], tmrow[layer].partition_broadcast(128))
            P.dma(cmc[:], cmcol[layer])
            P.dma(lup[:], lora_up[layer])
            P.dma(gup[:], gla_up[layer])
            P.dma(rtw[:], router_w[layer].rearrange("(k p) e -> p k e", p=128))
            P.act(tmf('a_log'), tmf('a_log'), AF.Exp)
            P.ts('dve', tmf('a_log'), tmf('a_log'), -1.0, ALU.mult)
            for s_ in [ssd_S, gl_S] + rw_S + ml_S + [cntb]:
                P.memset('dve', s_[:], 0.0)
            for s_ in [ssd_Sb, gl_Sb] + rw_Sb + ml_Sb:
                P.memset('pool', s_[:], 0.0)
            for cb in cbuf:
                P.memset('pool', cb[:, 0:3], 0.0)
            P.memset('dve', xTe[0][:, :, 0:1], 0.0)

            for c in range(ntiles):
                xt = G[0]
                xe = xTe[c % 2]
                xn = xTe[(c + 1) % 2]
                P.dma(xt[:], xsrc[c * 128:(c + 1) * 128, :])
                for k in range(8):
                    P.tr(PS[k // 4][:, (k % 4) * 128:(k % 4 + 1) * 128], xt[:, k * 128:(k + 1) * 128], ident)
                for hf in range(2):
                    P.cp('act' if hf == 0 else 'dve', xe[:, hf * 4:(hf + 1) * 4, 1:129],
                         PS[hf][:, :].rearrange("p (k n) -> p k n", k=4))
                P.cp('pool', xn[:, :, 0:1], xe[:, :, 128:129])

                def cm_mm(ps_ap, c0, ncols, shifted=False):
                    for k in range(8):
                        rhs = xe[:, k, 0:128] if shifted else xe[:, k, 1:129]
                        P.mm(ps_ap, W3[:, k, c0:c0 + ncols], rhs, start=(k == 0), stop=(k == 7))

                def tm_mm(ps_ap, c0, ncols, shifted=False):
                    for k in range(8):
                        lhsT = xe[:, k, 0:128] if shifted else xe[:, k, 1:129]
                        P.mm(ps_ap, lhsT, W3[:, k, c0:c0 + ncols], start=(k == 0), stop=(k == 7))

                def conv_silu(ci, wcol, bcol, c0, dst):
                    cb = cbuf[ci]
                    ps = PS[2 + (ci % 4 if CONVPS else ci % 2)]
                    cm_mm(ps[:, 0:128], c0, 128)
                    P.cp('act', cb[:, 3:131], ps[:, 0:128])
                    tmp = G[6][:, 0:128]
                    P.ts('dve', tmp, cb[:, 0:128], cmc[:, wcol:wcol + 1], ALU.mult, cmc[:, bcol:bcol + 1], ALU.add)
                    for j in range(1, 4):
                        P.stt('dve', tmp, cb[:, j:j + 128], cmc[:, wcol + j:wcol + j + 1], tmp, ALU.mult, ALU.add)
                    P.act(dst, tmp, AF.Silu)
                    P.cp('pool', cb[:, 0:3], cb[:, 128:131])

                Y = G[1]

                if 'ssd' not in skip:
                    cmf = G[2]
                    for ci in range(4):
                        conv_silu(ci, ci * 4, 16 + ci, SSD0 + 256 + ci * 128, cmf[:, ci * 128:(ci + 1) * 128])
                    P.cp('pool', H[0][:, 0:512], cmf[:, 0:512])
                    BT = H[0][:, 256:384]
                    CT = H[0][:, 384:512]
                    ps = PS[4]
                    for j in range(3):
                        P.tr(ps[:, j * 128:(j + 1) * 128], cmf[:, j * 128:(j + 1) * 128], ident)
                    xs = G[3][:, 0:256]
                    P.cp('act', xs, ps[:, 0:256])
                    Btm = H[1][:, 0:128]
                    P.cp('dve', Btm, ps[:, 256:384])
                    ps = PS[5]
                    tm_mm(ps[:, 0:256], SSD0 + 0, 256)
                    tm_mm(ps[:, 256:260], SSD0 + 768, 4)
                    zs = G[3][:, 256:512]
                    P.act(zs, ps[:, 0:256], AF.Silu)
                    sc = small
                    dt = sc[:, 0:4]
                    P.tt('dve', dt, ps[:, 256:260], tmf('dt_bias'), ALU.add)
                    P.act(dt, dt, AF.Exp)
                    P.act(dt, dt, AF.Ln, bias=1.0)
                    adt = sc[:, 4:8]
                    P.tt('dve', adt, dt, tmf('a_log'), ALU.mult)
                    ps = PS[6]
                    P.mm(ps[:, 0:4], TRI, adt)
                    P.mm(ps[:, 4:8], ones[:], adt)
                    acum = sc[:, 8:12]
                    P.cp('dve', acum, ps[:, 0:4])
                    ea = sc[:, 12:16]
                    P.act(ea, ps[:, 0:4], AF.Exp)
                    dsx = sc[:, 16:20]
                    P.tt('dve', dsx, ps[:, 4:8], acum, ALU.subtract)
                    P.act(dsx, dsx, AF.Exp)
                    eal = sc[:, 20:24]
                    P.act(eal, ps[:, 4:8], AF.Exp)
                    xdt = H[1][:, 128:384]
                    xdt2 = H[1][:, 384:640]
                    xdtf = G[3][:, 512:768]
                    P.tt('dve', h4(xdtf), h4(xs), b4(dt, 64), ALU.mult)
                    P.cp('pool', xdt, xdtf)
                    P.tt('dve', h4(xdt2), h4(xdtf), b4(dsx, 64), ALU.mult)
                    ps = PS[2]
                    P.mm(ps[:, 0:128], BT, CT)
                    GTm = G[4][:, 0:128]
                    P.tt('dve', GTm, ps[:, 0:128], TRI, ALU.mult)
                    psy = PS[3]
                    for h in range(4):
                        psl = PS[4 + h % 2]
                        adt_bc = G[4][:, 128:256]
                        P.cp('pool', adt_bc, adt[:, h:h + 1].to_broadcast([128, 128]))
                        P.mm(psl[:, 0:128], adt_bc, TRI)
                        lt = G[4][:, 256:384]
                        P.ts('dve', lt, psl[:, 0:128], acum[:, h:h + 1], ALU.subtract, 0.0, ALU.min)
                        P.act(lt, lt, AF.Exp)
                        MT = H[2][:, h * 128:(h + 1) * 128]
                        P.tt('dve', MT, lt, GTm, ALU.mult)
                        P.mm(psy[:, h * 64:(h + 1) * 64], MT, xdt[:, h * 64:(h + 1) * 64])
                    pso = PS[6]
                    P.mm(pso[:, 256:512], CT, ssd_Sb[:])
                    yv = Y[:, 0:256]
                    P.tt('dve', h4(yv), h4(pso[:, 256:512]), b4(ea, 64), ALU.mult)
                    P.tt('dve', yv, yv, psy[:, 0:256], ALU.add)
                    psn = PS[2]
                    P.mm(psn[:, 128:384], Btm, xdt2)
                    P.tt('dve', h4(ssd_S[:]), h4(ssd_S[:]), b4(eal, 64), ALU.mult)
                    P.tt('dve', ssd_S[:], ssd_S[:], psn[:, 128:384], ALU.add)
                    P.cp('pool', ssd_Sb[:], ssd_S[:])
                    t1 = G[4][:, 512:768]
                    P.tt('pool', h4(t1), h4(xs), b4(tmf('ssd_d'), 64), ALU.mult)
                    P.tt('dve', yv, yv, t1, ALU.add)
                    P.tt('dve', yv, yv, zs, ALU.mult)
                    groupnorm(yv, 1, tmf('ssd_ng'), None, 1e-5, False, small2, G[4][:, 768:1024])

                if 'gla' not in skip:
                    ps = PS[5]
                    tm_mm(ps[:, 0:512], GL0, 512)
                    qkv = G[2]
                    P.cp('act', qkv[:, 0:512], ps[:, 0:512])
                    vb = H[1][:, 0:256]
                    P.cp('pool', vb, qkv[:, 256:512])
                    ps = PS[6]
                    tm_mm(ps[:, 0:256], GL0 + 528, 256)
                    og = G[3][:, 0:256]
                    P.act(og, ps[:, 0:256], AF.Silu)
                    ps = PS[2]
                    cm_mm(ps[0:16, 0:128], GL0 + 512, 16)
                    gdT = G[3][0:16, 256:384]
                    P.cp('act', gdT, ps[0:16, 0:128])
                    ps = PS[3]
                    P.mm(ps[:, 0:128], gdT, gup[:])
                    la = G[3][:, 384:512]
                    P.tt('dve', la, ps[:, 0:128], tmf('gl_gb'), ALU.add)
                    P.act(la, la, AF.Exp, scale=-1.0)
                    P.act(la, la, AF.Ln, bias=1.0)
                    P.ts('dve', la, la, -1.0 / 16.0, ALU.mult)
                    ps = PS[4]
                    P.mm(ps[:, 0:128], TRI, la)
                    P.mm(ps[:, 128:256], ones[:], la)
                    P.mm(ps[:, 256:257], la, ones[:, 0:1])
                    ebc = G[3][:, 512:640]
                    P.act(ebc, ps[:, 0:128], AF.Exp)
                    enb = G[3][:, 640:768]
                    P.act(enb, ps[:, 0:128], AF.Exp, scale=-1.0)
                    kd = G[3][:, 768:896]
                    bc_sb = G[3][:, 896:1024]
                    P.cp('dve', bc_sb, ps[:, 0:128])
                    P.tt('dve', kd, ps[:, 128:256], bc_sb, ALU.subtract)
                    P.act(kd, kd, AF.Exp)
                    ebl = small[:, 32:33]
                    P.act(ebl, ps[:, 256:257], AF.Exp)
                    qd = G[4][:, 0:128]
                    P.stt('dve', qd, qkv[:, 0:128], 32 ** -0.5, ebc, ALU.mult, ALU.mult)
                    ki = G[4][:, 128:256]
                    P.tt('dve', ki, qkv[:, 128:256], enb, ALU.mult)
                    kdb = H[1][:, 256:384]
                    P.tt('dve', kdb, qkv[:, 128:256], kd, ALU.mult)
                    ps = PS[2]
                    P.tr(ps[:, 0:128], qd, ident)
                    P.tr(ps[:, 128:256], ki, ident)
                    qdT = H[1][:, 384:512]
                    P.cp('act', qdT, ps[:, 0:128])
                    pso = PS[3]
                    P.mm(pso[:, 0:256], qdT, gl_Sb[:], start=True, stop=False)
                    for h in range(4):
                        kim = H[1][:, 512:640]
                        P.ts('dve', kim, ps[:, 128:256], cst2[:, 1, 64 + h:65 + h], ALU.mult)
                        psa = PS[4 + h % 2]
                        P.mm(psa[:, 384:512], kim, qdT)
                        at = H[2][:, h * 128:(h + 1) * 128]
                        P.tt('dve', at, psa[:, 384:512], TRI, ALU.mult)
                        P.mm(pso[:, h * 64:(h + 1) * 64], at, vb[:, h * 64:(h + 1) * 64], start=False, stop=(h == 3))
                    yv = Y[:, 512:768]
                    P.cp('act', yv, pso[:, 0:256])
                    psn = PS[6]
                    P.mm(psn[:, 256:512], kdb, vb)
                    P.ts('dve', gl_S[:], gl_S[:], ebl, ALU.mult)
                    t1 = G[4][:, 256:512]
                    P.tt('dve', t1, psn[:, 256:512], GLMASK, ALU.mult)
                    P.tt('dve', gl_S[:], gl_S[:], t1, ALU.add)
                    P.cp('pool', gl_Sb[:], gl_S[:])
                    groupnorm(yv, 4, tmf('gl_ng'), None, 1e-5, False, small2, G[4][:, 768:1024])
                    P.tt('dve', yv, yv, og, ALU.mult)

                if 'mlstm' not in skip:
                    cmf = G[2]
                    for ci in range(4):
                        conv_silu(4 + ci, 20 + ci * 4, 36 + ci, ML0 + ci * 128, cmf[:, ci * 128:(ci + 1) * 128])
                    P.cp('pool', H[0][:, 0:512], cmf[:, 0:512])
                    ps = PS[4]
                    P.tr(ps[:, 0:128], cmf[:, 256:384], ident)
                    P.tr(ps[:, 128:256], cmf[:, 384:512], ident)
                    ktm = G[3][:, 0:256]
                    P.cp('act', ktm, ps[:, 0:256])
                    ps = PS[5]
                    tm_mm(ps[:, 0:264], ML0 + 512, 264)
                    vaug = H[1][:, 0:260].rearrange("p (h d) -> p h d", h=4)
                    P.cp('act', vaug[:, :, 0:64], h4(ps[:, 0:256]))
                    P.memset('pool', vaug[:, :, 64:65], 1.0)
                    ig = small[:, 48:52]
                    P.tt('dve', ig, ps[:, 256:260], tmf('ml_ib'), ALU.add)
                    lf = small[:, 52:56]
                    P.tt('dve', lf, ps[:, 260:264], tmf('ml_fb'), ALU.add)
                    P.act(lf, lf, AF.Exp, scale=-1.0)
                    P.act(lf, lf, AF.Ln, bias=1.0)
                    P.ts('dve', lf, lf, -1.0, ALU.mult)
                    ps = PS[6]
                    tm_mm(ps[:, 0:256], ML0 + 776, 256)
                    ogs = G[3][:, 256:512]
                    P.act(ogs, ps[:, 0:256], AF.Sigmoid)
                    ps = PS[2]
                    P.mm(ps[:, 0:4], TRI, lf)
                    P.mm(ps[:, 4:8], ones[:], lf)
                    bb = small[:, 56:60]
                    P.cp('dve', bb, ps[:, 0:4])
                    eb = small[:, 64:68]
                    P.act(eb, ps[:, 0:4], AF.Exp)
                    wst = small[:, 68:72]
                    P.tt('dve', wst, ps[:, 4:8], bb, ALU.subtract)
                    P.tt('dve', wst, wst, ig, ALU.add)
                    P.act(wst, wst, AF.Exp)
                    ebl4 = small[:, 72:76]
                    P.act(ebl4, ps[:, 4:8], AF.Exp)
                    pso = PS[3]
                    for h in range(4):
                        hp, hq = h // 2, (h % 2) * 64
                        qT_h = H[0][hq:hq + 64, hp * 128:(hp + 1) * 128]
                        kT_h = H[0][hq:hq + 64, 256 + hp * 128:256 + (hp + 1) * 128]
                        psl = PS[4 + h % 2]
                        lfb = G[4][:, 128:256]
                        P.cp('pool', lfb, lf[:, h:h + 1].to_broadcast([128, 128]))
                        P.mm(psl[:, 0:128], lfb, TRI)
                        dm = G[4][:, 256:384]
                        P.ts('dve', dm, psl[:, 0:128], bb[:, h:h + 1], ALU.subtract, 0.0, ALU.min)
                        P.act(dm, dm, AF.Exp, bias=ig[:, h:h + 1])
                        P.tt('pool', dm, dm, TRI, ALU.mult)
                        P.mm(psl[:, 128:256], kT_h, qT_h)
                        sT = H[2][:, h * 128:(h + 1) * 128]
                        P.stt('dve', sT, psl[:, 128:256], 0.125, dm, ALU.mult, ALU.mult)
                        P.mm(pso[:, h * 65:(h + 1) * 65], sT, vaug[:, h, :])
                        P.mm(PS[2][:, h * 65:(h + 1) * 65], qT_h, ml_Sb[hp][hq:hq + 64, :])
                    numf = G[4][:, 512:772]
                    num = h4(numf)
                    P.tt('dve', num, h4(PS[2][:, 0:260]), b4(eb, 65), ALU.mult)
                    P.stt('dve', numf, numf, 0.125, pso[:, 0:260], ALU.mult, ALU.add)
                    den = small[:, 76:80]
                    den3 = den.rearrange("p (h o) -> p h o", o=1)
                    P.stt('dve', den3, num[:, :, 64:65], -1.0, num[:, :, 64:65], ALU.mult, ALU.max)
                    P.ts('dve', den, den, 1.0, ALU.max)
                    P.recip(den, den)
                    yv = Y[:, 768:1024]
                    P.tt('dve', h4(yv), num[:, :, 0:64], b4(den, 64), ALU.mult)
                    P.tt('dve', yv, yv, ogs, ALU.mult)
                    kw = H[1][:, 512:768]
                    P.tt('dve', h4(kw), h4(ktm), b4(wst, 64), ALU.mult)
                    psn = PS[6]
                    for h in range(4):
                        hp, hq = h // 2, (h % 2) * 64
                        P.mm(psn[hq:hq + 64, 256 + hp * 65:256 + (hp + 1) * 65], kw[:, h * 64:(h + 1) * 64], vaug[:, h, :])
                    for hp in range(2):
                        for hh in range(2):
                            h = hp * 2 + hh
                            hq = hh * 64
                            P.ts('dve', ml_S[hp][hq:hq + 64, :], ml_S[hp][hq:hq + 64, :], ebl4[hq:hq + 64, h:h + 1], ALU.mult)
                        P.tt('dve', ml_S[hp][:], ml_S[hp][:], psn[:, 256 + hp * 65:256 + (hp + 1) * 65], ALU.add)
                        P.cp('pool', ml_Sb[hp][:], ml_S[hp][:])
                    groupnorm(yv, 4, tmf('ml_ng'), None, 1e-5, True, small2, G[4][:, 768:1024])

                if 'rwkv' not in skip:
                    rkv = G[2]
                    for half in range(2):
                        c0 = RW0 + half * 384
                        tm_mm(PS[4][:, 0:384], c0, 384)
                        tm_mm(PS[5][:, 0:384], c0, 384, shifted=True)
                        cur = G[4][:, 0:384]
                        P.cp('act', cur, PS[4][:, 0:384])
                        dl = G[4][:, 384:768]
                        P.tt('dve', dl, PS[5][:, 0:384], cur, ALU.subtract)
                        P.tt('pool', dl, dl, tmf('rw_mu')[:, half * 384:(half + 1) * 384], ALU.mult)
                        P.tt('dve', rkv[:, half * 384:(half + 1) * 384], cur, dl, ALU.add)
                    r_ = rkv[:, 0:256]
                    k_ = rkv[:, 256:512]
                    v_ = rkv[:, 512:768]
                    cm_mm(PS[2][:, 0:128], RW0 + 768, 128)
                    cm_mm(PS[2][:, 128:256], RW0 + 768, 128, shifted=True)
                    lo = G[3][:, 0:128]
                    P.cp('act', lo, PS[2][:, 0:128])
                    dl = G[3][:, 128:256]
                    P.tt('dve', dl, PS[2][:, 128:256], lo, ALU.subtract)
                    P.stt('dve', lo, dl, cmc[:, 40:41], lo, ALU.mult, ALU.add)
                    P.act(lo[0:32, :], lo[0:32, :], AF.Tanh)
                    P.act(lo[64:128, :], lo[64:128, :], AF.Sigmoid)
                    ps = PS[3]
                    P.mm(ps[:, 0:256], lo[0:32, :], lup[0:32, :])
                    ps2 = PS[6]
                    P.mm(ps2[:, 0:256], lo[32:64, :], lup[32:64, :])
                    P.mm(ps2[:, 256:512], lo[64:128, :], lup[64:128, :])
                    gg = G[3][:, 256:512]
                    P.cp('act', gg, ps2[:, 256:512])
                    lw = G[3][:, 512:768]
                    P.tt('dve', lw, ps[:, 0:256], tmf('rw_w0'), ALU.add)
                    P.act(lw, lw, AF.Exp, scale=-1.0)
                    P.act(lw, lw, AF.Ln, bias=1.0)
                    P.ts('dve', lw, lw, -1.0, ALU.mult, -0.5, ALU.add)
                    P.act(lw, lw, AF.Exp)
                    P.ts('dve', lw, lw, -1.0, ALU.mult)
                    av = G[3][:, 768:1024]
                    P.tt('dve', av, ps2[:, 0:256], tmf('rw_a0'), ALU.add)
                    P.act(av, av, AF.Sigmoid)
                    kk = G[4][:, 0:256]
                    P.tt('dve', kk, k_, tmf('rw_kk'), ALU.mult)
                    sq = G[4][:, 256:512]
                    P.tt('pool', sq, kk, kk, ALU.mult)
                    nrm = small[:, 80:84]
                    P.treduce('dve', nrm, h4(sq))
                    P.ts('dve', nrm, nrm, 1e-24, ALU.max)
                    P.act(nrm, nrm, AF.Ln)
                    P.act(nrm, nrm, AF.Exp, scale=-0.5)
                    P.tt('dve', h4(kk), h4(kk), b4(nrm, 64), ALU.mult)
                    km = G[4][:, 256:512]
                    P.ts('dve', km, av, -1.0, ALU.add)
                    P.tt('dve', km, km, tmf('rw_ka'), ALU.mult)
                    P.ts('dve', km, km, 1.0, ALU.add)
                    P.tt('dve', km, km, k_, ALU.mult)
                    bt_ = G[4][:, 512:768]
                    P.tt('pool', bt_, r_, km, ALU.mult)
                    P.tt('pool', bt_, bt_, tmf('rw_rk'), ALU.mult)
                    bon = small[:, 84:88]
                    P.treduce('dve', bon, h4(bt_))
                    ps = PS[3]
                    P.mm(ps[:, 0:256], BTRI, lw)
                    Wt = G[4][:, 512:768]
                    P.act(Wt, ps[:, 0:256], AF.Exp)
                    Wi = G[4][:, 768:1024]
                    P.act(Wi, ps[:, 0:256], AF.Exp, scale=-1.0)
                    Wp = G[5][:, 0:256]
                    cwsb = G[5][:, 256:512]
                    P.cp('dve', cwsb, ps[:, 0:256])
                    P.tt('dve', Wp, cwsb, lw, ALU.subtract)
                    P.act(Wp, Wp, AF.Exp)
                    ps = PS[2]
                    for hp in range(2):
                        for j in range(2):
                            P.mm(ps[:, hp * 2 + j:hp * 2 + j + 1], lw[j * 64:(j + 1) * 64, hp * 128:(hp + 1) * 128],
                                 ones[j * 64:(j + 1) * 64, 0:1])
                    WL = small[:, 88:92]
                    P.act(WL, ps[:, 0:4], AF.Exp)
                    rt = G[5][:, 256:512]
                    P.tt('dve', rt, r_, Wt, ALU.mult)
                    at_ = G[5][:, 512:768]
                    P.stt('dve', at_, kk, -1.0, Wp, ALU.mult, ALU.mult)
                    btl = G[5][:, 768:1024]
                    P.tt('dve', btl, kk, av, ALU.mult)
                    P.tt('dve', btl, btl, Wi, ALU.mult)
                    kt = G[4][:, 0:256]
                    P.tt('dve', kt, km, Wi, ALU.mult)
                    btb = H[1][:, 0:256]
                    ktb = H[1][:, 256:512]
                    vbb = H[1][:, 512:768]
                    P.cp('pool', btb, btl)
                    P.cp('pool', ktb, kt)
                    P.cp('pool', vbb, v_)
                    cmT = {}
                    for ai, (nm, arr) in enumerate([('r', rt), ('a', at_), ('b', btl), ('k', kt)]):
                        ps = PS[4 + ai % 2]
                        P.tr(ps[:, 0:128], arr[:, 0:128], ident)
                        P.tr(ps[:, 128:256], arr[:, 128:256], ident)
                        dstT = G[6][:, ai * 256:(ai + 1) * 256]
                        P.cp('act' if ai % 2 == 0 else 'dve', dstT, ps[:, 0:256])
                        cmT[nm] = dstT
                    o_rw = Y[:, 256:512]
                    for h in range(4):
                        hp, hq = h // 2, (h % 2) * 64
                        sl = slice(hq, hq + 64)
                        rT = cmT['r'][sl, hp * 128:(hp + 1) * 128]
                        aT = cmT['a'][sl, hp * 128:(hp + 1) * 128]
                        bT = cmT['b'][sl, hp * 128:(hp + 1) * 128]
                        kT = cmT['k'][sl, hp * 128:(hp + 1) * 128]
                        psA = PS[2]
                        P.mm(psA[:, 0:128], bT, aT)
                        P.mm(psA[:, 128:256], aT, bT)
                        P.mm(psA[:, 256:384], kT, aT)
                        psB = PS[3]
                        P.mm(psB[:, 0:128], bT, rT)
                        P.mm(psB[:, 128:256], kT, rT)
                        Pm = G[2][:, 768:896]
                        PTm = G[2][:, 896:1024]
                        TT = TTt[:]
                        P.tt('dve', Pm, psA[:, 0:128], BTRIS, ALU.mult)
                        P.tt('dve', PTm, psA[:, 128:256], BLOW, ALU.mult)
                        AakT = H[3][:, 0:128]
                        P.tt('dve', AakT, psA[:, 256:384], BTRIS, ALU.mult)
                        ArbT = H[3][:, 128:256]
                        P.tt('dve', ArbT, psB[:, 0:128], BTRI, ALU.mult)
                        ArkT = H[3][:, 256:384]
                        P.tt('dve', ArkT, psB[:, 128:256], BTRI, ALU.mult)
                        P.tt('dve', TT, Pm, ident, ALU.add)
                        for step in range(5):
                            psq = PS[4]
                            P.mm(psq[:, 0:128], PTm, Pm)
                            P.mm(psq[:, 128:256], Pm, PTm)
                            P.cp('dve', Pm, psq[:, 0:128])
                            P.cp('act', PTm, psq[:, 128:256])
                            psq2 = PS[5]
                            P.mm(psq2[:, 0:128], PTm, TT)
                            P.tt('dve', TT, TT, psq2[:, 0:128], ALU.add)
                        TTb = H[3][:, 384:512]
                        P.cp('pool', TTb, TT)
                        aTb = H[3][:, 512:640]
                        rTb = H[3][:, 640:768]
                        P.cp('pool', aTb[sl, :], aT)
                        P.cp('pool', rTb[sl, :], rT)
                        for j in range(2):
                            js = slice(j * 64, (j + 1) * 64)
                            psx = PS[6]
                            vh = vbb[js, h * 64:(h + 1) * 64]
                            P.mm(psx[js, 0:64], aTb[sl, js], rw_Sb[hp][sl, :], start=True, stop=False)
                            P.mm(psx[js, 0:64], AakT[js, js], vh, start=False, stop=True)
                            X1 = H[4][:, 0:64]
                            P.cp('act', X1[js, :], psx[js, 0:64])
                            P.mm(psx[js, 64:128], TTb[js, js], X1[js, :])
                            Ub = H[4][:, 64:128]
                            P.cp('act', Ub[js, :], psx[js, 64:128])
                            P.mm(psx[js, 128:192], rTb[sl, js], rw_Sb[hp][sl, :], start=True, stop=False)
                            P.mm(psx[js, 128:192], ArbT[js, js], Ub[js, :], start=False, stop=False)
                            P.mm(psx[js, 128:192], ArkT[js, js], vh, start=False, stop=True)
                            P.cp('dve', o_rw[js, h * 64:(h + 1) * 64], psx[js, 128:192])
                            P.mm(psx[sl, 192:256], btb[js, h * 64:(h + 1) * 64], Ub[js, :], start=True, stop=False)
                            P.mm(psx[sl, 192:256], ktb[js, h * 64:(h + 1) * 64], vh, start=False, stop=True)
                            P.tt('dve', rw_S[hp][sl, :], rw_S[hp][sl, :], psx[sl, 192:256], ALU.add)
                            P.ts('dve', rw_S[hp][sl, :], rw_S[hp][sl, :], WL[sl, hp * 2 + j:hp * 2 + j + 1], ALU.mult)
                            P.cp('pool', rw_Sb[hp][sl, :], rw_S[hp][sl, :])
                    groupnorm(o_rw, 4, tmf('rw_lng'), tmf('rw_lnb'), 64e-5, True, small2, G[4][:, 768:1024])
                    t1 = G[4][:, 512:768]
                    P.tt('dve', h4(t1), h4(v_), b4(bon, 64), ALU.mult)
                    P.tt('dve', o_rw, o_rw, t1, ALU.add)
                    P.tt('dve', o_rw, o_rw, gg, ALU.mult)
                if debug and layer == 0:
                    for nm, c0 in [('d_ssd', 0), ('d_rwkv', 256), ('d_gla', 512), ('d_mlstm', 768)]:
                        P.dma(dbg[nm][c * 128:(c + 1) * 128, :], Y[:, c0:c0 + 256], comm=True)
                if stop_after == 'mixers':
                    continue

                YT = H[2].rearrange("p (k n) -> p k n", k=8)
                for k in range(8):
                    P.tr(PS[2 + k // 4][:, (k % 4) * 128:(k % 4 + 1) * 128], Y[:, k * 128:(k + 1) * 128], ident)
                for hf in range(2):
                    P.cp('act' if hf == 0 else 'dve', YT[:, hf * 4:(hf + 1) * 4, :],
                         PS[2 + hf][:, :].rearrange("p (k n) -> p k n", k=4))
                x1 = G[2]
                for hf in range(2):
                    ps = PS[4 + hf]
                    for k in range(8):
                        P.mm(ps[:, :], YT[:, k, :], WO[:, k, hf * 512:(hf + 1) * 512], start=(k == 0), stop=(k == 7))
                    P.stt('dve', x1[:, hf * 512:(hf + 1) * 512], xt[:, hf * 512:(hf + 1) * 512], ALPHA, ps[:, :], ALU.mult, ALU.add)
                layernorm(x1[:], x1[:], 'ln1_g', 'ln1_b', small2)
                P.dma(x1buf[c * 128:(c + 1) * 128, :], x1[:], comm=True)
                if debug and layer == 0:
                    P.dma(dbg['d_x1'][c * 128:(c + 1) * 128, :], x1[:], comm=True)
                x1b = H[0]
                P.cp('pool', x1b[:], x1[:])
                x1T = G[3].rearrange("p (k n) -> p k n", k=8)
                for k in range(8):
                    P.tr(PS[2 + k // 4][:, (k % 4) * 128:(k % 4 + 1) * 128], x1[:, k * 128:(k + 1) * 128], ident)
                for hf in range(2):
                    P.cp('act' if hf == 0 else 'dve', x1T[:, hf * 4:(hf + 1) * 4, :],
                         PS[2 + hf][:, :].rearrange("p (k n) -> p k n", k=4))
                ps = PS[6]
                for k in range(8):
                    P.mm(ps[:, 0:32], x1T[:, k, :], rtw[:, k, :], start=(k == 0), stop=(k == 7))
                lg = rsc[:, 0:32]
                P.tt('dve', lg, ps[:, 0:32], tmf('rt_b'), ALU.add)
                mx8 = rsc[:, 32:40]
                P.op('dve', lambda e, mx8=mx8, lg=lg: e.max(out=mx8, in_=lg), reads=K(lg), writes=K(mx8))
                P.op('dve', lambda e, mx8=mx8, lg=lg: e.max_index(out=idx8[:], in_max=mx8, in_values=lg),
                     reads=K(lg, mx8), writes=K(idx8))
                msk = rsc[:, 40:72]
                P.ts('dve', msk, lg, mx8[:, 3:4], ALU.is_ge)
                nmx = rsc[:, 72:73]
                P.ts('dve', nmx, mx8[:, 0:1], -1.0, ALU.mult)
                ex = rsc[:, 76:108]
                P.act(ex, lg, AF.Exp, bias=nmx)
                P.tt('dve', ex, ex, msk, ALU.mult)
                ssum = rsc[:, 73:74]
                P.rsum('dve', ssum, ex)
                P.recip(ssum, ssum)
                P.ts('dve', ex, ex, ssum, ALU.mult)
                ps = PS[5]
                P.mm(ps[:, 0:32], TRIS, msk)
                P.mm(ps[:, 32:64], ones[:], msk)
                pos = rsc[:, 108:140]
                P.tt('dve', pos, ps[:, 0:32], cntb[:], ALU.add)
                P.tt('dve', cntb[:], cntb[:], ps[:, 32:64], ALU.add)
                idxf = rsc[:, 140:144]
                P.cp('dve', idxf, idx8[:, 0:4])
                destf = rsc[:, 144:148]
                for k4 in range(4):
                    oh = rsc[:, 152:184]
                    P.ts('dve', oh, IOTA32, idxf[:, k4:k4 + 1], ALU.is_equal)
                    tmp = rsc[:, 184:216]
                    P.tt('dve', tmp, oh, pos, ALU.mult)
                    P.rsum('dve', destf[:, k4:k4 + 1], tmp)
                    P.tt('dve', tmp, oh, ex, ALU.mult)
                    P.rsum('dve', gate_all[:, c, k4:k4 + 1], tmp)
                ovf = rsc[:, 148:152]
                P.ts('dve', ovf, destf, float(CAP), ALU.is_ge, float(NE * CAP), ALU.mult)
                P.stt('dve', destf, idxf, float(CAP), destf, ALU.mult, ALU.add)
                P.tt('dve', destf, destf, ovf, ALU.add)
                P.cp('dve', dest_all[:, c, :], destf)
                for k4 in range(4):
                    P.scatter(xg, dest_all[:, c, k4:k4 + 1], x1b[:], NE * CAP - 1)
                if debug and layer == 0:
                    P.dma(dbg['d_logits'][c * 128:(c + 1) * 128, :], lg, comm=True)
                    P.dma(dbg['d_dest'][c * 128:(c + 1) * 128, :], destf, comm=True)
                    P.dma(dbg['d_gate'][c * 128:(c + 1) * 128, :], gate_all[:, c, :], comm=True)
            if stop_after in ('mixers', 'phaseA'):
                continue

            EB = [R0, R1]

            def eb_views(i):
                gu = EB[i][:, 0:16384].rearrange("p (k n) -> p k n", k=8)
                dn = EB[i][:, 16384:24576].rearrange("p (k n) -> p k n", k=8)
                return gu, dn

            def load_expert(e):
                gu, dn = eb_views(e % 2)
                for k in range(8):
                    P.dmac(gu[:, k, :], w_gu[layer, e, k * 128:(k + 1) * 128, :])
                for k in range(8):
                    P.dmac(dn[:, k, :], w_dn[layer, e, k * 128:(k + 1) * 128, :])
                P.dma(bgu[e % 2][:], b_gu[layer, e])

            bdn = G[6]
            load_expert(0)
            for e in range(NE):
                if e + 1 < NE:
                    load_expert(e + 1)
                gu, dn = eb_views(e % 2)
                P.dma(bdn[:], b_dn[layer, e].partition_broadcast(128))
                for grp in range(CAP // 256):
                    xgT = G[(grp % 2) * 2][:].bitcast(BF16).rearrange("p (k n) -> p k n", k=8)
                    actT = G[(grp % 2) * 2 + 1][:].bitcast(BF16).rearrange("p (k n) -> p k n", k=8)
                    base = e * CAP + grp * 256
                    for stl in range(2):
                        xr = H[stl]
                        P.dma(xr[:], xg[base + stl * 128:base + (stl + 1) * 128, :])
                        for k in range(8):
                            P.tr(PSB[:, k * 128:(k + 1) * 128], xr[:, k * 128:(k + 1) * 128], identb[:])
                        P.cp('act' if stl == 0 else 'dve', xgT[:, :, stl * 128:(stl + 1) * 128],
                             PSB[:, :].rearrange("p (k n) -> p k n", k=8))
                    for fc in range(8):
                        psg = PS[(fc % 2) * 2]
                        psl = PS[(fc % 2) * 2 + 1]
                        for k in range(8):
                            P.mm(psg[:, 0:256], gu[:, k, fc * 128:(fc + 1) * 128], xgT[:, k, :], start=(k == 0), stop=(k == 7))
                        for k in range(8):
                            P.mm(psl[:, 0:256], gu[:, k, 1024 + fc * 128:1024 + (fc + 1) * 128], xgT[:, k, :], start=(k == 0), stop=(k == 7))
                        tg = G[4][:, (fc % 2) * 512:(fc % 2) * 512 + 256]
                        tl = G[4][:, (fc % 2) * 512 + 256:(fc % 2) * 512 + 512]
                        bg = bgu[e % 2]
                        P.ts('dve', tg, psg[:, 0:256], bg[:, fc:fc + 1], ALU.add, 7.0, ALU.min)
                        sg = psg[:, 256:512]
                        P.act(sg, tg, AF.Sigmoid, scale=1.702)
                        P.ts('dve', tl, psl[:, 0:256], bg[:, 8 + fc:9 + fc], ALU.add, 7.0, ALU.min)
                        P.ts('dve', tl, tl, -7.0, ALU.max, 1.0, ALU.add)
                        P.tt('dve', tg, tg, sg, ALU.mult)
                        P.tt('dve', actT[:, fc, :], tl, tg, ALU.mult)
                    for stl in range(2):
                        yb = G[5]
                        for hf in range(2):
                            ps = PS[4 + hf]
                            for k in range(8):
                                P.mm(ps[:, :], actT[:, k, stl * 128:(stl + 1) * 128], dn[:, k, hf * 512:(hf + 1) * 512],
                                     start=(k == 0), stop=(k == 7))
                            P.tt('dve', yb[:, hf * 512:(hf + 1) * 512], ps[:, :], bdn[:, hf * 512:(hf + 1) * 512], ALU.add)
                        P.dma(yg[base + stl * 128:base + (stl + 1) * 128, :], yb[:], comm=True)
            if stop_after == 'phaseB':
                continue

            load_r1(layer)
            for c in range(ntiles):
                x1 = G[2]
                P.dma(x1[:], x1buf[c * 128:(c + 1) * 128, :])
                hh = G[0]
                P.ts('dve', hh[:], x1[:], ALPHA, ALU.mult)
                for k4 in range(4):
                    gt = G[3 + (k4 % 2)]
                    P.memset('dve', gt[:], 0.0)
                    P.gather(gt[:], yg, dest_all[:, c, k4:k4 + 1], NE * CAP - 1)
                    P.stt('dve', hh[:], gt[:], gate_all[:, c, k4:k4 + 1], hh[:], ALU.mult, ALU.add)
                if debug and layer == 0:
                    ff = G[5]
                    P.stt('dve', ff[:], x1[:], -ALPHA, hh[:], ALU.mult, ALU.add)
                    P.dma(dbg['d_ffn'][c * 128:(c + 1) * 128, :], ff[:], comm=True)
                hT = H[2].rearrange("p (k n) -> p k n", k=8)
                for k in range(8):
                    P.tr(PS[k // 4][:, (k % 4) * 128:(k % 4 + 1) * 128], hh[:, k * 128:(k + 1) * 128], ident)
                for hf in range(2):
                    P.cp('act' if hf == 0 else 'dve', hT[:, hf * 4:(hf + 1) * 4, :],
                         PS[hf][:, :].rearrange("p (k n) -> p k n", k=4))
                pt = G[5][:, 0:256]
                P.dma(pt, p_in[layer, c * 128:(c + 1) * 128, :])
                P.tr(PS[2][:, 0:128], pt[:, 0:128], ident)
                P.tr(PS[2][:, 128:256], pt[:, 128:256], ident)
                pT = H[3][:, 0:256].rearrange("p (k n) -> p k n", k=2)
                P.cp('act', pT, PS[2][:, 0:256].rearrange("p (k n) -> p k n", k=2))
                sig = G[1]
                for hf in range(2):
                    ps = PS[3 + hf]
                    for k in range(8):
                        P.mm(ps[:, :], hT[:, k, :], PG[:, k, hf * 512:(hf + 1) * 512], start=(k == 0), stop=(k == 7))
                    P.act(sig[:, hf * 512:(hf + 1) * 512], ps[:, :], AF.Sigmoid)
                    ps2 = PS[5 + hf]
                    for k in range(2):
                        P.mm(ps2[:, :], pT[:, k, :], PP[:, k, hf * 512:(hf + 1) * 512], start=(k == 0), stop=(k == 1))
                    P.tt('dve', sig[:, hf * 512:(hf + 1) * 512], sig[:, hf * 512:(hf + 1) * 512], ps2[:, :], ALU.mult)
                P.tt('dve', hh[:], hh[:], sig[:], ALU.add)
                layernorm(hh[:], hh[:], 'ln2_g', 'ln2_b', small2)
                P.dma(xdst[c * 128:(c + 1) * 128, :], hh[:], comm=True)
        print('total ops recorded', P.nops)
        P.emit()
    return nc


def _consts():
    s = np.arange(128)[:, None]
    t = np.arange(128)[None, :]
    blk = (s // 64) == (t // 64)
    c = np.zeros((128, 6, 128), np.float32)
    c[:, 0] = np.eye(128)
    c[:, 1] = (s <= t)
    c[:, 2] = (s < t)
    c[:, 3] = blk & (s <= t)
    c[:, 4] = blk & (s < t)
    c[:, 5] = blk & (t < s)
    c2 = np.zeros((128, 2, 256), np.float32)
    c2[:, 0] = (np.arange(128)[:, None] // 32) == (np.arange(256)[None, :] // 64)
    c2[:, 1, 0:32] = np.arange(32)[None, :]
    c2[:, 1, 64:68] = (np.arange(128)[:, None] // 32) == np.arange(4)[None, :]
    return c, c2


def prep_inputs(inp):
    f = lambda k: np.asarray(inp[k], dtype=np.float32)
    L = DEPTH
    tm = np.zeros((L, 1, TM_W), np.float32)
    src = {
        'dt_bias': f('ssd_dt_bias'), 'a_log': f('ssd_a_log'), 'ssd_d': f('ssd_d'), 'ssd_ng': f('ssd_norm_g'),
        'rw_mu': f('rwkv_mu')[:, 0:768], 'rw_w0': f('rwkv_w0'), 'rw_a0': f('rwkv_a0'), 'rw_kk': f('rwkv_k_k'),
        'rw_ka': f('rwkv_k_a'), 'rw_rk': f('rwkv_r_k').reshape(L, 256), 'rw_lng': f('rwkv_ln_g'), 'rw_lnb': f('rwkv_ln_b'),
        'gl_gb': f('gla_gate_b'), 'gl_ng': f('gla_norm_g'),
        'ml_ib': f('mlstm_i_b'), 'ml_fb': f('mlstm_f_b'), 'ml_ng': f('mlstm_norm_g'),
        'ln1_g': f('ln1_g'), 'ln1_b': f('ln1_b'), 'ln2_g': f('ln2_g'), 'ln2_b': f('ln2_b'), 'rt_b': f('router_b'),
    }
    for nm, (o, w) in TM_OFF.items():
        tm[:, 0, o:o + w] = src[nm]
    cm = np.zeros((L, 128, CM_W), np.float32)
    scw = f('ssd_conv_w'); scb = f('ssd_conv_b'); mcw = f('mlstm_conv_w'); mcb = f('mlstm_conv_b')
    for ci in range(4):
        for j in range(4):
            cm[:, :, ci * 4 + j] = scw[:, j, ci * 128:(ci + 1) * 128]
            cm[:, :, 20 + ci * 4 + j] = mcw[:, j, ci * 128:(ci + 1) * 128]
        cm[:, :, 16 + ci] = scb[:, ci * 128:(ci + 1) * 128]
        cm[:, :, 36 + ci] = mcb[:, ci * 128:(ci + 1) * 128]
    cm[:, :, 40] = f('rwkv_mu')[:, 768:896]
    lora = np.concatenate([f('rwkv_w_up'), f('rwkv_a_up'), f('rwkv_g_up')], axis=1)
    bgu = np.ascontiguousarray(f('exp_b_gu').reshape(L, NE, 16, 128).transpose(0, 1, 3, 2))
    bdn = f('exp_b_down').reshape(L, NE, 1, D)
    c, c2 = _consts()
    shared = {
        'w_in': f('w_in'), 'w_out': f('w_out'), 'tmrow': tm, 'cmcol': cm, 'lora_up': np.ascontiguousarray(lora),
        'gla_up': f('gla_gate_up'), 'router_w': f('router_w'), 'w_gu': f('exp_w_gu'), 'b_gu': bgu,
        'w_dn': f('exp_w_down'), 'b_dn': bdn, 'ple_g': f('ple_gate_w'), 'ple_p': f('ple_proj'),
        'consts': c, 'consts2': c2,
    }
    return shared


_NC_CACHE = {}


def kernel(**inputs):
    shared = prep_inputs(inputs)
    x = np.asarray(inputs['x'], dtype=np.float32)
    p = np.asarray(inputs['p'], dtype=np.float32)
    if 'nc' not in _NC_CACHE:
        _NC_CACHE['nc'] = build()
    nc = _NC_CACHE['nc']
    in_maps = []
    for core in range(8):
        b = core % 4
        m = dict(shared)
        m['x'] = np.ascontiguousarray(x[b])
        m['p'] = np.ascontiguousarray(p[:, b])
        in_maps.append(m)
    res = run_bass_kernel_spmd(nc, in_maps, core_ids=list(range(8)))
    outs = [res.results[b]['out'] for b in range(4)]
    return np.stack(outs, axis=0).astype(np.float32)
```

```python
import math
from contextlib import ExitStack
import numpy as np
import concourse.bass as bass
import concourse.mybir as mybir
from concourse.bass_utils import run_bass_kernel_spmd

F32 = mybir.dt.float32
BF16 = mybir.dt.bfloat16
I32 = mybir.dt.int32
U32 = mybir.dt.uint32
AF = mybir.ActivationFunctionType
ALU = mybir.AluOpType
AX = mybir.AxisListType

D = 1024
T = 4096
NT = T // 128
DEPTH = 4
NCOL = 3484
NE = 32
CAP = 768
ALPHA = (2 * DEPTH) ** 0.25
LN_EPS = 1e-5
SSD0, RW0, GL0, ML0 = 0, 772, 1668, 2452


def _isap(a):
    return hasattr(a, 'tensor')


def K(*aps):
    return list({a.tensor.name for a in aps if _isap(a)})


class Prog:
    ENGS = ['pe', 'act', 'dve', 'pool', 'sp']
    NROT = 4
    CH = 1500
    CHC = 4000

    def __init__(self, nc):
        self.nc = nc
        self.ops = {e: [] for e in self.ENGS}
        self.cnt = {}
        self.last_w = {}
        self.readers = {}
        self.waited = {e: {} for e in self.ENGS}
        self.ndma = {e: 0 for e in self.ENGS}
        self.sems = {}
        self.cwr = {}
        self.pe_bank = {}
        self._bound_reg = None
        self._cur = None
        self._streams = {}
        self.nops = 0
        self.maxops = None

    def op(self, eng, fn, reads=(), writes=(), dma=False, cwrites=(), pebank=None):
        if self._cur is not None:
            self._streams[self._cur].append((eng, fn, reads, writes, dma, cwrites, pebank))
            return
        self.nops += 1
        if self.maxops is not None and self.nops > self.maxops:
            return
        selfwait = []
        if pebank is not None:
            bank, r0, r1 = pebank
            last = self.pe_bank.get(bank)
            if last is not None:
                (l0, l1), lidx = last
                if r1 <= l0 or l1 <= r0:
                    selfwait = [lidx]
            self.pe_bank[bank] = ((r0, r1), self.cnt.get('pe', 0))
        pr = [r for r in reads if r.startswith('ps')]
        if pr:
            reads = [r for r in reads if not r.startswith('ps')]
            writes = list(writes) + [r for r in pr if r not in writes]
        if dma:
            j = self.ndma[eng]
            self.ndma[eng] += 1
            counter = 'dma_%s_%d' % (eng, j % self.NROT)
        else:
            counter = eng
        idx = self.cnt.get(counter, 0)
        self.cnt[counter] = idx + 1
        deps = {}

        def need(c, j):
            if deps.get(c, -1) < j:
                deps[c] = j
        for r in reads:
            if r in self.last_w:
                need(*self.last_w[r])
            for c, j in self.cwr.get(r, {}).items():
                need(c, j)
        for w in writes:
            if w in self.last_w:
                need(*self.last_w[w])
            for c, j in self.readers.get(w, {}).items():
                need(c, j)
            for c, j in self.cwr.get(w, {}).items():
                need(c, j)
        for w in cwrites:
            if w in self.last_w:
                need(*self.last_w[w])
            for c, j in self.readers.get(w, {}).items():
                need(c, j)
        waits = []
        for j in selfwait:
            if self.waited[eng].get(counter, -1) < j:
                self.waited[eng][counter] = j
                waits.append((counter, j))
        for c, j in deps.items():
            if c == counter and eng == 'pe' and not dma:
                continue
            if self.waited[eng].get(c, -1) >= j:
                continue
            self.waited[eng][c] = j
            waits.append((c, j))
        self.ops[eng].append((waits, fn, counter, idx))
        for w in writes:
            self.last_w[w] = (counter, idx)
            self.readers[w] = {}
            self.cwr[w] = {}
        for w in cwrites:
            d = self.cwr.setdefault(w, {})
            if d.get(counter, -1) < idx:
                d[counter] = idx
        for r in reads:
            if r in writes:
                continue
            d = self.readers.setdefault(r, {})
            if d.get(counter, -1) < idx:
                d[counter] = idx


    def begin(self, name):
        self._cur = name
        self._streams.setdefault(name, [])

    def merge(self):
        streams = {k: v for k, v in self._streams.items() if v}
        self._cur = None
        self._streams = {}
        pos = {k: 0 for k in streams}
        total = sum(len(v) for v in streams.values())
        for _ in range(total):
            k = min((k for k in streams if pos[k] < len(streams[k])), key=lambda k: pos[k] / len(streams[k]))
            eng, fn, reads, writes, dma, cwrites, pebank = streams[k][pos[k]]
            pos[k] += 1
            self.op(eng, fn, reads=reads, writes=writes, dma=dma, cwrites=cwrites, pebank=pebank)

    @staticmethod
    def _prange(ap):
        st, n = ap.ap[0]
        p0 = (ap.offset // st) if st else 0
        return (p0, p0 + n)

    def mm(self, out, lhsT, rhs, start=True, stop=True):
        r0, r1 = self._prange(lhsT)
        self.op('pe', lambda e: e.matmul(out, lhsT=lhsT, rhs=rhs, start=start, stop=stop),
                reads=K(lhsT, rhs), writes=K(out), pebank=(out.tensor.name, r0, r1))

    def tr(self, out, in_, ident):
        r0, r1 = self._prange(in_)
        self.op('pe', lambda e: e.transpose(out, in_, ident), reads=K(in_, ident), writes=K(out),
                pebank=(out.tensor.name, r0, r1))

    def act(self, out, in_, func, bias=None, scale=None, accum_out=None, eng='act'):
        kw = {}
        if bias is not None:
            kw['bias'] = bias
        if scale is not None:
            kw['scale'] = scale
        if accum_out is not None:
            kw['accum_out'] = accum_out
        self.op('act', lambda e: e.activation(out=out, in_=in_, func=func, **kw),
                reads=K(in_, bias, scale), writes=K(out, accum_out))

    def cp(self, eng, out, in_):
        if eng == 'act':
            self.op('act', lambda e: e.copy(out=out, in_=in_), reads=K(in_), writes=K(out))
        else:
            self.op(eng, lambda e: e.tensor_copy(out=out, in_=in_), reads=K(in_), writes=K(out))

    def tt(self, eng, out, in0, in1, op):
        self.op(eng, lambda e: e.tensor_tensor(out=out, in0=in0, in1=in1, op=op),
                reads=K(in0, in1), writes=K(out))

    def ts(self, eng, out, in0, s1, op0, s2=None, op1=None, accum_out=None):
        kw = {}
        if op1 is not None:
            kw['op1'] = op1
        if accum_out is not None:
            kw['accum_out'] = accum_out
        self.op(eng, lambda e: e.tensor_scalar(out=out, in0=in0, scalar1=s1, scalar2=s2, op0=op0, **kw),
                reads=K(in0, s1, s2), writes=K(out, accum_out))

    def stt(self, eng, out, in0, scalar, in1, op0, op1):
        self.op(eng, lambda e: e.scalar_tensor_tensor(out=out, in0=in0, scalar=scalar, in1=in1, op0=op0, op1=op1),
                reads=K(in0, scalar, in1), writes=K(out))

    def rsum(self, eng, out, in_):
        self.op(eng, lambda e: e.reduce_sum(out=out, in_=in_, axis=AX.X), reads=K(in_), writes=K(out))

    def memset(self, eng, out, val):
        self.op(eng, lambda e: e.memset(out, val), writes=K(out))

    def dma(self, out, in_, eng='sp', comm=False):
        if comm:
            self.op(eng, lambda e: e.dma_start(out=out, in_=in_), reads=K(in_), cwrites=K(out), dma=True)
        else:
            self.op(eng, lambda e: e.dma_start(out=out, in_=in_), reads=K(in_), writes=K(out), dma=True)


    def dmac(self, out, in_):
        self.op('pool', lambda e: e.dma_start(out=out, in_=in_), reads=K(in_), cwrites=K(out), dma=True)

    def _breg(self, e, bound):
        if self._bound_reg is None:
            self._bound_reg = (bound, e.to_reg(bound))
        assert self._bound_reg[0] == bound
        return self._bound_reg[1]

    def scatter(self, out_dram, idx_ap, in_sb, bound):
        self.op('pool', lambda e: e.indirect_dma_start(
            out=out_dram, out_offset=bass.IndirectOffsetOnAxis(ap=idx_ap, axis=0),
            in_=in_sb, in_offset=None, bounds_check=self._breg(e, bound), oob_is_err=False),
            reads=K(in_sb, idx_ap), cwrites=K(out_dram), dma=True)

    def gather(self, out_sb, in_dram, idx_ap, bound):
        self.op('pool', lambda e: e.indirect_dma_start(
            out=out_sb, out_offset=None, in_=in_dram,
            in_offset=bass.IndirectOffsetOnAxis(ap=idx_ap, axis=0), bounds_check=self._breg(e, bound), oob_is_err=False),
            reads=K(in_dram, idx_ap), writes=K(out_sb), dma=True)

    def treduce(self, eng, out, in_, op=None):
        op = ALU.add if op is None else op
        self.op(eng, lambda e: e.tensor_reduce(out=out, in_=in_, axis=AX.X, op=op), reads=K(in_), writes=K(out))

    def recip(self, out, in_):
        self.op('dve', lambda e: e.reciprocal(out=out, in_=in_), reads=K(in_), writes=K(out))

    def _ch(self, counter):
        return self.CH if counter.startswith('dma_') else self.CHC

    def _sem(self, counter, idx):
        return self.sems[(counter, idx // self._ch(counter))]

    def _val(self, counter, idx):
        inc = 16 if counter.startswith('dma_') else 1
        return (idx % self._ch(counter) + 1) * inc

    def emit(self):
        nc = self.nc
        with ExitStack() as st:
            for counter, n in self.cnt.items():
                ch = self._ch(counter)
                for k in range((n + ch - 1) // ch):
                    self.sems[(counter, k)] = st.enter_context(nc.semaphore('s_%s_%d' % (counter, k)))
            block = st.enter_context(nc.Block())

            def mk(engname):
                def body(e):
                    for waits, fn, counter, idx in self.ops[engname]:
                        for c, j in waits:
                            e.wait_ge(self._sem(c, j), self._val(c, j))
                        ins = fn(e)
                        ins.then_inc(self._sem(counter, idx), 16 if counter.startswith('dma_') else 1)
                    if engname == 'sp':
                        for c, n in self.cnt.items():
                            if n > 0:
                                e.wait_ge(self._sem(c, n - 1), self._val(c, n - 1))
                return body
            block.tensor(mk('pe'))
            block.scalar(mk('act'))
            block.vector(mk('dve'))
            block.gpsimd(mk('pool'))
            block.sync(mk('sp'))


TM_FIELDS = [
    ('dt_bias', 4), ('a_log', 4), ('ssd_d', 4), ('ssd_ng', 256),
    ('rw_mu', 768), ('rw_w0', 256), ('rw_a0', 256), ('rw_kk', 256), ('rw_ka', 256), ('rw_rk', 256),
    ('rw_lng', 256), ('rw_lnb', 256),
    ('gl_gb', 128), ('gl_ng', 256),
    ('ml_ib', 4), ('ml_fb', 4), ('ml_ng', 256),
    ('ln1_g', 1024), ('ln1_b', 1024), ('ln2_g', 1024), ('ln2_b', 1024), ('rt_b', 32),
]
TM_OFF = {}
_o = 0
for _n, _w in TM_FIELDS:
    TM_OFF[_n] = (_o, _w)
    _o += _w
TM_W = _o
CM_W = 41


def h4(ap, h=4):
    return ap.rearrange("p (h d) -> p h d", h=h)


def b4(ap, w, h=4):
    return ap.rearrange("p (h o) -> p h o", o=1).to_broadcast([128, h, w])


import os
CONVPS = int(os.environ.get("CONVPS", "0"))


def build(n_layers=DEPTH, debug=False, stop_after=None, ntiles=NT, skip=(), maxops=None):
    nc = bass.Bass("TRN2", target_bir_lowering=False)
    P = Prog(nc)
    P.maxops = maxops
    LD = n_layers
    NED = 1 if stop_after in ('mixers', 'phaseA') else NE

    def din(name, shape, dt=F32):
        return nc.dram_tensor(name, list(shape), dt, kind="ExternalInput").ap()
    x_in = din('x', [T, D])
    p_in = din('p', [LD, T, 256])
    w_in = din('w_in', [LD, D, NCOL])
    w_out = din('w_out', [LD, D, D])
    tmrow = din('tmrow', [LD, 1, TM_W])
    cmcol = din('cmcol', [LD, 128, CM_W])
    lora_up = din('lora_up', [LD, 128, 256])
    gla_up = din('gla_up', [LD, 16, 128])
    router_w = din('router_w', [LD, D, NE])
    w_gu = din('w_gu', [LD, NED, D, 2 * D])
    b_gu = din('b_gu', [LD, NE, 128, 16])
    w_dn = din('w_dn', [LD, NED, D, D])
    b_dn = din('b_dn', [LD, NE, 1, D])
    ple_g = din('ple_g', [LD, D, D])
    ple_p = din('ple_p', [LD, 256, D])
    consts = din('consts', [128, 6, 128])
    consts2 = din('consts2', [128, 324])
    out = nc.dram_tensor('out', [T, D], F32, kind="ExternalOutput").ap()
    xbuf = nc.dram_tensor('xbuf', [T, D], F32).ap()
    x1buf = nc.dram_tensor('x1buf', [T, D], F32).ap()
    xg = nc.dram_tensor('xg', [NE * CAP, D], BF16).ap()
    yg = nc.dram_tensor('yg', [NE * CAP, D], F32).ap()
    dbg = {}
    if debug:
        for nm, w in [('d_ssd', 256), ('d_rwkv', 256), ('d_gla', 256), ('d_mlstm', 256), ('d_x1', 1024),
                      ('d_ffn', 1024), ('d_logits', 32), ('d_dest', 4), ('d_gate', 4)]:
            dbg[nm] = nc.dram_tensor(nm, [T, w], F32, kind="ExternalOutput").ap()

    with ExitStack() as st:
        def sb(name, shape, dt=F32):
            return st.enter_context(nc.sbuf_tensor(name, list(shape), dt))

        def psb(name, shape, dt=F32):
            return st.enter_context(nc.psum_tensor(name, list(shape), dt))

        R0 = sb('R0', [128, 8 * NCOL], BF16)
        R1 = sb('R1', [128, 24576], BF16)
        tmc = sb('tmc', [128, TM_W])
        cmc = sb('cmc', [128, CM_W])
        cst = sb('cst', [128, 6, 128])
        cst2 = sb('cst2', [128, 324])
        identb = sb('identb', [128, 128], BF16)
        lup = sb('lup', [128, 256])
        gup = sb('gup', [16, 128])
        rtw = sb('rtw', [128, 8, NE])
        ones = sb('ones', [128, 128])
        G = [sb('G%d' % i, [128, 1024]) for i in range(7)]
        H = [sb('H%d' % i, [128, 1024], BF16) for i in range(5)]
        xTe = [sb('xTe%d' % i, [128, 8, 129], BF16) for i in range(2)]
        cbuf = [sb('cbuf%d' % i, [128, 131]) for i in range(8)]
        small = sb('small', [128, 128])
        small2 = sb('small2', [128, 32])
        smallS = sb('smallS', [128, 80])
        small2S = sb('small2S', [128, 16])
        Q2 = sb('Q2', [128, 512]); Q3 = sb('Q3', [128, 1024]); Q4 = sb('Q4', [128, 1024])
        J0 = sb('J0', [128, 512], BF16); J1 = sb('J1', [128, 640], BF16); J2 = sb('J2', [128, 512], BF16)
        ssd_S = sb('ssd_S', [128, 256]); ssd_Sb = sb('ssd_Sb', [128, 256], BF16)
        rw_S = [sb('rw_S%d' % i, [128, 64]) for i in range(2)]
        rw_Sb = [sb('rw_Sb%d' % i, [128, 64], BF16) for i in range(2)]
        gl_S = sb('gl_S', [128, 256]); gl_Sb = sb('gl_Sb', [128, 256], BF16)
        ml_S = [sb('ml_S%d' % i, [128, 65]) for i in range(2)]
        ml_Sb = [sb('ml_Sb%d' % i, [128, 65], BF16) for i in range(2)]
        dest_all = sb('dest_all', [128, NT, 4], I32)
        gate_all = sb('gate_all', [128, NT, 4])
        cntb = sb('cntb', [128, NE])
        rsc = sb('rsc', [128, 256])
        idx8 = sb('idx8', [128, 8], U32)
        bgu = [sb('bgu%d' % i, [128, 16]) for i in range(2)]
        PS = [psb('ps%d' % i, [128, 512]) for i in range(8)]
        PSB = PS[7][:].bitcast(BF16)

        ident = cst[:, 0, :]
        TRI = cst[:, 1, :]
        TRIS = cst[:, 2, :]
        BTRI = cst[:, 3, :]
        BTRIS = cst[:, 4, :]
        BLOW = cst[:, 5, :]
        GLMASK = cst2[:, 0:256]
        IOTA32 = cst2[:, 256:288]

        def tmf(name):
            o, w = TM_OFF[name]
            return tmc[:, o:o + w]

        P.dma(cst[:], consts)
        P.dma(cst2[:], consts2)
        P.cp('dve', identb[:], cst[:, 0, :])
        P.memset('dve', ones[:], 1.0)

        W3 = R0[:, 0:8 * NCOL].rearrange("p (k n) -> p k n", k=8)
        WO = R1[:, 0:8192].rearrange("p (k n) -> p k n", k=8)
        PG = R1[:, 8192:16384].rearrange("p (k n) -> p k n", k=8)
        PP = R1[:, 16384:18432].rearrange("p (k n) -> p k n", k=2)

        def layernorm(dst, src, gname, bname, sc):
            P.rsum('dve', sc[:, 0:1], src)
            P.ts('dve', sc[:, 1:2], sc[:, 0:1], -1.0 / D, ALU.mult)
            P.act(G[6][:], src, AF.Square, bias=sc[:, 1:2], accum_out=sc[:, 2:3])
            P.act(sc[:, 3:4], sc[:, 2:3], AF.Ln, bias=LN_EPS, scale=1.0 / D)
            P.act(sc[:, 3:4], sc[:, 3:4], AF.Exp, scale=-0.5)
            P.ts('dve', dst, src, sc[:, 1:2], ALU.add, sc[:, 3:4], ALU.mult)
            P.tt('dve', dst, dst, tmf(gname), ALU.mult)
            P.tt('pool', dst, dst, tmf(bname), ALU.add)

        def groupnorm(y, nh, gain, bias, eps, center, sc, tmp):
            hd = 256 // nh
            y3 = h4(y, nh)
            t3 = h4(tmp, nh)
            if center:
                P.treduce('dve', sc[:, 0:nh], y3)
                P.ts('dve', sc[:, 0:nh], sc[:, 0:nh], -1.0 / hd, ALU.mult)
                P.tt('dve', y3, y3, b4(sc[:, 0:nh], hd, nh), ALU.add)
            P.tt('pool', tmp, y, y, ALU.mult)
            P.treduce('dve', sc[:, 4:4 + nh], t3)
            P.act(sc[:, 4:4 + nh], sc[:, 4:4 + nh], AF.Ln, bias=eps, scale=1.0 / hd)
            P.act(sc[:, 4:4 + nh], sc[:, 4:4 + nh], AF.Exp, scale=-0.5)
            P.tt('dve', y3, y3, b4(sc[:, 4:4 + nh], hd, nh), ALU.mult)
            P.tt('dve', y, y, gain, ALU.mult)
            if bias is not None:
                P.tt('dve', y, y, bias, ALU.add)

        def load_r1(layer):
            for k in range(8):
                P.dmac(WO[:, k, :], w_out[layer, k * 128:(k + 1) * 128, :])
            for k in range(8):
                P.dmac(PG[:, k, :], ple_g[layer, k * 128:(k + 1) * 128, :])
            for k in range(2):
                P.dmac(PP[:, k, :], ple_p[layer, k * 128:(k + 1) * 128, :])

        for layer in range(n_layers):
            xsrc = x_in if layer == 0 else xbuf
            xdst = out if layer == n_layers - 1 else xbuf
            for k in range(8):
                P.dmac(W3[:, k, :], w_in[layer, k * 128:(k + 1) * 128, :])
            load_r1(layer)
            P.dma(tmc[:], tmrow[layer].partition_broadcast(128))
            P.dma(cmc[:], cmcol[layer])
            P.dma(lup[:], lora_up[layer])
            P.dma(gup[:], gla_up[layer])
            P.dma(rtw[:], router_w[layer].rearrange("(k p) e -> p k e", p=128))
            P.act(tmf('a_log'), tmf('a_log'), AF.Exp)
            P.ts('dve', tmf('a_log'), tmf('a_log'), -1.0, ALU.mult)
            for s_ in [ssd_S, gl_S] + rw_S + ml_S + [cntb]:
                P.memset('dve', s_[:], 0.0)
            for s_ in [ssd_Sb, gl_Sb] + rw_Sb + ml_Sb:
                P.memset('pool', s_[:], 0.0)
            for cb in cbuf:
                P.memset('pool', cb[:, 0:3], 0.0)
            P.memset('dve', xTe[0][:, :, 0:1], 0.0)

            for c in range(ntiles):
                xt = G[0]
                xe = xTe[c % 2]
                xn = xTe[(c + 1) % 2]
                P.dma(xt[:], xsrc[c * 128:(c + 1) * 128, :])
                for k in range(8):
                    P.tr(PS[k // 4][:, (k % 4) * 128:(k % 4 + 1) * 128], xt[:, k * 128:(k + 1) * 128], ident)
                for hf in range(2):
                    P.cp('act' if hf == 0 else 'dve', xe[:, hf * 4:(hf + 1) * 4, 1:129],
                         PS[hf][:, :].rearrange("p (k n) -> p k n", k=4))
                P.cp('pool', xn[:, :, 0:1], xe[:, :, 128:129])

                def cm_mm(ps_ap, c0, ncols, shifted=False):
                    for k in range(8):
                        rhs = xe[:, k, 0:128] if shifted else xe[:, k, 1:129]
                        P.mm(ps_ap, W3[:, k, c0:c0 + ncols], rhs, start=(k == 0), stop=(k == 7))

                def tm_mm(ps_ap, c0, ncols, shifted=False):
                    for k in range(8):
                        lhsT = xe[:, k, 0:128] if shifted else xe[:, k, 1:129]
                        P.mm(ps_ap, lhsT, W3[:, k, c0:c0 + ncols], start=(k == 0), stop=(k == 7))

                def conv_silu(ci, wcol, bcol, c0, dst):
                    cb = cbuf[ci]
                    ps = (PS[0], PS[1])[ci % 2]
                    cm_mm(ps[:, 0:128], c0, 128)
                    P.cp('act', cb[:, 3:131], ps[:, 0:128])
                    tmp = Q4[:, 384:512]
                    P.ts('dve', tmp, cb[:, 0:128], cmc[:, wcol:wcol + 1], ALU.mult, cmc[:, bcol:bcol + 1], ALU.add)
                    for j in range(1, 4):
                        P.stt('dve', tmp, cb[:, j:j + 128], cmc[:, wcol + j:wcol + j + 1], tmp, ALU.mult, ALU.add)
                    P.act(dst, tmp, AF.Silu)
                    P.cp('pool', cb[:, 0:3], cb[:, 128:131])

                Y = G[1]

                P.begin('s2')
                if 'ssd' not in skip:
                    cmf = Q2
                    for ci in range(4):
                        conv_silu(ci, ci * 4, 16 + ci, SSD0 + 256 + ci * 128, cmf[:, ci * 128:(ci + 1) * 128])
                    P.cp('pool', J0[:, 0:512], cmf[:, 0:512])
                    BT = J0[:, 256:384]
                    CT = J0[:, 384:512]
                    ps = PS[4]
                    for j in range(3):
                        P.tr(ps[:, j * 128:(j + 1) * 128], cmf[:, j * 128:(j + 1) * 128], ident)
                    xs = Q3[:, 0:256]
                    P.cp('act', xs, ps[:, 0:256])
                    Btm = J1[:, 0:128]
                    P.cp('dve', Btm, ps[:, 256:384])
                    ps = PS[5]
                    tm_mm(ps[:, 0:256], SSD0 + 0, 256)
                    tm_mm(ps[:, 256:260], SSD0 + 768, 4)
                    zs = Q3[:, 256:512]
                    P.act(zs, ps[:, 0:256], AF.Silu)
                    sc = smallS
                    dt = sc[:, 0:4]
                    P.tt('dve', dt, ps[:, 256:260], tmf('dt_bias'), ALU.add)
                    P.act(dt, dt, AF.Exp)
                    P.act(dt, dt, AF.Ln, bias=1.0)
                    adt = sc[:, 4:8]
                    P.tt('dve', adt, dt, tmf('a_log'), ALU.mult)
                    ps = PS[0]
                    P.mm(ps[:, 0:4], TRI, adt)
                    P.mm(ps[:, 4:8], ones[:], adt)
                    acum = sc[:, 8:12]
                    P.cp('dve', acum, ps[:, 0:4])
                    ea = sc[:, 12:16]
                    P.act(ea, ps[:, 0:4], AF.Exp)
                    dsx = sc[:, 16:20]
                    P.tt('dve', dsx, ps[:, 4:8], acum, ALU.subtract)
                    P.act(dsx, dsx, AF.Exp)
                    eal = sc[:, 20:24]
                    P.act(eal, ps[:, 4:8], AF.Exp)
                    xdt = J1[:, 128:384]
                    xdt2 = J1[:, 384:640]
                    xdtf = Q3[:, 512:768]
                    P.tt('dve', h4(xdtf), h4(xs), b4(dt, 64), ALU.mult)
                    P.cp('pool', xdt, xdtf)
                    P.tt('dve', h4(xdt2), h4(xdtf), b4(dsx, 64), ALU.mult)
                    ps = PS[0]
                    P.mm(ps[:, 0:128], BT, CT)
                    GTm = Q4[:, 0:128]
                    P.tt('dve', GTm, ps[:, 0:128], TRI, ALU.mult)
                    psy = PS[1]
                    for h in range(4):
                        psl = (PS[4], PS[5])[h % 2]
                        adt_bc = Q4[:, 128:256]
                        P.cp('pool', adt_bc, adt[:, h:h + 1].to_broadcast([128, 128]))
                        P.mm(psl[:, 0:128], adt_bc, TRI)
                        lt = Q4[:, 256:384]
                        P.ts('dve', lt, psl[:, 0:128], acum[:, h:h + 1], ALU.subtract, 0.0, ALU.min)
                        P.act(lt, lt, AF.Exp)
                        MT = J2[:, h * 128:(h + 1) * 128]
                        P.tt('dve', MT, lt, GTm, ALU.mult)
                        P.mm(psy[:, h * 64:(h + 1) * 64], MT, xdt[:, h * 64:(h + 1) * 64])
                    pso = PS[0]
                    P.mm(pso[:, 256:512], CT, ssd_Sb[:])
                    yv = Y[:, 0:256]
                    P.tt('dve', h4(yv), h4(pso[:, 256:512]), b4(ea, 64), ALU.mult)
                    P.tt('dve', yv, yv, psy[:, 0:256], ALU.add)
                    psn = PS[0]
                    P.mm(psn[:, 128:384], Btm, xdt2)
                    P.tt('dve', h4(ssd_S[:]), h4(ssd_S[:]), b4(eal, 64), ALU.mult)
                    P.tt('dve', ssd_S[:], ssd_S[:], psn[:, 128:384], ALU.add)
                    P.cp('pool', ssd_Sb[:], ssd_S[:])
                    t1 = Q4[:, 512:768]
                    P.tt('pool', h4(t1), h4(xs), b4(tmf('ssd_d'), 64), ALU.mult)
                    P.tt('dve', yv, yv, t1, ALU.add)
                    P.tt('dve', yv, yv, zs, ALU.mult)
                    groupnorm(yv, 1, tmf('ssd_ng'), None, 1e-5, False, small2S, Q4[:, 768:1024])

                if 'gla' not in skip:
                    ps = PS[5]
                    tm_mm(ps[:, 0:512], GL0, 512)
                    qkv = Q2
                    P.cp('act', qkv[:, 0:512], ps[:, 0:512])
                    vb = J1[:, 0:256]
                    P.cp('pool', vb, qkv[:, 256:512])
                    ps = PS[0]
                    tm_mm(ps[:, 0:256], GL0 + 528, 256)
                    og = Q3[:, 0:256]
                    P.act(og, ps[:, 0:256], AF.Silu)
                    ps = PS[0]
                    cm_mm(ps[0:16, 0:128], GL0 + 512, 16)
                    gdT = Q3[0:16, 256:384]
                    P.cp('act', gdT, ps[0:16, 0:128])
                    ps = PS[1]
                    P.mm(ps[:, 0:128], gdT, gup[:])
                    la = Q3[:, 384:512]
                    P.tt('dve', la, ps[:, 0:128], tmf('gl_gb'), ALU.add)
                    P.act(la, la, AF.Exp, scale=-1.0)
                    P.act(la, la, AF.Ln, bias=1.0)
                    P.ts('dve', la, la, -1.0 / 16.0, ALU.mult)
                    ps = PS[4]
                    P.mm(ps[:, 0:128], TRI, la)
                    P.mm(ps[:, 128:256], ones[:], la)
                    P.mm(ps[:, 256:257], la, ones[:, 0:1])
                    ebc = Q3[:, 512:640]
                    P.act(ebc, ps[:, 0:128], AF.Exp)
                    enb = Q3[:, 640:768]
                    P.act(enb, ps[:, 0:128], AF.Exp, scale=-1.0)
                    kd = Q3[:, 768:896]
                    bc_sb = Q3[:, 896:1024]
                    P.cp('dve', bc_sb, ps[:, 0:128])
                    P.tt('dve', kd, ps[:, 128:256], bc_sb, ALU.subtract)
                    P.act(kd, kd, AF.Exp)
                    ebl = smallS[:, 32:33]
                    P.act(ebl, ps[:, 256:257], AF.Exp)
                    qd = Q4[:, 0:128]
                    P.stt('dve', qd, qkv[:, 0:128], 32 ** -0.5, ebc, ALU.mult, ALU.mult)
                    ki = Q4[:, 128:256]
                    P.tt('dve', ki, qkv[:, 128:256], enb, ALU.mult)
                    kdb = J1[:, 256:384]
                    P.tt('dve', kdb, qkv[:, 128:256], kd, ALU.mult)
                    ps = PS[0]
                    P.tr(ps[:, 0:128], qd, ident)
                    P.tr(ps[:, 128:256], ki, ident)
                    qdT = J1[:, 384:512]
                    P.cp('act', qdT, ps[:, 0:128])
                    pso = PS[1]
                    P.mm(pso[:, 0:256], qdT, gl_Sb[:], start=True, stop=False)
                    for h in range(4):
                        kim = J1[:, 512:640]
                        P.ts('dve', kim, ps[:, 128:256], cst2[:, 320 + h:321 + h], ALU.mult)
                        psa = (PS[4], PS[5])[h % 2]
                        P.mm(psa[:, 384:512], kim, qdT)
                        at = J2[:, h * 128:(h + 1) * 128]
                        P.tt('dve', at, psa[:, 384:512], TRI, ALU.mult)
                        P.mm(pso[:, h * 64:(h + 1) * 64], at, vb[:, h * 64:(h + 1) * 64], start=False, stop=(h == 3))
                    yv = Y[:, 512:768]
                    P.cp('act', yv, pso[:, 0:256])
                    psn = PS[0]
                    P.mm(psn[:, 256:512], kdb, vb)
                    P.ts('dve', gl_S[:], gl_S[:], ebl, ALU.mult)
                    t1 = Q4[:, 256:512]
                    P.tt('dve', t1, psn[:, 256:512], GLMASK, ALU.mult)
                    P.tt('dve', gl_S[:], gl_S[:], t1, ALU.add)
                    P.cp('pool', gl_Sb[:], gl_S[:])
                    groupnorm(yv, 4, tmf('gl_ng'), None, 1e-5, False, small2S, Q4[:, 768:1024])
                    P.tt('dve', yv, yv, og, ALU.mult)

                P.begin('s1')
                if 'rwkv' not in skip:
                    rkv = G[2]
                    for half in range(2):
                        c0 = RW0 + half * 384
                        tm_mm(PS[2][:, 0:384], c0, 384)
                        tm_mm(PS[3][:, 0:384], c0, 384, shifted=True)
                        cur = G[4][:, 0:384]
                        P.cp('act', cur, PS[2][:, 0:384])
                        dl = G[4][:, 384:768]
                        P.tt('dve', dl, PS[3][:, 0:384], cur, ALU.subtract)
                        P.tt('pool', dl, dl, tmf('rw_mu')[:, half * 384:(half + 1) * 384], ALU.mult)
                        P.tt('dve', rkv[:, half * 384:(half + 1) * 384], cur, dl, ALU.add)
                    r_ = rkv[:, 0:256]
                    k_ = rkv[:, 256:512]
                    v_ = rkv[:, 512:768]
                    cm_mm(PS[6][:, 0:128], RW0 + 768, 128)
                    cm_mm(PS[6][:, 128:256], RW0 + 768, 128, shifted=True)
                    lo = G[3][:, 0:128]
                    P.cp('act', lo, PS[6][:, 0:128])
                    dl = G[3][:, 128:256]
                    P.tt('dve', dl, PS[6][:, 128:256], lo, ALU.subtract)
                    P.stt('dve', lo, dl, cmc[:, 40:41], lo, ALU.mult, ALU.add)
                    P.act(lo[0:32, :], lo[0:32, :], AF.Tanh)
                    P.act(lo[64:128, :], lo[64:128, :], AF.Sigmoid)
                    ps = PS[7]
                    P.mm(ps[:, 0:256], lo[0:32, :], lup[0:32, :])
                    ps2 = PS[6]
                    P.mm(ps2[:, 0:256], lo[32:64, :], lup[32:64, :])
                    P.mm(ps2[:, 256:512], lo[64:128, :], lup[64:128, :])
                    gg = G[3][:, 256:512]
                    P.cp('act', gg, ps2[:, 256:512])
                    lw = G[3][:, 512:768]
                    P.tt('dve', lw, ps[:, 0:256], tmf('rw_w0'), ALU.add)
                    P.act(lw, lw, AF.Exp, scale=-1.0)
                    P.act(lw, lw, AF.Ln, bias=1.0)
                    P.ts('dve', lw, lw, -1.0, ALU.mult, -0.5, ALU.add)
                    P.act(lw, lw, AF.Exp)
                    P.ts('dve', lw, lw, -1.0, ALU.mult)
                    av = G[3][:, 768:1024]
                    P.tt('dve', av, ps2[:, 0:256], tmf('rw_a0'), ALU.add)
                    P.act(av, av, AF.Sigmoid)
                    kk = G[4][:, 0:256]
                    P.tt('dve', kk, k_, tmf('rw_kk'), ALU.mult)
                    sq = G[4][:, 256:512]
                    P.tt('pool', sq, kk, kk, ALU.mult)
                    nrm = small[:, 80:84]
                    P.treduce('dve', nrm, h4(sq))
                    P.ts('dve', nrm, nrm, 1e-24, ALU.max)
                    P.act(nrm, nrm, AF.Ln)
                    P.act(nrm, nrm, AF.Exp, scale=-0.5)
                    P.tt('dve', h4(kk), h4(kk), b4(nrm, 64), ALU.mult)
                    km = G[4][:, 256:512]
                    P.ts('dve', km, av, -1.0, ALU.add)
                    P.tt('dve', km, km, tmf('rw_ka'), ALU.mult)
                    P.ts('dve', km, km, 1.0, ALU.add)
                    P.tt('dve', km, km, k_, ALU.mult)
                    bt_ = G[4][:, 512:768]
                    P.tt('pool', bt_, r_, km, ALU.mult)
                    P.tt('pool', bt_, bt_, tmf('rw_rk'), ALU.mult)
                    bon = small[:, 84:88]
                    P.treduce('dve', bon, h4(bt_))
                    ps = PS[7]
                    P.mm(ps[:, 0:256], BTRI, lw)
                    Wt = G[4][:, 512:768]
                    P.act(Wt, ps[:, 0:256], AF.Exp)
                    Wi = G[4][:, 768:1024]
                    P.act(Wi, ps[:, 0:256], AF.Exp, scale=-1.0)
                    Wp = G[5][:, 0:256]
                    cwsb = G[5][:, 256:512]
                    P.cp('dve', cwsb, ps[:, 0:256])
                    P.tt('dve', Wp, cwsb, lw, ALU.subtract)
                    P.act(Wp, Wp, AF.Exp)
                    ps = PS[2]
                    for hp in range(2):
                        for j in range(2):
                            P.mm(ps[:, hp * 2 + j:hp * 2 + j + 1], lw[j * 64:(j + 1) * 64, hp * 128:(hp + 1) * 128],
                                 ones[j * 64:(j + 1) * 64, 0:1])
                    WL = small[:, 88:92]
                    P.act(WL, ps[:, 0:4], AF.Exp)
                    rt = G[5][:, 256:512]
                    P.tt('dve', rt, r_, Wt, ALU.mult)
                    at_ = G[5][:, 512:768]
                    P.stt('dve', at_, kk, -1.0, Wp, ALU.mult, ALU.mult)
                    btl = G[5][:, 768:1024]
                    P.tt('dve', btl, kk, av, ALU.mult)
                    P.tt('dve', btl, btl, Wi, ALU.mult)
                    kt = G[4][:, 0:256]
                    P.tt('dve', kt, km, Wi, ALU.mult)
                    btb = H[1][:, 0:256]
                    ktb = H[1][:, 256:512]
                    vbb = H[1][:, 512:768]
                    P.cp('pool', btb, btl)
                    P.cp('pool', ktb, kt)
                    P.cp('pool', vbb, v_)
                    cmT = {}
                    for ai, (nm, arr) in enumerate([('r', rt), ('a', at_), ('b', btl), ('k', kt)]):
                        ps = PS[2 + ai % 2]
                        P.tr(ps[:, 0:128], arr[:, 0:128], ident)
                        P.tr(ps[:, 128:256], arr[:, 128:256], ident)
                        dstT = G[6][:, ai * 256:(ai + 1) * 256]
                        P.cp('act' if ai % 2 == 0 else 'dve', dstT, ps[:, 0:256])
                        cmT[nm] = dstT
                    o_rw = Y[:, 256:512]
                    def rw_head(h, Fs, Bs, bk1, bk2):
                        hp, hq = h // 2, (h % 2) * 64
                        sl = slice(hq, hq + 64)
                        rT = cmT['r'][sl, hp * 128:(hp + 1) * 128]
                        aT = cmT['a'][sl, hp * 128:(hp + 1) * 128]
                        bT = cmT['b'][sl, hp * 128:(hp + 1) * 128]
                        kT = cmT['k'][sl, hp * 128:(hp + 1) * 128]
                        psA = bk1
                        P.mm(psA[:, 0:128], bT, aT)
                        P.mm(psA[:, 128:256], aT, bT)
                        P.mm(psA[:, 256:384], kT, aT)
                        psB = bk2
                        P.mm(psB[:, 0:128], bT, rT)
                        P.mm(psB[:, 128:256], kT, rT)
                        Pm = Fs[:, 0:128]
                        PTm = Fs[:, 128:256]
                        TT = Fs[:, 256:384]
                        P.tt('dve', Pm, psA[:, 0:128], BTRIS, ALU.mult)
                        P.tt('dve', PTm, psA[:, 128:256], BLOW, ALU.mult)
                        AakT = Bs[:, 0:128]
                        P.tt('dve', AakT, psA[:, 256:384], BTRIS, ALU.mult)
                        ArbT = Bs[:, 128:256]
                        P.tt('dve', ArbT, psB[:, 0:128], BTRI, ALU.mult)
                        ArkT = Bs[:, 256:384]
                        P.tt('dve', ArkT, psB[:, 128:256], BTRI, ALU.mult)
                        P.tt('dve', TT, Pm, ident, ALU.add)
                        for step in range(5):
                            psq = bk1
                            P.mm(psq[:, 0:128], PTm, Pm)
                            P.mm(psq[:, 128:256], Pm, PTm)
                            P.cp('dve', Pm, psq[:, 0:128])
                            P.cp('act', PTm, psq[:, 128:256])
                            psq2 = bk2
                            P.mm(psq2[:, 256:384], PTm, TT)
                            P.tt('dve', TT, TT, psq2[:, 256:384], ALU.add)
                        TTb = Bs[:, 384:512]
                        P.cp('pool', TTb, TT)
                        aTb = Bs[:, 512:640]
                        rTb = Bs[:, 640:768]
                        P.cp('pool', aTb[sl, :], aT)
                        P.cp('pool', rTb[sl, :], rT)
                        for j in range(2):
                            js = slice(j * 64, (j + 1) * 64)
                            psx = bk2
                            vh = vbb[js, h * 64:(h + 1) * 64]
                            P.mm(psx[js, 0:64], aTb[sl, js], rw_Sb[hp][sl, :], start=True, stop=False)
                            P.mm(psx[js, 0:64], AakT[js, js], vh, start=False, stop=True)
                            X1 = Bs[:, 768:832]
                            P.cp('act', X1[js, :], psx[js, 0:64])
                            P.mm(psx[js, 64:128], TTb[js, js], X1[js, :])
                            Ub = Bs[:, 832:896]
                            P.cp('act', Ub[js, :], psx[js, 64:128])
                            P.mm(psx[js, 128:192], rTb[sl, js], rw_Sb[hp][sl, :], start=True, stop=False)
                            P.mm(psx[js, 128:192], ArbT[js, js], Ub[js, :], start=False, stop=False)
                            P.mm(psx[js, 128:192], ArkT[js, js], vh, start=False, stop=True)
                            P.cp('dve', o_rw[js, h * 64:(h + 1) * 64], psx[js, 128:192])
                            P.mm(psx[sl, 192:256], btb[js, h * 64:(h + 1) * 64], Ub[js, :], start=True, stop=False)
                            P.mm(psx[sl, 192:256], ktb[js, h * 64:(h + 1) * 64], vh, start=False, stop=True)
                            P.tt('dve', rw_S[hp][sl, :], rw_S[hp][sl, :], psx[sl, 192:256], ALU.add)
                            P.ts('dve', rw_S[hp][sl, :], rw_S[hp][sl, :], WL[sl, hp * 2 + j:hp * 2 + j + 1], ALU.mult)
                            P.cp('pool', rw_Sb[hp][sl, :], rw_S[hp][sl, :])
                P.merge()
                P.begin('s2')
                if 'mlstm' not in skip:
                    cmf = Q2
                    for ci in range(4):
                        conv_silu(4 + ci, 20 + ci * 4, 36 + ci, ML0 + ci * 128, cmf[:, ci * 128:(ci + 1) * 128])
                    P.cp('pool', J0[:, 0:512], cmf[:, 0:512])
                    ps = PS[4]
                    P.tr(ps[:, 0:128], cmf[:, 256:384], ident)
                    P.tr(ps[:, 128:256], cmf[:, 384:512], ident)
                    ktm = Q3[:, 0:256]
                    P.cp('act', ktm, ps[:, 0:256])
                    ps = PS[5]
                    tm_mm(ps[:, 0:264], ML0 + 512, 264)
                    vaug = J1[:, 0:260].rearrange("p (h d) -> p h d", h=4)
                    P.cp('act', vaug[:, :, 0:64], h4(ps[:, 0:256]))
                    P.memset('pool', vaug[:, :, 64:65], 1.0)
                    ig = smallS[:, 48:52]
                    P.tt('dve', ig, ps[:, 256:260], tmf('ml_ib'), ALU.add)
                    lf = smallS[:, 52:56]
                    P.tt('dve', lf, ps[:, 260:264], tmf('ml_fb'), ALU.add)
                    P.act(lf, lf, AF.Exp, scale=-1.0)
                    P.act(lf, lf, AF.Ln, bias=1.0)
                    P.ts('dve', lf, lf, -1.0, ALU.mult)
                    ps = PS[0]
                    tm_mm(ps[:, 0:256], ML0 + 776, 256)
                    ogs = Q3[:, 256:512]
                    P.act(ogs, ps[:, 0:256], AF.Sigmoid)
                    ps = PS[0]
                    P.mm(ps[:, 0:4], TRI, lf)
                    P.mm(ps[:, 4:8], ones[:], lf)
                    bb = smallS[:, 56:60]
                    P.cp('dve', bb, ps[:, 0:4])
                    eb = smallS[:, 64:68]
                    P.act(eb, ps[:, 0:4], AF.Exp)
                    wst = smallS[:, 68:72]
                    P.tt('dve', wst, ps[:, 4:8], bb, ALU.subtract)
                    P.tt('dve', wst, wst, ig, ALU.add)
                    P.act(wst, wst, AF.Exp)
                    ebl4 = smallS[:, 72:76]
                    P.act(ebl4, ps[:, 4:8], AF.Exp)
                    pso = PS[1]
                    for h in range(4):
                        hp, hq = h // 2, (h % 2) * 64
                        qT_h = J0[hq:hq + 64, hp * 128:(hp + 1) * 128]
                        kT_h = J0[hq:hq + 64, 256 + hp * 128:256 + (hp + 1) * 128]
                        psl = (PS[4], PS[5])[h % 2]
                        lfb = Q4[:, 128:256]
                        P.cp('pool', lfb, lf[:, h:h + 1].to_broadcast([128, 128]))
                        P.mm(psl[:, 0:128], lfb, TRI)
                        dm = Q4[:, 256:384]
                        P.ts('dve', dm, psl[:, 0:128], bb[:, h:h + 1], ALU.subtract, 0.0, ALU.min)
                        P.act(dm, dm, AF.Exp, bias=ig[:, h:h + 1])
                        P.tt('pool', dm, dm, TRI, ALU.mult)
                        P.mm(psl[:, 128:256], kT_h, qT_h)
                        sT = J2[:, h * 128:(h + 1) * 128]
                        P.stt('dve', sT, psl[:, 128:256], 0.125, dm, ALU.mult, ALU.mult)
                        P.mm(pso[:, h * 65:(h + 1) * 65], sT, vaug[:, h, :])
                        P.mm(PS[0][:, h * 65:(h + 1) * 65], qT_h, ml_Sb[hp][hq:hq + 64, :])
                    numf = Q4[:, 512:772]
                    num = h4(numf)
                    P.tt('dve', num, h4(PS[0][:, 0:260]), b4(eb, 65), ALU.mult)
                    P.stt('dve', numf, numf, 0.125, pso[:, 0:260], ALU.mult, ALU.add)
                    den = smallS[:, 76:80]
                    den3 = den.rearrange("p (h o) -> p h o", o=1)
                    P.stt('dve', den3, num[:, :, 64:65], -1.0, num[:, :, 64:65], ALU.mult, ALU.max)
                    P.ts('dve', den, den, 1.0, ALU.max)
                    P.recip(den, den)
                    yv = Y[:, 768:1024]
                    P.tt('dve', h4(yv), num[:, :, 0:64], b4(den, 64), ALU.mult)
                    P.tt('dve', yv, yv, ogs, ALU.mult)
                    kw = J1[:, 260:516]
                    P.tt('dve', h4(kw), h4(ktm), b4(wst, 64), ALU.mult)
                    psn = PS[0]
                    for h in range(4):
                        hp, hq = h // 2, (h % 2) * 64
                        P.mm(psn[hq:hq + 64, 256 + hp * 65:256 + (hp + 1) * 65], kw[:, h * 64:(h + 1) * 64], vaug[:, h, :])
                    for hp in range(2):
                        for hh in range(2):
                            h = hp * 2 + hh
                            hq = hh * 64
                            P.ts('dve', ml_S[hp][hq:hq + 64, :], ml_S[hp][hq:hq + 64, :], ebl4[hq:hq + 64, h:h + 1], ALU.mult)
                        P.tt('dve', ml_S[hp][:], ml_S[hp][:], psn[:, 256 + hp * 65:256 + (hp + 1) * 65], ALU.add)
                        P.cp('pool', ml_Sb[hp][:], ml_S[hp][:])
                    groupnorm(yv, 4, tmf('ml_ng'), None, 1e-5, True, small2S, Q4[:, 768:1024])

                if 'rwkv' not in skip:
                    P.begin('ha')
                    rw_head(0, G[5], H[3], PS[2], PS[6])
                    P.begin('hb')
                    rw_head(2, G[4], H[4], PS[3], PS[7])
                P.merge()
                if 'rwkv' not in skip:
                    P.begin('ha')
                    rw_head(1, G[5], H[3], PS[2], PS[6])
                    P.begin('hb')
                    rw_head(3, G[4], H[4], PS[3], PS[7])
                P.merge()
                if 'rwkv' not in skip:
                    groupnorm(o_rw, 4, tmf('rw_lng'), tmf('rw_lnb'), 64e-5, True, small2, G[4][:, 768:1024])
                    t1 = G[4][:, 512:768]
                    P.tt('dve', h4(t1), h4(v_), b4(bon, 64), ALU.mult)
                    P.tt('dve', o_rw, o_rw, t1, ALU.add)
                    P.tt('dve', o_rw, o_rw, gg, ALU.mult)
                if debug and layer == 0:
                    for nm, c0 in [('d_ssd', 0), ('d_rwkv', 256), ('d_gla', 512), ('d_mlstm', 768)]:
                        P.dma(dbg[nm][c * 128:(c + 1) * 128, :], Y[:, c0:c0 + 256], comm=True)
                if stop_after == 'mixers':
                    continue

                YT = H[2].rearrange("p (k n) -> p k n", k=8)
                for k in range(8):
                    P.tr(PS[2 + k // 4][:, (k % 4) * 128:(k % 4 + 1) * 128], Y[:, k * 128:(k + 1) * 128], ident)
                for hf in range(2):
                    P.cp('act' if hf == 0 else 'dve', YT[:, hf * 4:(hf + 1) * 4, :],
                         PS[2 + hf][:, :].rearrange("p (k n) -> p k n", k=4))
                x1 = G[2]
                for hf in range(2):
                    ps = PS[4 + hf]
                    for k in range(8):
                        P.mm(ps[:, :], YT[:, k, :], WO[:, k, hf * 512:(hf + 1) * 512], start=(k == 0), stop=(k == 7))
                    P.stt('dve', x1[:, hf * 512:(hf + 1) * 512], xt[:, hf * 512:(hf + 1) * 512], ALPHA, ps[:, :], ALU.mult, ALU.add)
                layernorm(x1[:], x1[:], 'ln1_g', 'ln1_b', small2)
                P.dma(x1buf[c * 128:(c + 1) * 128, :], x1[:], comm=True)
                if debug and layer == 0:
                    P.dma(dbg['d_x1'][c * 128:(c + 1) * 128, :], x1[:], comm=True)
                x1b = H[0]
                P.cp('pool', x1b[:], x1[:])
                x1T = G[3].rearrange("p (k n) -> p k n", k=8)
                for k in range(8):
                    P.tr(PS[2 + k // 4][:, (k % 4) * 128:(k % 4 + 1) * 128], x1[:, k * 128:(k + 1) * 128], ident)
                for hf in range(2):
                    P.cp('act' if hf == 0 else 'dve', x1T[:, hf * 4:(hf + 1) * 4, :],
                         PS[2 + hf][:, :].rearrange("p (k n) -> p k n", k=4))
                ps = PS[6]
                for k in range(8):
                    P.mm(ps[:, 0:32], x1T[:, k, :], rtw[:, k, :], start=(k == 0), stop=(k == 7))
                lg = rsc[:, 0:32]
                P.tt('dve', lg, ps[:, 0:32], tmf('rt_b'), ALU.add)
                mx8 = rsc[:, 32:40]
                P.op('dve', lambda e, mx8=mx8, lg=lg: e.max(out=mx8, in_=lg), reads=K(lg), writes=K(mx8))
                P.op('dve', lambda e, mx8=mx8, lg=lg: e.max_index(out=idx8[:], in_max=mx8, in_values=lg),
                     reads=K(lg, mx8), writes=K(idx8))
                msk = rsc[:, 40:72]
                P.ts('dve', msk, lg, mx8[:, 3:4], ALU.is_ge)
                nmx = rsc[:, 72:73]
                P.ts('dve', nmx, mx8[:, 0:1], -1.0, ALU.mult)
                ex = rsc[:, 76:108]
                P.act(ex, lg, AF.Exp, bias=nmx)
                P.tt('dve', ex, ex, msk, ALU.mult)
                ssum = rsc[:, 73:74]
                P.rsum('dve', ssum, ex)
                P.recip(ssum, ssum)
                P.ts('dve', ex, ex, ssum, ALU.mult)
                ps = PS[5]
                P.mm(ps[:, 0:32], TRIS, msk)
                P.mm(ps[:, 32:64], ones[:], msk)
                pos = rsc[:, 108:140]
                P.tt('dve', pos, ps[:, 0:32], cntb[:], ALU.add)
                P.tt('dve', cntb[:], cntb[:], ps[:, 32:64], ALU.add)
                idxf = rsc[:, 140:144]
                P.cp('dve', idxf, idx8[:, 0:4])
                destf = rsc[:, 144:148]
                for k4 in range(4):
                    oh = rsc[:, 152:184]
                    P.ts('dve', oh, IOTA32, idxf[:, k4:k4 + 1], ALU.is_equal)
                    tmp = rsc[:, 184:216]
                    P.tt('dve', tmp, oh, pos, ALU.mult)
                    P.rsum('dve', destf[:, k4:k4 + 1], tmp)
                    P.tt('dve', tmp, oh, ex, ALU.mult)
                    P.rsum('dve', gate_all[:, c, k4:k4 + 1], tmp)
                ovf = rsc[:, 148:152]
                P.ts('dve', ovf, destf, float(CAP), ALU.is_ge, float(NE * CAP), ALU.mult)
                P.stt('dve', destf, idxf, float(CAP), destf, ALU.mult, ALU.add)
                P.tt('dve', destf, destf, ovf, ALU.add)
                P.cp('dve', dest_all[:, c, :], destf)
                for k4 in range(4):
                    P.scatter(xg, dest_all[:, c, k4:k4 + 1], x1b[:], NE * CAP - 1)
                if debug and layer == 0:
                    P.dma(dbg['d_logits'][c * 128:(c + 1) * 128, :], lg, comm=True)
                    P.dma(dbg['d_dest'][c * 128:(c + 1) * 128, :], destf, comm=True)
                    P.dma(dbg['d_gate'][c * 128:(c + 1) * 128, :], gate_all[:, c, :], comm=True)
            if stop_after in ('mixers', 'phaseA'):
                continue

            EB = [R0, R1]

            def eb_views(i):
                gu = EB[i][:, 0:16384].rearrange("p (k n) -> p k n", k=8)
                dn = EB[i][:, 16384:24576].rearrange("p (k n) -> p k n", k=8)
                return gu, dn

            def load_expert(e):
                gu, dn = eb_views(e % 2)
                for k in range(8):
                    P.dmac(gu[:, k, :], w_gu[layer, e, k * 128:(k + 1) * 128, :])
                for k in range(8):
                    P.dmac(dn[:, k, :], w_dn[layer, e, k * 128:(k + 1) * 128, :])
                P.dma(bgu[e % 2][:], b_gu[layer, e])

            bdn = G[6]
            groups = [(e, grp) for e in range(NE) for grp in range(CAP // 256)]

            def views(gi):
                xgT = G[(gi % 2) * 2][:].bitcast(BF16).rearrange("p (k n) -> p k n", k=8)
                actT = G[(gi % 2) * 2 + 1][:].bitcast(BF16).rearrange("p (k n) -> p k n", k=8)
                return xgT, actT

            def prep(gi):
                e, grp = groups[gi]
                xgT, _ = views(gi)
                base = e * CAP + grp * 256
                for stl in range(2):
                    xr = H[stl]
                    P.dma(xr[:], xg[base + stl * 128:base + (stl + 1) * 128, :])
                    for k in range(8):
                        P.tr(PSB[:, k * 128:(k + 1) * 128], xr[:, k * 128:(k + 1) * 128], identb[:])
                    P.cp('act', xgT[:, :, stl * 128:(stl + 1) * 128],
                         PSB[:, :].rearrange("p (k n) -> p k n", k=8))

            def hphase(gi):
                e, grp = groups[gi]
                gu, dn = eb_views(e % 2)
                xgT, actT = views(gi)
                for fc in range(8):
                    psg = PS[(fc % 2) * 2]
                    psl = PS[(fc % 2) * 2 + 1]
                    for k in range(8):
                        P.mm(psg[:, 0:256], gu[:, k, fc * 128:(fc + 1) * 128], xgT[:, k, :], start=(k == 0), stop=(k == 7))
                    for k in range(8):
                        P.mm(psl[:, 0:256], gu[:, k, 1024 + fc * 128:1024 + (fc + 1) * 128], xgT[:, k, :], start=(k == 0), stop=(k == 7))
                    tgl = (G[4], Q4)[fc % 2]
                    tg = tgl[:, 0:256]
                    tl = tgl[:, 256:512]
                    bg = bgu[e % 2]
                    P.ts('dve', tg, psg[:, 0:256], bg[:, fc:fc + 1], ALU.add, 7.0, ALU.min)
                    P.ts('dve', tl, psl[:, 0:256], bg[:, 8 + fc:9 + fc], ALU.add, 7.0, ALU.min)
                    sg = Q2[:, (fc % 2) * 256:(fc % 2) * 256 + 256]
                    P.act(sg, tg, AF.Sigmoid, scale=1.702)
                    P.ts('dve', tl, tl, -7.0, ALU.max, 1.0, ALU.add)
                    P.tt('dve', tg, tg, sg, ALU.mult)
                    P.tt('dve', actT[:, fc, :], tl, tg, ALU.mult)

            def down(gi):
                e, grp = groups[gi]
                gu, dn = eb_views(e % 2)
                _, actT = views(gi)
                base = e * CAP + grp * 256
                if grp == 0:
                    P.dma(bdn[:], b_dn[layer, e].partition_broadcast(128))
                for stl in range(2):
                    yb = (G[5], Q3)[stl]
                    for hf in range(2):
                        ps = PS[4 + hf]
                        for k in range(8):
                            P.mm(ps[:, :], actT[:, k, stl * 128:(stl + 1) * 128], dn[:, k, hf * 512:(hf + 1) * 512],
                                 start=(k == 0), stop=(k == 7))
                        P.tt('dve', yb[:, hf * 512:(hf + 1) * 512], ps[:, :], bdn[:, hf * 512:(hf + 1) * 512], ALU.add)
                    P.dma(yg[base + stl * 128:base + (stl + 1) * 128, :], yb[:], comm=True)

            load_expert(0)
            prep(0)
            for gi in range(len(groups)):
                e, grp = groups[gi]
                if grp == 0 and e + 1 < NE:
                    load_expert(e + 1)
                hphase(gi)
                if gi + 1 < len(groups):
                    prep(gi + 1)
                down(gi)
            if stop_after == 'phaseB':
                continue

            load_r1(layer)
            for c in range(ntiles):
                x1 = G[2]
                P.dma(x1[:], x1buf[c * 128:(c + 1) * 128, :])
                hh = G[0]
                P.ts('dve', hh[:], x1[:], ALPHA, ALU.mult)
                for k4 in range(4):
                    gt = G[3 + (k4 % 2)]
                    P.memset('dve', gt[:], 0.0)
                    P.gather(gt[:], yg, dest_all[:, c, k4:k4 + 1], NE * CAP - 1)
                    P.stt('dve', hh[:], gt[:], gate_all[:, c, k4:k4 + 1], hh[:], ALU.mult, ALU.add)
                if debug and layer == 0:
                    ff = G[5]
                    P.stt('dve', ff[:], x1[:], -ALPHA, hh[:], ALU.mult, ALU.add)
                    P.dma(dbg['d_ffn'][c * 128:(c + 1) * 128, :], ff[:], comm=True)
                hT = H[2].rearrange("p (k n) -> p k n", k=8)
                for k in range(8):
                    P.tr(PS[k // 4][:, (k % 4) * 128:(k % 4 + 1) * 128], hh[:, k * 128:(k + 1) * 128], ident)
                for hf in range(2):
                    P.cp('act' if hf == 0 else 'dve', hT[:, hf * 4:(hf + 1) * 4, :],
                         PS[hf][:, :].rearrange("p (k n) -> p k n", k=4))
                pt = G[5][:, 0:256]
                P.dma(pt, p_in[layer, c * 128:(c + 1) * 128, :])
                P.tr(PS[2][:, 0:128], pt[:, 0:128], ident)
                P.tr(PS[2][:, 128:256], pt[:, 128:256], ident)
                pT = H[3][:, 0:256].rearrange("p (k n) -> p k n", k=2)
                P.cp('act', pT, PS[2][:, 0:256].rearrange("p (k n) -> p k n", k=2))
                sig = G[1]
                for hf in range(2):
                    ps = PS[3 + hf]
                    for k in range(8):
                        P.mm(ps[:, :], hT[:, k, :], PG[:, k, hf * 512:(hf + 1) * 512], start=(k == 0), stop=(k == 7))
                    P.act(sig[:, hf * 512:(hf + 1) * 512], ps[:, :], AF.Sigmoid)
                    ps2 = PS[5 + hf]
                    for k in range(2):
                        P.mm(ps2[:, :], pT[:, k, :], PP[:, k, hf * 512:(hf + 1) * 512], start=(k == 0), stop=(k == 1))
                    P.tt('dve', sig[:, hf * 512:(hf + 1) * 512], sig[:, hf * 512:(hf + 1) * 512], ps2[:, :], ALU.mult)
                P.tt('dve', hh[:], hh[:], sig[:], ALU.add)
                layernorm(hh[:], hh[:], 'ln2_g', 'ln2_b', small2)
                P.dma(xdst[c * 128:(c + 1) * 128, :], hh[:], comm=True)
        print('total ops recorded', P.nops)
        P.emit()
    return nc


def _consts():
    s = np.arange(128)[:, None]
    t = np.arange(128)[None, :]
    blk = (s // 64) == (t // 64)
    c = np.zeros((128, 6, 128), np.float32)
    c[:, 0] = np.eye(128)
    c[:, 1] = (s <= t)
    c[:, 2] = (s < t)
    c[:, 3] = blk & (s <= t)
    c[:, 4] = blk & (s < t)
    c[:, 5] = blk & (t < s)
    c2 = np.zeros((128, 324), np.float32)
    c2[:, 0:256] = (np.arange(128)[:, None] // 32) == (np.arange(256)[None, :] // 64)
    c2[:, 256:288] = np.arange(32)[None, :]
    c2[:, 320:324] = (np.arange(128)[:, None] // 32) == np.arange(4)[None, :]
    return c, c2


def prep_inputs(inp):
    f = lambda k: np.asarray(inp[k], dtype=np.float32)
    L = DEPTH
    tm = np.zeros((L, 1, TM_W), np.float32)
    src = {
        'dt_bias': f('ssd_dt_bias'), 'a_log': f('ssd_a_log'), 'ssd_d': f('ssd_d'), 'ssd_ng': f('ssd_norm_g'),
        'rw_mu': f('rwkv_mu')[:, 0:768], 'rw_w0': f('rwkv_w0'), 'rw_a0': f('rwkv_a0'), 'rw_kk': f('rwkv_k_k'),
        'rw_ka': f('rwkv_k_a'), 'rw_rk': f('rwkv_r_k').reshape(L, 256), 'rw_lng': f('rwkv_ln_g'), 'rw_lnb': f('rwkv_ln_b'),
        'gl_gb': f('gla_gate_b'), 'gl_ng': f('gla_norm_g'),
        'ml_ib': f('mlstm_i_b'), 'ml_fb': f('mlstm_f_b'), 'ml_ng': f('mlstm_norm_g'),
        'ln1_g': f('ln1_g'), 'ln1_b': f('ln1_b'), 'ln2_g': f('ln2_g'), 'ln2_b': f('ln2_b'), 'rt_b': f('router_b'),
    }
    for nm, (o, w) in TM_OFF.items():
        tm[:, 0, o:o + w] = src[nm]
    cm = np.zeros((L, 128, CM_W), np.float32)
    scw = f('ssd_conv_w'); scb = f('ssd_conv_b'); mcw = f('mlstm_conv_w'); mcb = f('mlstm_conv_b')
    for ci in range(4):
        for j in range(4):
            cm[:, :, ci * 4 + j] = scw[:, j, ci * 128:(ci + 1) * 128]
            cm[:, :, 20 + ci * 4 + j] = mcw[:, j, ci * 128:(ci + 1) * 128]
        cm[:, :, 16 + ci] = scb[:, ci * 128:(ci + 1) * 128]
        cm[:, :, 36 + ci] = mcb[:, ci * 128:(ci + 1) * 128]
    cm[:, :, 40] = f('rwkv_mu')[:, 768:896]
    lora = np.concatenate([f('rwkv_w_up'), f('rwkv_a_up'), f('rwkv_g_up')], axis=1)
    bgu = np.ascontiguousarray(f('exp_b_gu').reshape(L, NE, 16, 128).transpose(0, 1, 3, 2))
    bdn = f('exp_b_down').reshape(L, NE, 1, D)
    c, c2 = _consts()
    shared = {
        'w_in': f('w_in'), 'w_out': f('w_out'), 'tmrow': tm, 'cmcol': cm, 'lora_up': np.ascontiguousarray(lora),
        'gla_up': f('gla_gate_up'), 'router_w': f('router_w'), 'w_gu': f('exp_w_gu'), 'b_gu': bgu,
        'w_dn': f('exp_w_down'), 'b_dn': bdn, 'ple_g': f('ple_gate_w'), 'ple_p': f('ple_proj'),
        'consts': c, 'consts2': c2,
    }
    return shared


_NC_CACHE = {}


def kernel(**inputs):
    shared = prep_inputs(inputs)
    x = np.asarray(inputs['x'], dtype=np.float32)
    p = np.asarray(inputs['p'], dtype=np.float32)
    if 'nc' not in _NC_CACHE:
        _NC_CACHE['nc'] = build()
    nc = _NC_CACHE['nc']
    in_maps = []
    for core in range(8):
        b = core % 4
        m = dict(shared)
        m['x'] = np.ascontiguousarray(x[b])
        m['p'] = np.ascontiguousarray(p[:, b])
        in_maps.append(m)
    res = run_bass_kernel_spmd(nc, in_maps, core_ids=list(range(8)))
    outs = [res.results[b]['out'] for b in range(4)]
    return np.stack(outs, axis=0).astype(np.float32)
```

```python
import math
from contextlib import ExitStack
import numpy as np
import concourse.bass as bass
import concourse.mybir as mybir
from concourse.bass_utils import run_bass_kernel_spmd

F32 = mybir.dt.float32
BF16 = mybir.dt.bfloat16
I32 = mybir.dt.int32
U32 = mybir.dt.uint32
AF = mybir.ActivationFunctionType
ALU = mybir.AluOpType
AX = mybir.AxisListType

D = 1024
T = 4096
NT = T // 128
DEPTH = 4
NCOL = 3484
NE = 32
CAP = 768
ALPHA = (2 * DEPTH) ** 0.25
LN_EPS = 1e-5
SSD0, RW0, GL0, ML0 = 0, 772, 1668, 2452


def _isap(a):
    return hasattr(a, 'tensor')


def K(*aps):
    return list({a.tensor.name for a in aps if _isap(a)})


class Prog:
    ENGS = ['pe', 'act', 'dve', 'pool', 'sp']
    NROT = 4
    CH = 1500
    CHC = 4000

    def __init__(self, nc):
        self.nc = nc
        self.ops = {e: [] for e in self.ENGS}
        self.cnt = {}
        self.last_w = {}
        self.readers = {}
        self.waited = {e: {} for e in self.ENGS}
        self.ndma = {e: 0 for e in self.ENGS}
        self.sems = {}
        self.cwr = {}
        self.pe_bank = {}
        self._bound_reg = None
        self._cur = None
        self._streams = {}
        self.nops = 0
        self.maxops = None

    def op(self, eng, fn, reads=(), writes=(), dma=False, cwrites=(), pebank=None):
        if self._cur is not None:
            self._streams[self._cur].append((eng, fn, reads, writes, dma, cwrites, pebank))
            return
        self.nops += 1
        if self.maxops is not None and self.nops > self.maxops:
            return
        selfwait = []
        if pebank is not None:
            bank, r0, r1 = pebank
            last = self.pe_bank.get(bank)
            if last is not None:
                (l0, l1), lidx = last
                if r1 <= l0 or l1 <= r0:
                    selfwait = [lidx]
            self.pe_bank[bank] = ((r0, r1), self.cnt.get('pe', 0))
        pr = [r for r in reads if r.startswith('ps')]
        if pr:
            reads = [r for r in reads if not r.startswith('ps')]
            writes = list(writes) + [r for r in pr if r not in writes]
        if dma:
            j = self.ndma[eng]
            self.ndma[eng] += 1
            counter = 'dma_%s_%d' % (eng, j % self.NROT)
        else:
            counter = eng
        idx = self.cnt.get(counter, 0)
        self.cnt[counter] = idx + 1
        deps = {}

        def need(c, j):
            if deps.get(c, -1) < j:
                deps[c] = j
        for r in reads:
            if r in self.last_w:
                need(*self.last_w[r])
            for c, j in self.cwr.get(r, {}).items():
                need(c, j)
        for w in writes:
            if w in self.last_w:
                need(*self.last_w[w])
            for c, j in self.readers.get(w, {}).items():
                need(c, j)
            for c, j in self.cwr.get(w, {}).items():
                need(c, j)
        for w in cwrites:
            if w in self.last_w:
                need(*self.last_w[w])
            for c, j in self.readers.get(w, {}).items():
                need(c, j)
        waits = []
        for j in selfwait:
            if self.waited[eng].get(counter, -1) < j:
                self.waited[eng][counter] = j
                waits.append((counter, j))
        for c, j in deps.items():
            if c == counter and eng == 'pe' and not dma:
                continue
            if self.waited[eng].get(c, -1) >= j:
                continue
            self.waited[eng][c] = j
            waits.append((c, j))
        self.ops[eng].append((waits, fn, counter, idx))
        for w in writes:
            self.last_w[w] = (counter, idx)
            self.readers[w] = {}
            self.cwr[w] = {}
        for w in cwrites:
            d = self.cwr.setdefault(w, {})
            if d.get(counter, -1) < idx:
                d[counter] = idx
        for r in reads:
            if r in writes:
                continue
            d = self.readers.setdefault(r, {})
            if d.get(counter, -1) < idx:
                d[counter] = idx


    def begin(self, name):
        self._cur = name
        self._streams.setdefault(name, [])

    def merge(self):
        streams = {k: v for k, v in self._streams.items() if v}
        self._cur = None
        self._streams = {}
        pos = {k: 0 for k in streams}
        total = sum(len(v) for v in streams.values())
        for _ in range(total):
            k = min((k for k in streams if pos[k] < len(streams[k])), key=lambda k: pos[k] / len(streams[k]))
            eng, fn, reads, writes, dma, cwrites, pebank = streams[k][pos[k]]
            pos[k] += 1
            self.op(eng, fn, reads=reads, writes=writes, dma=dma, cwrites=cwrites, pebank=pebank)

    @staticmethod
    def _prange(ap):
        st, n = ap.ap[0]
        p0 = (ap.offset // st) if st else 0
        return (p0, p0 + n)

    def mm(self, out, lhsT, rhs, start=True, stop=True):
        r0, r1 = self._prange(lhsT)
        self.op('pe', lambda e: e.matmul(out, lhsT=lhsT, rhs=rhs, start=start, stop=stop),
                reads=K(lhsT, rhs), writes=K(out), pebank=(out.tensor.name, r0, r1))

    def tr(self, out, in_, ident):
        r0, r1 = self._prange(in_)
        self.op('pe', lambda e: e.transpose(out, in_, ident), reads=K(in_, ident), writes=K(out),
                pebank=(out.tensor.name, r0, r1))

    def act(self, out, in_, func, bias=None, scale=None, accum_out=None, eng='act'):
        kw = {}
        if bias is not None:
            kw['bias'] = bias
        if scale is not None:
            kw['scale'] = scale
        if accum_out is not None:
            kw['accum_out'] = accum_out
        self.op('act', lambda e: e.activation(out=out, in_=in_, func=func, **kw),
                reads=K(in_, bias, scale), writes=K(out, accum_out))

    def cp(self, eng, out, in_):
        if eng == 'act':
            self.op('act', lambda e: e.copy(out=out, in_=in_), reads=K(in_), writes=K(out))
        else:
            self.op(eng, lambda e: e.tensor_copy(out=out, in_=in_), reads=K(in_), writes=K(out))

    def tt(self, eng, out, in0, in1, op):
        self.op(eng, lambda e: e.tensor_tensor(out=out, in0=in0, in1=in1, op=op),
                reads=K(in0, in1), writes=K(out))

    def ts(self, eng, out, in0, s1, op0, s2=None, op1=None, accum_out=None):
        kw = {}
        if op1 is not None:
            kw['op1'] = op1
        if accum_out is not None:
            kw['accum_out'] = accum_out
        self.op(eng, lambda e: e.tensor_scalar(out=out, in0=in0, scalar1=s1, scalar2=s2, op0=op0, **kw),
                reads=K(in0, s1, s2), writes=K(out, accum_out))

    def stt(self, eng, out, in0, scalar, in1, op0, op1):
        self.op(eng, lambda e: e.scalar_tensor_tensor(out=out, in0=in0, scalar=scalar, in1=in1, op0=op0, op1=op1),
                reads=K(in0, scalar, in1), writes=K(out))

    def rsum(self, eng, out, in_):
        self.op(eng, lambda e: e.reduce_sum(out=out, in_=in_, axis=AX.X), reads=K(in_), writes=K(out))

    def memset(self, eng, out, val):
        self.op(eng, lambda e: e.memset(out, val), writes=K(out))

    def dma(self, out, in_, eng='sp', comm=False):
        if comm:
            self.op(eng, lambda e: e.dma_start(out=out, in_=in_), reads=K(in_), cwrites=K(out), dma=True)
        else:
            self.op(eng, lambda e: e.dma_start(out=out, in_=in_), reads=K(in_), writes=K(out), dma=True)


    def dmac(self, out, in_):
        self.op('pool', lambda e: e.dma_start(out=out, in_=in_), reads=K(in_), cwrites=K(out), dma=True)

    def _breg(self, e, bound):
        if self._bound_reg is None:
            self._bound_reg = (bound, e.to_reg(bound))
        assert self._bound_reg[0] == bound
        return self._bound_reg[1]

    def scatter(self, out_dram, idx_ap, in_sb, bound):
        self.op('pool', lambda e: e.indirect_dma_start(
            out=out_dram, out_offset=bass.IndirectOffsetOnAxis(ap=idx_ap, axis=0),
            in_=in_sb, in_offset=None, bounds_check=self._breg(e, bound), oob_is_err=False),
            reads=K(in_sb, idx_ap), cwrites=K(out_dram), dma=True)

    def gather(self, out_sb, in_dram, idx_ap, bound):
        self.op('pool', lambda e: e.indirect_dma_start(
            out=out_sb, out_offset=None, in_=in_dram,
            in_offset=bass.IndirectOffsetOnAxis(ap=idx_ap, axis=0), bounds_check=self._breg(e, bound), oob_is_err=False),
            reads=K(in_dram, idx_ap), writes=K(out_sb), dma=True)

    def treduce(self, eng, out, in_, op=None):
        op = ALU.add if op is None else op
        self.op(eng, lambda e: e.tensor_reduce(out=out, in_=in_, axis=AX.X, op=op), reads=K(in_), writes=K(out))

    def recip(self, out, in_):
        self.op('dve', lambda e: e.reciprocal(out=out, in_=in_), reads=K(in_), writes=K(out))

    def _ch(self, counter):
        return self.CH if counter.startswith('dma_') else self.CHC

    def _sem(self, counter, idx):
        return self.sems[(counter, idx // self._ch(counter))]

    def _val(self, counter, idx):
        inc = 16 if counter.startswith('dma_') else 1
        return (idx % self._ch(counter) + 1) * inc

    def emit(self):
        nc = self.nc
        with ExitStack() as st:
            for counter, n in self.cnt.items():
                ch = self._ch(counter)
                for k in range((n + ch - 1) // ch):
                    self.sems[(counter, k)] = st.enter_context(nc.semaphore('s_%s_%d' % (counter, k)))
            block = st.enter_context(nc.Block())

            def mk(engname):
                def body(e):
                    for waits, fn, counter, idx in self.ops[engname]:
                        for c, j in waits:
                            e.wait_ge(self._sem(c, j), self._val(c, j))
                        ins = fn(e)
                        ins.then_inc(self._sem(counter, idx), 16 if counter.startswith('dma_') else 1)
                    if engname == 'sp':
                        for c, n in self.cnt.items():
                            if n > 0:
                                e.wait_ge(self._sem(c, n - 1), self._val(c, n - 1))
                return body
            block.tensor(mk('pe'))
            block.scalar(mk('act'))
            block.vector(mk('dve'))
            block.gpsimd(mk('pool'))
            block.sync(mk('sp'))


TM_FIELDS = [
    ('dt_bias', 4), ('a_log', 4), ('ssd_d', 4), ('ssd_ng', 256),
    ('rw_mu', 768), ('rw_w0', 256), ('rw_a0', 256), ('rw_kk', 256), ('rw_ka', 256), ('rw_rk', 256),
    ('rw_lng', 256), ('rw_lnb', 256),
    ('gl_gb', 128), ('gl_ng', 256),
    ('ml_ib', 4), ('ml_fb', 4), ('ml_ng', 256),
    ('ln1_g', 1024), ('ln1_b', 1024), ('ln2_g', 1024), ('ln2_b', 1024), ('rt_b', 32),
]
TM_OFF = {}
_o = 0
for _n, _w in TM_FIELDS:
    TM_OFF[_n] = (_o, _w)
    _o += _w
TM_W = _o
CM_W = 41


def h4(ap, h=4):
    return ap.rearrange("p (h d) -> p h d", h=h)


def b4(ap, w, h=4):
    return ap.rearrange("p (h o) -> p h o", o=1).to_broadcast([128, h, w])


import os
CONVPS = int(os.environ.get("CONVPS", "0"))


def build(n_layers=DEPTH, debug=False, stop_after=None, ntiles=NT, skip=(), maxops=None):
    nc = bass.Bass("TRN2", target_bir_lowering=False)
    P = Prog(nc)
    P.maxops = maxops
    LD = n_layers
    NED = 1 if stop_after in ('mixers', 'phaseA') else NE

    def din(name, shape, dt=F32):
        return nc.dram_tensor(name, list(shape), dt, kind="ExternalInput").ap()
    x_in = din('x', [T, D])
    p_in = din('p', [LD, T, 256])
    w_in = din('w_in', [LD, D, NCOL])
    w_out = din('w_out', [LD, D, D])
    tmrow = din('tmrow', [LD, 1, TM_W])
    cmcol = din('cmcol', [LD, 128, CM_W])
    lora_up = din('lora_up', [LD, 128, 256])
    gla_up = din('gla_up', [LD, 16, 128])
    router_w = din('router_w', [LD, D, NE])
    w_gu = din('w_gu', [LD, NED, D, 2 * D])
    b_gu = din('b_gu', [LD, NE, 128, 16])
    w_dn = din('w_dn', [LD, NED, D, D])
    b_dn = din('b_dn', [LD, NE, 1, D])
    ple_g = din('ple_g', [LD, D, D])
    ple_p = din('ple_p', [LD, 256, D])
    consts = din('consts', [128, 6, 128])
    consts2 = din('consts2', [128, 324])
    out = nc.dram_tensor('out', [T, D], F32, kind="ExternalOutput").ap()
    xbuf = nc.dram_tensor('xbuf', [T, D], F32).ap()
    x1buf = nc.dram_tensor('x1buf', [T, D], F32).ap()
    xg = nc.dram_tensor('xg', [NE * CAP, D], BF16).ap()
    yg = nc.dram_tensor('yg', [NE * CAP, D], F32).ap()
    dbg = {}
    if debug:
        for nm, w in [('d_ssd', 256), ('d_rwkv', 256), ('d_gla', 256), ('d_mlstm', 256), ('d_x1', 1024),
                      ('d_ffn', 1024), ('d_logits', 32), ('d_dest', 4), ('d_gate', 4)]:
            dbg[nm] = nc.dram_tensor(nm, [T, w], F32, kind="ExternalOutput").ap()

    with ExitStack() as st:
        def sb(name, shape, dt=F32):
            return st.enter_context(nc.sbuf_tensor(name, list(shape), dt))

        def psb(name, shape, dt=F32):
            return st.enter_context(nc.psum_tensor(name, list(shape), dt))

        R0 = sb('R0', [128, 8 * NCOL], BF16)
        R1 = sb('R1', [128, 24576], BF16)
        tmc = sb('tmc', [128, TM_W])
        cmc = sb('cmc', [128, CM_W])
        cst = sb('cst', [128, 6, 128])
        cst2 = sb('cst2', [128, 324])
        identb = sb('identb', [128, 128], BF16)
        lup = sb('lup', [128, 256])
        gup = sb('gup', [16, 128])
        rtw = sb('rtw', [128, 8, NE])
        ones = sb('ones', [128, 128])
        G = [sb('G%d' % i, [128, 1024]) for i in range(7)]
        H = [sb('H%d' % i, [128, 1024], BF16) for i in range(5)]
        xTe = [sb('xTe%d' % i, [128, 8, 129], BF16) for i in range(2)]
        cbuf = [sb('cbuf%d' % i, [128, 131]) for i in range(8)]
        small = sb('small', [128, 128])
        small2 = sb('small2', [128, 32])
        smallS = sb('smallS', [128, 80])
        small2S = sb('small2S', [128, 16])
        Q2 = sb('Q2', [128, 512]); Q3 = sb('Q3', [128, 1024]); Q4 = sb('Q4', [128, 1024])
        J0 = sb('J0', [128, 512], BF16); J1 = sb('J1', [128, 640], BF16); J2 = sb('J2', [128, 512], BF16)
        ssd_S = sb('ssd_S', [128, 256]); ssd_Sb = sb('ssd_Sb', [128, 256], BF16)
        rw_S = [sb('rw_S%d' % i, [128, 64]) for i in range(2)]
        rw_Sb = [sb('rw_Sb%d' % i, [128, 64], BF16) for i in range(2)]
        gl_S = sb('gl_S', [128, 256]); gl_Sb = sb('gl_Sb', [128, 256], BF16)
        ml_S = [sb('ml_S%d' % i, [128, 65]) for i in range(2)]
        ml_Sb = [sb('ml_Sb%d' % i, [128, 65], BF16) for i in range(2)]
        dest_all = sb('dest_all', [128, NT, 4], I32)
        gate_all = sb('gate_all', [128, NT, 4])
        cntb = sb('cntb', [128, NE])
        rsc = sb('rsc', [128, 256])
        idx8 = sb('idx8', [128, 8], U32)
        bgu = [sb('bgu%d' % i, [128, 16]) for i in range(2)]
        PS = [psb('ps%d' % i, [128, 512]) for i in range(8)]
        PSB = PS[7][:].bitcast(BF16)

        ident = cst[:, 0, :]
        TRI = cst[:, 1, :]
        TRIS = cst[:, 2, :]
        BTRI = cst[:, 3, :]
        BTRIS = cst[:, 4, :]
        BLOW = cst[:, 5, :]
        GLMASK = cst2[:, 0:256]
        IOTA32 = cst2[:, 256:288]

        def tmf(name):
            o, w = TM_OFF[name]
            return tmc[:, o:o + w]

        P.dma(cst[:], consts)
        P.dma(cst2[:], consts2)
        P.cp('dve', identb[:], cst[:, 0, :])
        P.memset('dve', ones[:], 1.0)

        W3 = R0[:, 0:8 * NCOL].rearrange("p (k n) -> p k n", k=8)
        WO = R1[:, 0:8192].rearrange("p (k n) -> p k n", k=8)
        PG = R1[:, 8192:16384].rearrange("p (k n) -> p k n", k=8)
        PP = R1[:, 16384:18432].rearrange("p (k n) -> p k n", k=2)

        def layernorm(dst, src, gname, bname, sc, sq=None):
            sq = G[6] if sq is None else sq
            P.rsum('dve', sc[:, 0:1], src)
            P.ts('dve', sc[:, 1:2], sc[:, 0:1], -1.0 / D, ALU.mult)
            P.act(sq[:], src, AF.Square, bias=sc[:, 1:2], accum_out=sc[:, 2:3])
            P.act(sc[:, 3:4], sc[:, 2:3], AF.Ln, bias=LN_EPS, scale=1.0 / D)
            P.act(sc[:, 3:4], sc[:, 3:4], AF.Exp, scale=-0.5)
            P.ts('dve', dst, src, sc[:, 1:2], ALU.add, sc[:, 3:4], ALU.mult)
            P.tt('dve', dst, dst, tmf(gname), ALU.mult)
            P.tt('pool', dst, dst, tmf(bname), ALU.add)

        def groupnorm(y, nh, gain, bias, eps, center, sc, tmp):
            hd = 256 // nh
            y3 = h4(y, nh)
            t3 = h4(tmp, nh)
            if center:
                P.treduce('dve', sc[:, 0:nh], y3)
                P.ts('dve', sc[:, 0:nh], sc[:, 0:nh], -1.0 / hd, ALU.mult)
                P.tt('dve', y3, y3, b4(sc[:, 0:nh], hd, nh), ALU.add)
            P.tt('pool', tmp, y, y, ALU.mult)
            P.treduce('dve', sc[:, 4:4 + nh], t3)
            P.act(sc[:, 4:4 + nh], sc[:, 4:4 + nh], AF.Ln, bias=eps, scale=1.0 / hd)
            P.act(sc[:, 4:4 + nh], sc[:, 4:4 + nh], AF.Exp, scale=-0.5)
            P.tt('dve', y3, y3, b4(sc[:, 4:4 + nh], hd, nh), ALU.mult)
            P.tt('dve', y, y, gain, ALU.mult)
            if bias is not None:
                P.tt('dve', y, y, bias, ALU.add)

        def load_r1(layer):
            for k in range(8):
                P.dmac(WO[:, k, :], w_out[layer, k * 128:(k + 1) * 128, :])
            for k in range(8):
                P.dmac(PG[:, k, :], ple_g[layer, k * 128:(k + 1) * 128, :])
            for k in range(2):
                P.dmac(PP[:, k, :], ple_p[layer, k * 128:(k + 1) * 128, :])

        for layer in range(n_layers):
            xsrc = x_in if layer == 0 else xbuf
            xdst = out if layer == n_layers - 1 else xbuf
            for k in range(8):
                P.dmac(W3[:, k, :], w_in[layer, k * 128:(k + 1) * 128, :])
            load_r1(layer)
            P.dma(tmc[:], tmrow[layer].partition_broadcast(128))
            P.dma(cmc[:], cmcol[layer])
            P.dma(lup[:], lora_up[layer])
            P.dma(gup[:], gla_up[layer])
            P.dma(rtw[:], router_w[layer].rearrange("(k p) e -> p k e", p=128))
            P.act(tmf('a_log'), tmf('a_log'), AF.Exp)
            P.ts('dve', tmf('a_log'), tmf('a_log'), -1.0, ALU.mult)
            for s_ in [ssd_S, gl_S] + rw_S + ml_S + [cntb]:
                P.memset('dve', s_[:], 0.0)
            for s_ in [ssd_Sb, gl_Sb] + rw_Sb + ml_Sb:
                P.memset('pool', s_[:], 0.0)
            for cb in cbuf:
                P.memset('pool', cb[:, 0:3], 0.0)
            P.memset('dve', xTe[0][:, :, 0:1], 0.0)

            for c in range(ntiles):
                xt = G[0]
                xe = xTe[c % 2]
                xn = xTe[(c + 1) % 2]
                P.dma(xt[:], xsrc[c * 128:(c + 1) * 128, :])
                for k in range(8):
                    P.tr(PS[k // 4][:, (k % 4) * 128:(k % 4 + 1) * 128], xt[:, k * 128:(k + 1) * 128], ident)
                for hf in range(2):
                    P.cp('act' if hf == 0 else 'dve', xe[:, hf * 4:(hf + 1) * 4, 1:129],
                         PS[hf][:, :].rearrange("p (k n) -> p k n", k=4))
                P.cp('pool', xn[:, :, 0:1], xe[:, :, 128:129])

                def cm_mm(ps_ap, c0, ncols, shifted=False):
                    for k in range(8):
                        rhs = xe[:, k, 0:128] if shifted else xe[:, k, 1:129]
                        P.mm(ps_ap, W3[:, k, c0:c0 + ncols], rhs, start=(k == 0), stop=(k == 7))

                def tm_mm(ps_ap, c0, ncols, shifted=False):
                    for k in range(8):
                        lhsT = xe[:, k, 0:128] if shifted else xe[:, k, 1:129]
                        P.mm(ps_ap, lhsT, W3[:, k, c0:c0 + ncols], start=(k == 0), stop=(k == 7))

                def conv_silu(ci, wcol, bcol, c0, dst):
                    cb = cbuf[ci]
                    ps = (PS[0], PS[1])[ci % 2]
                    cm_mm(ps[:, 0:128], c0, 128)
                    P.cp('act', cb[:, 3:131], ps[:, 0:128])
                    tmp = Q4[:, 384:512]
                    P.ts('dve', tmp, cb[:, 0:128], cmc[:, wcol:wcol + 1], ALU.mult, cmc[:, bcol:bcol + 1], ALU.add)
                    for j in range(1, 4):
                        P.stt('dve', tmp, cb[:, j:j + 128], cmc[:, wcol + j:wcol + j + 1], tmp, ALU.mult, ALU.add)
                    P.act(dst, tmp, AF.Silu)
                    P.cp('pool', cb[:, 0:3], cb[:, 128:131])

                Y = G[1]

                P.begin('s2')
                if 'ssd' not in skip:
                    cmf = Q2
                    for ci in range(4):
                        conv_silu(ci, ci * 4, 16 + ci, SSD0 + 256 + ci * 128, cmf[:, ci * 128:(ci + 1) * 128])
                    P.cp('pool', J0[:, 0:512], cmf[:, 0:512])
                    BT = J0[:, 256:384]
                    CT = J0[:, 384:512]
                    ps = PS[4]
                    for j in range(3):
                        P.tr(ps[:, j * 128:(j + 1) * 128], cmf[:, j * 128:(j + 1) * 128], ident)
                    xs = Q3[:, 0:256]
                    P.cp('act', xs, ps[:, 0:256])
                    Btm = J1[:, 0:128]
                    P.cp('dve', Btm, ps[:, 256:384])
                    ps = PS[5]
                    tm_mm(ps[:, 0:256], SSD0 + 0, 256)
                    tm_mm(ps[:, 256:260], SSD0 + 768, 4)
                    zs = Q3[:, 256:512]
                    P.act(zs, ps[:, 0:256], AF.Silu)
                    sc = smallS
                    dt = sc[:, 0:4]
                    P.tt('dve', dt, ps[:, 256:260], tmf('dt_bias'), ALU.add)
                    P.act(dt, dt, AF.Exp)
                    P.act(dt, dt, AF.Ln, bias=1.0)
                    adt = sc[:, 4:8]
                    P.tt('dve', adt, dt, tmf('a_log'), ALU.mult)
                    ps = PS[0]
                    P.mm(ps[:, 0:4], TRI, adt)
                    P.mm(ps[:, 4:8], ones[:], adt)
                    acum = sc[:, 8:12]
                    P.cp('dve', acum, ps[:, 0:4])
                    ea = sc[:, 12:16]
                    P.act(ea, ps[:, 0:4], AF.Exp)
                    dsx = sc[:, 16:20]
                    P.tt('dve', dsx, ps[:, 4:8], acum, ALU.subtract)
                    P.act(dsx, dsx, AF.Exp)
                    eal = sc[:, 20:24]
                    P.act(eal, ps[:, 4:8], AF.Exp)
                    xdt = J1[:, 128:384]
                    xdt2 = J1[:, 384:640]
                    xdtf = Q3[:, 512:768]
                    P.tt('dve', h4(xdtf), h4(xs), b4(dt, 64), ALU.mult)
                    P.cp('pool', xdt, xdtf)
                    P.tt('dve', h4(xdt2), h4(xdtf), b4(dsx, 64), ALU.mult)
                    ps = PS[0]
                    P.mm(ps[:, 0:128], BT, CT)
                    GTm = Q4[:, 0:128]
                    P.tt('dve', GTm, ps[:, 0:128], TRI, ALU.mult)
                    psy = PS[1]
                    for h in range(4):
                        psl = (PS[4], PS[5])[h % 2]
                        adt_bc = Q4[:, 128:256]
                        P.cp('pool', adt_bc, adt[:, h:h + 1].to_broadcast([128, 128]))
                        P.mm(psl[:, 0:128], adt_bc, TRI)
                        lt = Q4[:, 256:384]
                        P.ts('dve', lt, psl[:, 0:128], acum[:, h:h + 1], ALU.subtract, 0.0, ALU.min)
                        P.act(lt, lt, AF.Exp)
                        MT = J2[:, h * 128:(h + 1) * 128]
                        P.tt('dve', MT, lt, GTm, ALU.mult)
                        P.mm(psy[:, h * 64:(h + 1) * 64], MT, xdt[:, h * 64:(h + 1) * 64])
                    pso = PS[0]
                    P.mm(pso[:, 256:512], CT, ssd_Sb[:])
                    yv = Y[:, 0:256]
                    P.tt('dve', h4(yv), h4(pso[:, 256:512]), b4(ea, 64), ALU.mult)
                    P.tt('dve', yv, yv, psy[:, 0:256], ALU.add)
                    psn = PS[0]
                    P.mm(psn[:, 128:384], Btm, xdt2)
                    P.tt('dve', h4(ssd_S[:]), h4(ssd_S[:]), b4(eal, 64), ALU.mult)
                    P.tt('dve', ssd_S[:], ssd_S[:], psn[:, 128:384], ALU.add)
                    P.cp('pool', ssd_Sb[:], ssd_S[:])
                    t1 = Q4[:, 512:768]
                    P.tt('pool', h4(t1), h4(xs), b4(tmf('ssd_d'), 64), ALU.mult)
                    P.tt('dve', yv, yv, t1, ALU.add)
                    P.tt('dve', yv, yv, zs, ALU.mult)
                    groupnorm(yv, 1, tmf('ssd_ng'), None, 1e-5, False, small2S, Q4[:, 768:1024])

                if 'gla' not in skip:
                    ps = PS[5]
                    tm_mm(ps[:, 0:512], GL0, 512)
                    qkv = Q2
                    P.cp('act', qkv[:, 0:512], ps[:, 0:512])
                    vb = J1[:, 0:256]
                    P.cp('pool', vb, qkv[:, 256:512])
                    ps = PS[0]
                    tm_mm(ps[:, 0:256], GL0 + 528, 256)
                    og = Q3[:, 0:256]
                    P.act(og, ps[:, 0:256], AF.Silu)
                    ps = PS[0]
                    cm_mm(ps[0:16, 0:128], GL0 + 512, 16)
                    gdT = Q3[0:16, 256:384]
                    P.cp('act', gdT, ps[0:16, 0:128])
                    ps = PS[1]
                    P.mm(ps[:, 0:128], gdT, gup[:])
                    la = Q3[:, 384:512]
                    P.tt('dve', la, ps[:, 0:128], tmf('gl_gb'), ALU.add)
                    P.act(la, la, AF.Exp, scale=-1.0)
                    P.act(la, la, AF.Ln, bias=1.0)
                    P.ts('dve', la, la, -1.0 / 16.0, ALU.mult)
                    ps = PS[4]
                    P.mm(ps[:, 0:128], TRI, la)
                    P.mm(ps[:, 128:256], ones[:], la)
                    P.mm(ps[:, 256:257], la, ones[:, 0:1])
                    ebc = Q3[:, 512:640]
                    P.act(ebc, ps[:, 0:128], AF.Exp)
                    enb = Q3[:, 640:768]
                    P.act(enb, ps[:, 0:128], AF.Exp, scale=-1.0)
                    kd = Q3[:, 768:896]
                    bc_sb = Q3[:, 896:1024]
                    P.cp('dve', bc_sb, ps[:, 0:128])
                    P.tt('dve', kd, ps[:, 128:256], bc_sb, ALU.subtract)
                    P.act(kd, kd, AF.Exp)
                    ebl = smallS[:, 32:33]
                    P.act(ebl, ps[:, 256:257], AF.Exp)
                    qd = Q4[:, 0:128]
                    P.stt('dve', qd, qkv[:, 0:128], 32 ** -0.5, ebc, ALU.mult, ALU.mult)
                    ki = Q4[:, 128:256]
                    P.tt('dve', ki, qkv[:, 128:256], enb, ALU.mult)
                    kdb = J1[:, 256:384]
                    P.tt('dve', kdb, qkv[:, 128:256], kd, ALU.mult)
                    ps = PS[0]
                    P.tr(ps[:, 0:128], qd, ident)
                    P.tr(ps[:, 128:256], ki, ident)
                    qdT = J1[:, 384:512]
                    P.cp('act', qdT, ps[:, 0:128])
                    pso = PS[1]
                    P.mm(pso[:, 0:256], qdT, gl_Sb[:], start=True, stop=False)
                    for h in range(4):
                        kim = J1[:, 512:640]
                        P.ts('dve', kim, ps[:, 128:256], cst2[:, 320 + h:321 + h], ALU.mult)
                        psa = (PS[4], PS[5])[h % 2]
                        P.mm(psa[:, 384:512], kim, qdT)
                        at = J2[:, h * 128:(h + 1) * 128]
                        P.tt('dve', at, psa[:, 384:512], TRI, ALU.mult)
                        P.mm(pso[:, h * 64:(h + 1) * 64], at, vb[:, h * 64:(h + 1) * 64], start=False, stop=(h == 3))
                    yv = Y[:, 512:768]
                    P.cp('act', yv, pso[:, 0:256])
                    psn = PS[0]
                    P.mm(psn[:, 256:512], kdb, vb)
                    P.ts('dve', gl_S[:], gl_S[:], ebl, ALU.mult)
                    t1 = Q4[:, 256:512]
                    P.tt('dve', t1, psn[:, 256:512], GLMASK, ALU.mult)
                    P.tt('dve', gl_S[:], gl_S[:], t1, ALU.add)
                    P.cp('pool', gl_Sb[:], gl_S[:])
                    groupnorm(yv, 4, tmf('gl_ng'), None, 1e-5, False, small2S, Q4[:, 768:1024])
                    P.tt('dve', yv, yv, og, ALU.mult)

                P.begin('s1')
                if 'rwkv' not in skip:
                    rkv = G[2]
                    for half in range(2):
                        c0 = RW0 + half * 384
                        tm_mm(PS[2][:, 0:384], c0, 384)
                        tm_mm(PS[3][:, 0:384], c0, 384, shifted=True)
                        cur = G[4][:, 0:384]
                        P.cp('act', cur, PS[2][:, 0:384])
                        dl = G[4][:, 384:768]
                        P.tt('dve', dl, PS[3][:, 0:384], cur, ALU.subtract)
                        P.tt('pool', dl, dl, tmf('rw_mu')[:, half * 384:(half + 1) * 384], ALU.mult)
                        P.tt('dve', rkv[:, half * 384:(half + 1) * 384], cur, dl, ALU.add)
                    r_ = rkv[:, 0:256]
                    k_ = rkv[:, 256:512]
                    v_ = rkv[:, 512:768]
                    cm_mm(PS[6][:, 0:128], RW0 + 768, 128)
                    cm_mm(PS[6][:, 128:256], RW0 + 768, 128, shifted=True)
                    lo = G[3][:, 0:128]
                    P.cp('act', lo, PS[6][:, 0:128])
                    dl = G[3][:, 128:256]
                    P.tt('dve', dl, PS[6][:, 128:256], lo, ALU.subtract)
                    P.stt('dve', lo, dl, cmc[:, 40:41], lo, ALU.mult, ALU.add)
                    P.act(lo[0:32, :], lo[0:32, :], AF.Tanh)
                    P.act(lo[64:128, :], lo[64:128, :], AF.Sigmoid)
                    ps = PS[7]
                    P.mm(ps[:, 0:256], lo[0:32, :], lup[0:32, :])
                    ps2 = PS[6]
                    P.mm(ps2[:, 0:256], lo[32:64, :], lup[32:64, :])
                    P.mm(ps2[:, 256:512], lo[64:128, :], lup[64:128, :])
                    gg = G[3][:, 256:512]
                    P.cp('act', gg, ps2[:, 256:512])
                    lw = G[3][:, 512:768]
                    P.tt('dve', lw, ps[:, 0:256], tmf('rw_w0'), ALU.add)
                    P.act(lw, lw, AF.Exp, scale=-1.0)
                    P.act(lw, lw, AF.Ln, bias=1.0)
                    P.ts('dve', lw, lw, -1.0, ALU.mult, -0.5, ALU.add)
                    P.act(lw, lw, AF.Exp)
                    P.ts('dve', lw, lw, -1.0, ALU.mult)
                    av = G[3][:, 768:1024]
                    P.tt('dve', av, ps2[:, 0:256], tmf('rw_a0'), ALU.add)
                    P.act(av, av, AF.Sigmoid)
                    kk = G[4][:, 0:256]
                    P.tt('dve', kk, k_, tmf('rw_kk'), ALU.mult)
                    sq = G[4][:, 256:512]
                    P.tt('pool', sq, kk, kk, ALU.mult)
                    nrm = small[:, 80:84]
                    P.treduce('dve', nrm, h4(sq))
                    P.ts('dve', nrm, nrm, 1e-24, ALU.max)
                    P.act(nrm, nrm, AF.Ln)
                    P.act(nrm, nrm, AF.Exp, scale=-0.5)
                    P.tt('dve', h4(kk), h4(kk), b4(nrm, 64), ALU.mult)
                    km = G[4][:, 256:512]
                    P.ts('dve', km, av, -1.0, ALU.add)
                    P.tt('dve', km, km, tmf('rw_ka'), ALU.mult)
                    P.ts('dve', km, km, 1.0, ALU.add)
                    P.tt('dve', km, km, k_, ALU.mult)
                    bt_ = G[4][:, 512:768]
                    P.tt('pool', bt_, r_, km, ALU.mult)
                    P.tt('pool', bt_, bt_, tmf('rw_rk'), ALU.mult)
                    bon = small[:, 84:88]
                    P.treduce('dve', bon, h4(bt_))
                    ps = PS[7]
                    P.mm(ps[:, 0:256], BTRI, lw)
                    Wt = G[4][:, 512:768]
                    P.act(Wt, ps[:, 0:256], AF.Exp)
                    Wi = G[4][:, 768:1024]
                    P.act(Wi, ps[:, 0:256], AF.Exp, scale=-1.0)
                    Wp = G[5][:, 0:256]
                    cwsb = G[5][:, 256:512]
                    P.cp('dve', cwsb, ps[:, 0:256])
                    P.tt('dve', Wp, cwsb, lw, ALU.subtract)
                    P.act(Wp, Wp, AF.Exp)
                    ps = PS[2]
                    for hp in range(2):
                        for j in range(2):
                            P.mm(ps[:, hp * 2 + j:hp * 2 + j + 1], lw[j * 64:(j + 1) * 64, hp * 128:(hp + 1) * 128],
                                 ones[j * 64:(j + 1) * 64, 0:1])
                    WL = small[:, 88:92]
                    P.act(WL, ps[:, 0:4], AF.Exp)
                    rt = G[5][:, 256:512]
                    P.tt('dve', rt, r_, Wt, ALU.mult)
                    at_ = G[5][:, 512:768]
                    P.stt('dve', at_, kk, -1.0, Wp, ALU.mult, ALU.mult)
                    btl = G[5][:, 768:1024]
                    P.tt('dve', btl, kk, av, ALU.mult)
                    P.tt('dve', btl, btl, Wi, ALU.mult)
                    kt = G[4][:, 0:256]
                    P.tt('dve', kt, km, Wi, ALU.mult)
                    btb = H[1][:, 0:256]
                    ktb = H[1][:, 256:512]
                    vbb = H[1][:, 512:768]
                    P.cp('pool', btb, btl)
                    P.cp('pool', ktb, kt)
                    P.cp('pool', vbb, v_)
                    cmT = {}
                    for ai, (nm, arr) in enumerate([('r', rt), ('a', at_), ('b', btl), ('k', kt)]):
                        ps = PS[2 + ai % 2]
                        P.tr(ps[:, 0:128], arr[:, 0:128], ident)
                        P.tr(ps[:, 128:256], arr[:, 128:256], ident)
                        dstT = G[6][:, ai * 256:(ai + 1) * 256]
                        P.cp('act' if ai % 2 == 0 else 'dve', dstT, ps[:, 0:256])
                        cmT[nm] = dstT
                    o_rw = Y[:, 256:512]
                    def rw_head(h, Fs, Bs, bk1, bk2):
                        hp, hq = h // 2, (h % 2) * 64
                        sl = slice(hq, hq + 64)
                        rT = cmT['r'][sl, hp * 128:(hp + 1) * 128]
                        aT = cmT['a'][sl, hp * 128:(hp + 1) * 128]
                        bT = cmT['b'][sl, hp * 128:(hp + 1) * 128]
                        kT = cmT['k'][sl, hp * 128:(hp + 1) * 128]
                        psA = bk1
                        P.mm(psA[:, 0:128], bT, aT)
                        P.mm(psA[:, 128:256], aT, bT)
                        P.mm(psA[:, 256:384], kT, aT)
                        psB = bk2
                        P.mm(psB[:, 0:128], bT, rT)
                        P.mm(psB[:, 128:256], kT, rT)
                        Pm = Fs[:, 0:128]
                        PTm = Fs[:, 128:256]
                        TT = Fs[:, 256:384]
                        P.tt('dve', Pm, psA[:, 0:128], BTRIS, ALU.mult)
                        P.tt('dve', PTm, psA[:, 128:256], BLOW, ALU.mult)
                        AakT = Bs[:, 0:128]
                        P.tt('dve', AakT, psA[:, 256:384], BTRIS, ALU.mult)
                        ArbT = Bs[:, 128:256]
                        P.tt('dve', ArbT, psB[:, 0:128], BTRI, ALU.mult)
                        ArkT = Bs[:, 256:384]
                        P.tt('dve', ArkT, psB[:, 128:256], BTRI, ALU.mult)
                        P.tt('dve', TT, Pm, ident, ALU.add)
                        for step in range(5):
                            psq = bk1
                            P.mm(psq[:, 0:128], PTm, Pm)
                            P.mm(psq[:, 128:256], Pm, PTm)
                            P.cp('dve', Pm, psq[:, 0:128])
                            P.cp('act', PTm, psq[:, 128:256])
                            psq2 = bk2
                            P.mm(psq2[:, 256:384], PTm, TT)
                            P.tt('dve', TT, TT, psq2[:, 256:384], ALU.add)
                        TTb = Bs[:, 384:512]
                        P.cp('pool', TTb, TT)
                        aTb = Bs[:, 512:640]
                        rTb = Bs[:, 640:768]
                        P.cp('pool', aTb[sl, :], aT)
                        P.cp('pool', rTb[sl, :], rT)
                        for j in range(2):
                            js = slice(j * 64, (j + 1) * 64)
                            psx = bk2
                            vh = vbb[js, h * 64:(h + 1) * 64]
                            P.mm(psx[js, 0:64], aTb[sl, js], rw_Sb[hp][sl, :], start=True, stop=False)
                            P.mm(psx[js, 0:64], AakT[js, js], vh, start=False, stop=True)
                            X1 = Bs[:, 768:832]
                            P.cp('act', X1[js, :], psx[js, 0:64])
                            P.mm(psx[js, 64:128], TTb[js, js], X1[js, :])
                            Ub = Bs[:, 832:896]
                            P.cp('act', Ub[js, :], psx[js, 64:128])
                            P.mm(psx[js, 128:192], rTb[sl, js], rw_Sb[hp][sl, :], start=True, stop=False)
                            P.mm(psx[js, 128:192], ArbT[js, js], Ub[js, :], start=False, stop=False)
                            P.mm(psx[js, 128:192], ArkT[js, js], vh, start=False, stop=True)
                            P.cp('dve', o_rw[js, h * 64:(h + 1) * 64], psx[js, 128:192])
                            P.mm(psx[sl, 192:256], btb[js, h * 64:(h + 1) * 64], Ub[js, :], start=True, stop=False)
                            P.mm(psx[sl, 192:256], ktb[js, h * 64:(h + 1) * 64], vh, start=False, stop=True)
                            P.tt('dve', rw_S[hp][sl, :], rw_S[hp][sl, :], psx[sl, 192:256], ALU.add)
                            P.ts('dve', rw_S[hp][sl, :], rw_S[hp][sl, :], WL[sl, hp * 2 + j:hp * 2 + j + 1], ALU.mult)
                            P.cp('pool', rw_Sb[hp][sl, :], rw_S[hp][sl, :])
                P.merge()
                P.begin('s2')
                if 'mlstm' not in skip:
                    cmf = Q2
                    for ci in range(4):
                        conv_silu(4 + ci, 20 + ci * 4, 36 + ci, ML0 + ci * 128, cmf[:, ci * 128:(ci + 1) * 128])
                    P.cp('pool', J0[:, 0:512], cmf[:, 0:512])
                    ps = PS[4]
                    P.tr(ps[:, 0:128], cmf[:, 256:384], ident)
                    P.tr(ps[:, 128:256], cmf[:, 384:512], ident)
                    ktm = Q3[:, 0:256]
                    P.cp('act', ktm, ps[:, 0:256])
                    ps = PS[5]
                    tm_mm(ps[:, 0:264], ML0 + 512, 264)
                    vaug = J1[:, 0:260].rearrange("p (h d) -> p h d", h=4)
                    P.cp('act', vaug[:, :, 0:64], h4(ps[:, 0:256]))
                    P.memset('pool', vaug[:, :, 64:65], 1.0)
                    ig = smallS[:, 48:52]
                    P.tt('dve', ig, ps[:, 256:260], tmf('ml_ib'), ALU.add)
                    lf = smallS[:, 52:56]
                    P.tt('dve', lf, ps[:, 260:264], tmf('ml_fb'), ALU.add)
                    P.act(lf, lf, AF.Exp, scale=-1.0)
                    P.act(lf, lf, AF.Ln, bias=1.0)
                    P.ts('dve', lf, lf, -1.0, ALU.mult)
                    ps = PS[0]
                    tm_mm(ps[:, 0:256], ML0 + 776, 256)
                    ogs = Q3[:, 256:512]
                    P.act(ogs, ps[:, 0:256], AF.Sigmoid)
                    ps = PS[0]
                    P.mm(ps[:, 0:4], TRI, lf)
                    P.mm(ps[:, 4:8], ones[:], lf)
                    bb = smallS[:, 56:60]
                    P.cp('dve', bb, ps[:, 0:4])
                    eb = smallS[:, 64:68]
                    P.act(eb, ps[:, 0:4], AF.Exp)
                    wst = smallS[:, 68:72]
                    P.tt('dve', wst, ps[:, 4:8], bb, ALU.subtract)
                    P.tt('dve', wst, wst, ig, ALU.add)
                    P.act(wst, wst, AF.Exp)
                    ebl4 = smallS[:, 72:76]
                    P.act(ebl4, ps[:, 4:8], AF.Exp)
                    pso = PS[1]
                    for h in range(4):
                        hp, hq = h // 2, (h % 2) * 64
                        qT_h = J0[hq:hq + 64, hp * 128:(hp + 1) * 128]
                        kT_h = J0[hq:hq + 64, 256 + hp * 128:256 + (hp + 1) * 128]
                        psl = (PS[4], PS[5])[h % 2]
                        lfb = Q4[:, 128:256]
                        P.cp('pool', lfb, lf[:, h:h + 1].to_broadcast([128, 128]))
                        P.mm(psl[:, 0:128], lfb, TRI)
                        dm = Q4[:, 256:384]
                        P.ts('dve', dm, psl[:, 0:128], bb[:, h:h + 1], ALU.subtract, 0.0, ALU.min)
                        P.act(dm, dm, AF.Exp, bias=ig[:, h:h + 1])
                        P.tt('pool', dm, dm, TRI, ALU.mult)
                        P.mm(psl[:, 128:256], kT_h, qT_h)
                        sT = J2[:, h * 128:(h + 1) * 128]
                        P.stt('dve', sT, psl[:, 128:256], 0.125, dm, ALU.mult, ALU.mult)
                        P.mm(pso[:, h * 65:(h + 1) * 65], sT, vaug[:, h, :])
                        P.mm(PS[0][:, h * 65:(h + 1) * 65], qT_h, ml_Sb[hp][hq:hq + 64, :])
                    numf = Q4[:, 512:772]
                    num = h4(numf)
                    P.tt('dve', num, h4(PS[0][:, 0:260]), b4(eb, 65), ALU.mult)
                    P.stt('dve', numf, numf, 0.125, pso[:, 0:260], ALU.mult, ALU.add)
                    den = smallS[:, 76:80]
                    den3 = den.rearrange("p (h o) -> p h o", o=1)
                    P.stt('dve', den3, num[:, :, 64:65], -1.0, num[:, :, 64:65], ALU.mult, ALU.max)
                    P.ts('dve', den, den, 1.0, ALU.max)
                    P.recip(den, den)
                    yv = Y[:, 768:1024]
                    P.tt('dve', h4(yv), num[:, :, 0:64], b4(den, 64), ALU.mult)
                    P.tt('dve', yv, yv, ogs, ALU.mult)
                    kw = J1[:, 260:516]
                    P.tt('dve', h4(kw), h4(ktm), b4(wst, 64), ALU.mult)
                    psn = PS[0]
                    for h in range(4):
                        hp, hq = h // 2, (h % 2) * 64
                        P.mm(psn[hq:hq + 64, 256 + hp * 65:256 + (hp + 1) * 65], kw[:, h * 64:(h + 1) * 64], vaug[:, h, :])
                    for hp in range(2):
                        for hh in range(2):
                            h = hp * 2 + hh
                            hq = hh * 64
                            P.ts('dve', ml_S[hp][hq:hq + 64, :], ml_S[hp][hq:hq + 64, :], ebl4[hq:hq + 64, h:h + 1], ALU.mult)
                        P.tt('dve', ml_S[hp][:], ml_S[hp][:], psn[:, 256 + hp * 65:256 + (hp + 1) * 65], ALU.add)
                        P.cp('pool', ml_Sb[hp][:], ml_S[hp][:])
                    groupnorm(yv, 4, tmf('ml_ng'), None, 1e-5, True, small2S, Q4[:, 768:1024])

                if 'rwkv' not in skip:
                    P.begin('ha')
                    rw_head(0, G[5], H[3], PS[2], PS[6])
                    P.begin('hb')
                    rw_head(2, G[4], H[4], PS[3], PS[7])
                P.merge()
                if 'rwkv' not in skip:
                    P.begin('ha')
                    rw_head(1, G[5], H[3], PS[2], PS[6])
                    P.begin('hb')
                    rw_head(3, G[4], H[4], PS[3], PS[7])
                P.merge()
                if 'rwkv' not in skip:
                    groupnorm(o_rw, 4, tmf('rw_lng'), tmf('rw_lnb'), 64e-5, True, small2, G[4][:, 768:1024])
                    t1 = G[4][:, 512:768]
                    P.tt('dve', h4(t1), h4(v_), b4(bon, 64), ALU.mult)
                    P.tt('dve', o_rw, o_rw, t1, ALU.add)
                    P.tt('dve', o_rw, o_rw, gg, ALU.mult)
                if debug and layer == 0:
                    for nm, c0 in [('d_ssd', 0), ('d_rwkv', 256), ('d_gla', 512), ('d_mlstm', 768)]:
                        P.dma(dbg[nm][c * 128:(c + 1) * 128, :], Y[:, c0:c0 + 256], comm=True)
                if stop_after == 'mixers':
                    continue

                YT = H[2].rearrange("p (k n) -> p k n", k=8)
                for k in range(8):
                    P.tr(PS[2 + k // 4][:, (k % 4) * 128:(k % 4 + 1) * 128], Y[:, k * 128:(k + 1) * 128], ident)
                for hf in range(2):
                    P.cp('act' if hf == 0 else 'dve', YT[:, hf * 4:(hf + 1) * 4, :],
                         PS[2 + hf][:, :].rearrange("p (k n) -> p k n", k=4))
                x1 = G[2]
                for hf in range(2):
                    ps = PS[4 + hf]
                    for k in range(8):
                        P.mm(ps[:, :], YT[:, k, :], WO[:, k, hf * 512:(hf + 1) * 512], start=(k == 0), stop=(k == 7))
                    P.stt('dve', x1[:, hf * 512:(hf + 1) * 512], xt[:, hf * 512:(hf + 1) * 512], ALPHA, ps[:, :], ALU.mult, ALU.add)
                layernorm(x1[:], x1[:], 'ln1_g', 'ln1_b', small2)
                P.dma(x1buf[c * 128:(c + 1) * 128, :], x1[:], comm=True)
                if debug and layer == 0:
                    P.dma(dbg['d_x1'][c * 128:(c + 1) * 128, :], x1[:], comm=True)
                x1b = H[0]
                P.cp('pool', x1b[:], x1[:])
                x1T = G[3].rearrange("p (k n) -> p k n", k=8)
                for k in range(8):
                    P.tr(PS[2 + k // 4][:, (k % 4) * 128:(k % 4 + 1) * 128], x1[:, k * 128:(k + 1) * 128], ident)
                for hf in range(2):
                    P.cp('act' if hf == 0 else 'dve', x1T[:, hf * 4:(hf + 1) * 4, :],
                         PS[2 + hf][:, :].rearrange("p (k n) -> p k n", k=4))
                ps = PS[6]
                for k in range(8):
                    P.mm(ps[:, 0:32], x1T[:, k, :], rtw[:, k, :], start=(k == 0), stop=(k == 7))
                lg = rsc[:, 0:32]
                P.tt('dve', lg, ps[:, 0:32], tmf('rt_b'), ALU.add)
                mx8 = rsc[:, 32:40]
                P.op('dve', lambda e, mx8=mx8, lg=lg: e.max(out=mx8, in_=lg), reads=K(lg), writes=K(mx8))
                P.op('dve', lambda e, mx8=mx8, lg=lg: e.max_index(out=idx8[:], in_max=mx8, in_values=lg),
                     reads=K(lg, mx8), writes=K(idx8))
                msk = rsc[:, 40:72]
                P.ts('dve', msk, lg, mx8[:, 3:4], ALU.is_ge)
                nmx = rsc[:, 72:73]
                P.ts('dve', nmx, mx8[:, 0:1], -1.0, ALU.mult)
                ex = rsc[:, 76:108]
                P.act(ex, lg, AF.Exp, bias=nmx)
                P.tt('dve', ex, ex, msk, ALU.mult)
                ssum = rsc[:, 73:74]
                P.rsum('dve', ssum, ex)
                P.recip(ssum, ssum)
                P.ts('dve', ex, ex, ssum, ALU.mult)
                ps = PS[5]
                P.mm(ps[:, 0:32], TRIS, msk)
                P.mm(ps[:, 32:64], ones[:], msk)
                pos = rsc[:, 108:140]
                P.tt('dve', pos, ps[:, 0:32], cntb[:], ALU.add)
                P.tt('dve', cntb[:], cntb[:], ps[:, 32:64], ALU.add)
                idxf = rsc[:, 140:144]
                P.cp('dve', idxf, idx8[:, 0:4])
                destf = rsc[:, 144:148]
                oh3 = Q3[:, 0:128].rearrange("p (k e) -> p k e", k=4)
                tmp3 = Q3[:, 128:256].rearrange("p (k e) -> p k e", k=4)
                bce = lambda a: a.rearrange("p (o e) -> p o e", o=1).to_broadcast([128, 4, NE])
                P.tt('dve', oh3, bce(IOTA32), b4(idxf, NE), ALU.is_equal)
                P.tt('dve', tmp3, oh3, bce(pos), ALU.mult)
                P.treduce('dve', destf, tmp3)
                P.tt('dve', tmp3, oh3, bce(ex), ALU.mult)
                P.treduce('dve', gate_all[:, c, :], tmp3)
                ovf = rsc[:, 148:152]
                P.ts('dve', ovf, destf, float(CAP), ALU.is_ge, float(NE * CAP), ALU.mult)
                P.stt('dve', destf, idxf, float(CAP), destf, ALU.mult, ALU.add)
                P.tt('dve', destf, destf, ovf, ALU.add)
                P.cp('dve', dest_all[:, c, :], destf)
                for k4 in range(4):
                    P.scatter(xg, dest_all[:, c, k4:k4 + 1], x1b[:], NE * CAP - 1)
                if debug and layer == 0:
                    P.dma(dbg['d_logits'][c * 128:(c + 1) * 128, :], lg, comm=True)
                    P.dma(dbg['d_dest'][c * 128:(c + 1) * 128, :], destf, comm=True)
                    P.dma(dbg['d_gate'][c * 128:(c + 1) * 128, :], gate_all[:, c, :], comm=True)
            if stop_after in ('mixers', 'phaseA'):
                continue

            EB = [R0, R1]

            def eb_views(i):
                gu = EB[i][:, 0:16384].rearrange("p (k n) -> p k n", k=8)
                dn = EB[i][:, 16384:24576].rearrange("p (k n) -> p k n", k=8)
                return gu, dn

            def load_expert(e):
                gu, dn = eb_views(e % 2)
                for k in range(8):
                    P.dmac(gu[:, k, :], w_gu[layer, e, k * 128:(k + 1) * 128, :])
                for k in range(8):
                    P.dmac(dn[:, k, :], w_dn[layer, e, k * 128:(k + 1) * 128, :])
                P.dma(bgu[e % 2][:], b_gu[layer, e])

            bdn = G[6]
            groups = [(e, grp) for e in range(NE) for grp in range(CAP // 256)]

            def views(gi):
                xgT = G[(gi % 2) * 2][:].bitcast(BF16).rearrange("p (k n) -> p k n", k=8)
                actT = G[(gi % 2) * 2 + 1][:].bitcast(BF16).rearrange("p (k n) -> p k n", k=8)
                return xgT, actT

            def prep(gi):
                e, grp = groups[gi]
                xgT, _ = views(gi)
                base = e * CAP + grp * 256
                for stl in range(2):
                    xr = H[stl]
                    P.dma(xr[:], xg[base + stl * 128:base + (stl + 1) * 128, :])
                    for k in range(8):
                        P.tr(PSB[:, k * 128:(k + 1) * 128], xr[:, k * 128:(k + 1) * 128], identb[:])
                    P.cp('act', xgT[:, :, stl * 128:(stl + 1) * 128],
                         PSB[:, :].rearrange("p (k n) -> p k n", k=8))

            def hphase(gi):
                e, grp = groups[gi]
                gu, dn = eb_views(e % 2)
                xgT, actT = views(gi)
                for fc in range(8):
                    psg = PS[(fc % 2) * 2]
                    psl = PS[(fc % 2) * 2 + 1]
                    for k in range(8):
                        P.mm(psg[:, 0:256], gu[:, k, fc * 128:(fc + 1) * 128], xgT[:, k, :], start=(k == 0), stop=(k == 7))
                    for k in range(8):
                        P.mm(psl[:, 0:256], gu[:, k, 1024 + fc * 128:1024 + (fc + 1) * 128], xgT[:, k, :], start=(k == 0), stop=(k == 7))
                    tgl = (G[4], Q4)[fc % 2]
                    tg = tgl[:, 0:256]
                    tl = tgl[:, 256:512]
                    bg = bgu[e % 2]
                    P.ts('dve', tg, psg[:, 0:256], bg[:, fc:fc + 1], ALU.add, 7.0, ALU.min)
                    P.ts('dve', tl, psl[:, 0:256], bg[:, 8 + fc:9 + fc], ALU.add, 7.0, ALU.min)
                    sg = Q2[:, (fc % 2) * 256:(fc % 2) * 256 + 256]
                    P.act(sg, tg, AF.Sigmoid, scale=1.702)
                    P.ts('dve', tl, tl, -7.0, ALU.max, 1.0, ALU.add)
                    P.tt('dve', tg, tg, sg, ALU.mult)
                    P.tt('dve', actT[:, fc, :], tl, tg, ALU.mult)

            def down(gi):
                e, grp = groups[gi]
                gu, dn = eb_views(e % 2)
                _, actT = views(gi)
                base = e * CAP + grp * 256
                if grp == 0:
                    P.dma(bdn[:], b_dn[layer, e].partition_broadcast(128))
                for stl in range(2):
                    yb = (G[5], Q3)[stl]
                    for hf in range(2):
                        ps = PS[4 + hf]
                        for k in range(8):
                            P.mm(ps[:, :], actT[:, k, stl * 128:(stl + 1) * 128], dn[:, k, hf * 512:(hf + 1) * 512],
                                 start=(k == 0), stop=(k == 7))
                        P.tt('dve', yb[:, hf * 512:(hf + 1) * 512], ps[:, :], bdn[:, hf * 512:(hf + 1) * 512], ALU.add)
                    P.dma(yg[base + stl * 128:base + (stl + 1) * 128, :], yb[:], comm=True)

            load_expert(0)
            prep(0)
            for gi in range(len(groups)):
                e, grp = groups[gi]
                if grp == 0 and e + 1 < NE:
                    load_expert(e + 1)
                hphase(gi)
                if gi + 1 < len(groups):
                    prep(gi + 1)
                down(gi)
            if stop_after == 'phaseB':
                continue

            load_r1(layer)
            for c in range(ntiles):
                x1 = G[2]
                P.dma(x1[:], x1buf[c * 128:(c + 1) * 128, :])
                hh = G[0]
                P.ts('dve', hh[:], x1[:], ALPHA, ALU.mult)
                for k4 in range(4):
                    gt = G[3 + k4]
                    P.memset('pool', gt[:], 0.0)
                    P.gather(gt[:], yg, dest_all[:, c, k4:k4 + 1], NE * CAP - 1)
                for k4 in range(4):
                    gt = G[3 + k4]
                    P.stt('dve', hh[:], gt[:], gate_all[:, c, k4:k4 + 1], hh[:], ALU.mult, ALU.add)
                if debug and layer == 0:
                    ff = Q4
                    P.stt('dve', ff[:], x1[:], -ALPHA, hh[:], ALU.mult, ALU.add)
                    P.dma(dbg['d_ffn'][c * 128:(c + 1) * 128, :], ff[:], comm=True)
                hT = H[2].rearrange("p (k n) -> p k n", k=8)
                for k in range(8):
                    P.tr(PS[k // 4][:, (k % 4) * 128:(k % 4 + 1) * 128], hh[:, k * 128:(k + 1) * 128], ident)
                for hf in range(2):
                    P.cp('act' if hf == 0 else 'dve', hT[:, hf * 4:(hf + 1) * 4, :],
                         PS[hf][:, :].rearrange("p (k n) -> p k n", k=4))
                pt = Q3[:, 0:256]
                P.dma(pt, p_in[layer, c * 128:(c + 1) * 128, :])
                P.tr(PS[2][:, 0:128], pt[:, 0:128], ident)
                P.tr(PS[2][:, 128:256], pt[:, 128:256], ident)
                pT = H[3][:, 0:256].rearrange("p (k n) -> p k n", k=2)
                P.cp('act', pT, PS[2][:, 0:256].rearrange("p (k n) -> p k n", k=2))
                sig = G[1]
                for hf in range(2):
                    ps = PS[3 + hf]
                    for k in range(8):
                        P.mm(ps[:, :], hT[:, k, :], PG[:, k, hf * 512:(hf + 1) * 512], start=(k == 0), stop=(k == 7))
                    P.act(sig[:, hf * 512:(hf + 1) * 512], ps[:, :], AF.Sigmoid)
                    ps2 = PS[5 + hf]
                    for k in range(2):
                        P.mm(ps2[:, :], pT[:, k, :], PP[:, k, hf * 512:(hf + 1) * 512], start=(k == 0), stop=(k == 1))
                    P.tt('dve', sig[:, hf * 512:(hf + 1) * 512], sig[:, hf * 512:(hf + 1) * 512], ps2[:, :], ALU.mult)
                P.tt('dve', hh[:], hh[:], sig[:], ALU.add)
                layernorm(hh[:], hh[:], 'ln2_g', 'ln2_b', small2, sq=Q4)
                P.dma(xdst[c * 128:(c + 1) * 128, :], hh[:], comm=True)
        print('total ops recorded', P.nops)
        P.emit()
    return nc


def _consts():
    s = np.arange(128)[:, None]
    t = np.arange(128)[None, :]
    blk = (s // 64) == (t // 64)
    c = np.zeros((128, 6, 128), np.float32)
    c[:, 0] = np.eye(128)
    c[:, 1] = (s <= t)
    c[:, 2] = (s < t)
    c[:, 3] = blk & (s <= t)
    c[:, 4] = blk & (s < t)
    c[:, 5] = blk & (t < s)
    c2 = np.zeros((128, 324), np.float32)
    c2[:, 0:256] = (np.arange(128)[:, None] // 32) == (np.arange(256)[None, :] // 64)
    c2[:, 256:288] = np.arange(32)[None, :]
    c2[:, 320:324] = (np.arange(128)[:, None] // 32) == np.arange(4)[None, :]
    return c, c2


def prep_inputs(inp):
    f = lambda k: np.asarray(inp[k], dtype=np.float32)
    L = DEPTH
    tm = np.zeros((L, 1, TM_W), np.float32)
    src = {
        'dt_bias': f('ssd_dt_bias'), 'a_log': f('ssd_a_log'), 'ssd_d': f('ssd_d'), 'ssd_ng': f('ssd_norm_g'),
        'rw_mu': f('rwkv_mu')[:, 0:768], 'rw_w0': f('rwkv_w0'), 'rw_a0': f('rwkv_a0'), 'rw_kk': f('rwkv_k_k'),
        'rw_ka': f('rwkv_k_a'), 'rw_rk': f('rwkv_r_k').reshape(L, 256), 'rw_lng': f('rwkv_ln_g'), 'rw_lnb': f('rwkv_ln_b'),
        'gl_gb': f('gla_gate_b'), 'gl_ng': f('gla_norm_g'),
        'ml_ib': f('mlstm_i_b'), 'ml_fb': f('mlstm_f_b'), 'ml_ng': f('mlstm_norm_g'),
        'ln1_g': f('ln1_g'), 'ln1_b': f('ln1_b'), 'ln2_g': f('ln2_g'), 'ln2_b': f('ln2_b'), 'rt_b': f('router_b'),
    }
    for nm, (o, w) in TM_OFF.items():
        tm[:, 0, o:o + w] = src[nm]
    cm = np.zeros((L, 128, CM_W), np.float32)
    scw = f('ssd_conv_w'); scb = f('ssd_conv_b'); mcw = f('mlstm_conv_w'); mcb = f('mlstm_conv_b')
    for ci in range(4):
        for j in range(4):
            cm[:, :, ci * 4 + j] = scw[:, j, ci * 128:(ci + 1) * 128]
            cm[:, :, 20 + ci * 4 + j] = mcw[:, j, ci * 128:(ci + 1) * 128]
        cm[:, :, 16 + ci] = scb[:, ci * 128:(ci + 1) * 128]
        cm[:, :, 36 + ci] = mcb[:, ci * 128:(ci + 1) * 128]
    cm[:, :, 40] = f('rwkv_mu')[:, 768:896]
    lora = np.concatenate([f('rwkv_w_up'), f('rwkv_a_up'), f('rwkv_g_up')], axis=1)
    bgu = np.ascontiguousarray(f('exp_b_gu').reshape(L, NE, 16, 128).transpose(0, 1, 3, 2))
    bdn = f('exp_b_down').reshape(L, NE, 1, D)
    c, c2 = _consts()
    shared = {
        'w_in': f('w_in'), 'w_out': f('w_out'), 'tmrow': tm, 'cmcol': cm, 'lora_up': np.ascontiguousarray(lora),
        'gla_up': f('gla_gate_up'), 'router_w': f('router_w'), 'w_gu': f('exp_w_gu'), 'b_gu': bgu,
        'w_dn': f('exp_w_down'), 'b_dn': bdn, 'ple_g': f('ple_gate_w'), 'ple_p': f('ple_proj'),
        'consts': c, 'consts2': c2,
    }
    return shared


_NC_CACHE = {}


def kernel(**inputs):
    shared = prep_inputs(inputs)
    x = np.asarray(inputs['x'], dtype=np.float32)
    p = np.asarray(inputs['p'], dtype=np.float32)
    if 'nc' not in _NC_CACHE:
        _NC_CACHE['nc'] = build()
    nc = _NC_CACHE['nc']
    in_maps = []
    for core in range(8):
        b = core % 4
        m = dict(shared)
        m['x'] = np.ascontiguousarray(x[b])
        m['p'] = np.ascontiguousarray(p[:, b])
        in_maps.append(m)
    res = run_bass_kernel_spmd(nc, in_maps, core_ids=list(range(8)))
    outs = [res.results[b]['out'] for b in range(4)]
    return np.stack(outs, axis=0).astype(np.float32)
```

```python
import math
from contextlib import ExitStack
import numpy as np
import concourse.bass as bass
import concourse.mybir as mybir
from concourse.bass_utils import run_bass_kernel_spmd

F32 = mybir.dt.float32
BF16 = mybir.dt.bfloat16
I32 = mybir.dt.int32
U32 = mybir.dt.uint32
AF = mybir.ActivationFunctionType
ALU = mybir.AluOpType
AX = mybir.AxisListType

D = 1024
T = 4096
NT = T // 128
DEPTH = 4
NCOL = 3484
NE = 32
CAP = 768
ALPHA = (2 * DEPTH) ** 0.25
LN_EPS = 1e-5
SSD0, RW0, GL0, ML0 = 0, 772, 1668, 2452


def _isap(a):
    return hasattr(a, 'tensor')


def K(*aps):
    return list({a.tensor.name for a in aps if _isap(a)})


class Prog:
    ENGS = ['pe', 'act', 'dve', 'pool', 'sp']
    NROT = 4
    CH = 1500
    CHC = 4000

    def __init__(self, nc):
        self.nc = nc
        self.ops = {e: [] for e in self.ENGS}
        self.cnt = {}
        self.last_w = {}
        self.readers = {}
        self.waited = {e: {} for e in self.ENGS}
        self.ndma = {e: 0 for e in self.ENGS}
        self.sems = {}
        self.cwr = {}
        self.pe_bank = {}
        self._bound_reg = None
        self._cur = None
        self._streams = {}
        self.nops = 0
        self.maxops = None

    def op(self, eng, fn, reads=(), writes=(), dma=False, cwrites=(), pebank=None):
        if self._cur is not None:
            self._streams[self._cur].append((eng, fn, reads, writes, dma, cwrites, pebank))
            return
        self.nops += 1
        if self.maxops is not None and self.nops > self.maxops:
            return
        selfwait = []
        if pebank is not None:
            bank, r0, r1 = pebank
            last = self.pe_bank.get(bank)
            if last is not None:
                (l0, l1), lidx = last
                if r1 <= l0 or l1 <= r0:
                    selfwait = [lidx]
            self.pe_bank[bank] = ((r0, r1), self.cnt.get('pe', 0))
        pr = [r for r in reads if r.startswith('ps')]
        if pr:
            reads = [r for r in reads if not r.startswith('ps')]
            writes = list(writes) + [r for r in pr if r not in writes]
        if dma:
            j = self.ndma[eng]
            self.ndma[eng] += 1
            counter = 'dma_%s_%d' % (eng, j % self.NROT)
        else:
            counter = eng
        idx = self.cnt.get(counter, 0)
        self.cnt[counter] = idx + 1
        deps = {}

        def need(c, j):
            if deps.get(c, -1) < j:
                deps[c] = j
        for r in reads:
            if r in self.last_w:
                need(*self.last_w[r])
            for c, j in self.cwr.get(r, {}).items():
                need(c, j)
        for w in writes:
            if w in self.last_w:
                need(*self.last_w[w])
            for c, j in self.readers.get(w, {}).items():
                need(c, j)
            for c, j in self.cwr.get(w, {}).items():
                need(c, j)
        for w in cwrites:
            if w in self.last_w:
                need(*self.last_w[w])
            for c, j in self.readers.get(w, {}).items():
                need(c, j)
        waits = []
        for j in selfwait:
            if self.waited[eng].get(counter, -1) < j:
                self.waited[eng][counter] = j
                waits.append((counter, j))
        for c, j in deps.items():
            if c == counter and eng == 'pe' and not dma:
                continue
            if self.waited[eng].get(c, -1) >= j:
                continue
            self.waited[eng][c] = j
            waits.append((c, j))
        self.ops[eng].append((waits, fn, counter, idx))
        for w in writes:
            self.last_w[w] = (counter, idx)
            self.readers[w] = {}
            self.cwr[w] = {}
        for w in cwrites:
            d = self.cwr.setdefault(w, {})
            if d.get(counter, -1) < idx:
                d[counter] = idx
        for r in reads:
            if r in writes:
                continue
            d = self.readers.setdefault(r, {})
            if d.get(counter, -1) < idx:
                d[counter] = idx


    def begin(self, name):
        self._cur = name
        self._streams.setdefault(name, [])

    def merge(self):
        streams = {k: v for k, v in self._streams.items() if v}
        self._cur = None
        self._streams = {}
        pos = {k: 0 for k in streams}
        total = sum(len(v) for v in streams.values())
        for _ in range(total):
            k = min((k for k in streams if pos[k] < len(streams[k])), key=lambda k: pos[k] / len(streams[k]))
            eng, fn, reads, writes, dma, cwrites, pebank = streams[k][pos[k]]
            pos[k] += 1
            self.op(eng, fn, reads=reads, writes=writes, dma=dma, cwrites=cwrites, pebank=pebank)

    @staticmethod
    def _prange(ap):
        st, n = ap.ap[0]
        p0 = (ap.offset // st) if st else 0
        return (p0, p0 + n)

    def mm(self, out, lhsT, rhs, start=True, stop=True):
        r0, r1 = self._prange(lhsT)
        self.op('pe', lambda e: e.matmul(out, lhsT=lhsT, rhs=rhs, start=start, stop=stop),
                reads=K(lhsT, rhs), writes=K(out), pebank=(out.tensor.name, r0, r1))

    def tr(self, out, in_, ident):
        r0, r1 = self._prange(in_)
        self.op('pe', lambda e: e.transpose(out, in_, ident), reads=K(in_, ident), writes=K(out),
                pebank=(out.tensor.name, r0, r1))

    def act(self, out, in_, func, bias=None, scale=None, accum_out=None, eng='act'):
        kw = {}
        if bias is not None:
            kw['bias'] = bias
        if scale is not None:
            kw['scale'] = scale
        if accum_out is not None:
            kw['accum_out'] = accum_out
        self.op('act', lambda e: e.activation(out=out, in_=in_, func=func, **kw),
                reads=K(in_, bias, scale), writes=K(out, accum_out))

    def cp(self, eng, out, in_):
        if eng == 'act':
            self.op('act', lambda e: e.copy(out=out, in_=in_), reads=K(in_), writes=K(out))
        else:
            self.op(eng, lambda e: e.tensor_copy(out=out, in_=in_), reads=K(in_), writes=K(out))

    def tt(self, eng, out, in0, in1, op):
        self.op(eng, lambda e: e.tensor_tensor(out=out, in0=in0, in1=in1, op=op),
                reads=K(in0, in1), writes=K(out))

    def ts(self, eng, out, in0, s1, op0, s2=None, op1=None, accum_out=None):
        kw = {}
        if op1 is not None:
            kw['op1'] = op1
        if accum_out is not None:
            kw['accum_out'] = accum_out
        self.op(eng, lambda e: e.tensor_scalar(out=out, in0=in0, scalar1=s1, scalar2=s2, op0=op0, **kw),
                reads=K(in0, s1, s2), writes=K(out, accum_out))

    def stt(self, eng, out, in0, scalar, in1, op0, op1):
        self.op(eng, lambda e: e.scalar_tensor_tensor(out=out, in0=in0, scalar=scalar, in1=in1, op0=op0, op1=op1),
                reads=K(in0, scalar, in1), writes=K(out))

    def rsum(self, eng, out, in_):
        self.op(eng, lambda e: e.reduce_sum(out=out, in_=in_, axis=AX.X), reads=K(in_), writes=K(out))

    def memset(self, eng, out, val):
        self.op(eng, lambda e: e.memset(out, val), writes=K(out))

    def dma(self, out, in_, eng='sp', comm=False):
        if comm:
            self.op(eng, lambda e: e.dma_start(out=out, in_=in_), reads=K(in_), cwrites=K(out), dma=True)
        else:
            self.op(eng, lambda e: e.dma_start(out=out, in_=in_), reads=K(in_), writes=K(out), dma=True)


    def dmac(self, out, in_):
        self.op('pool', lambda e: e.dma_start(out=out, in_=in_), reads=K(in_), cwrites=K(out), dma=True)

    def _breg(self, e, bound):
        if self._bound_reg is None:
            self._bound_reg = (bound, e.to_reg(bound))
        assert self._bound_reg[0] == bound
        return self._bound_reg[1]

    def scatter(self, out_dram, idx_ap, in_sb, bound):
        self.op('pool', lambda e: e.indirect_dma_start(
            out=out_dram, out_offset=bass.IndirectOffsetOnAxis(ap=idx_ap, axis=0),
            in_=in_sb, in_offset=None, bounds_check=self._breg(e, bound), oob_is_err=False),
            reads=K(in_sb, idx_ap), cwrites=K(out_dram), dma=True)

    def gather(self, out_sb, in_dram, idx_ap, bound):
        self.op('pool', lambda e: e.indirect_dma_start(
            out=out_sb, out_offset=None, in_=in_dram,
            in_offset=bass.IndirectOffsetOnAxis(ap=idx_ap, axis=0), bounds_check=self._breg(e, bound), oob_is_err=False),
            reads=K(in_dram, idx_ap), writes=K(out_sb), dma=True)

    def treduce(self, eng, out, in_, op=None):
        op = ALU.add if op is None else op
        self.op(eng, lambda e: e.tensor_reduce(out=out, in_=in_, axis=AX.X, op=op), reads=K(in_), writes=K(out))

    def recip(self, out, in_):
        self.op('dve', lambda e: e.reciprocal(out=out, in_=in_), reads=K(in_), writes=K(out))

    def _ch(self, counter):
        return self.CH if counter.startswith('dma_') else self.CHC

    def _sem(self, counter, idx):
        return self.sems[(counter, idx // self._ch(counter))]

    def _val(self, counter, idx):
        inc = 16 if counter.startswith('dma_') else 1
        return (idx % self._ch(counter) + 1) * inc

    def emit(self):
        nc = self.nc
        with ExitStack() as st:
            for counter, n in self.cnt.items():
                ch = self._ch(counter)
                for k in range((n + ch - 1) // ch):
                    self.sems[(counter, k)] = st.enter_context(nc.semaphore('s_%s_%d' % (counter, k)))
            block = st.enter_context(nc.Block())

            def mk(engname):
                def body(e):
                    for waits, fn, counter, idx in self.ops[engname]:
                        for c, j in waits:
                            e.wait_ge(self._sem(c, j), self._val(c, j))
                        ins = fn(e)
                        ins.then_inc(self._sem(counter, idx), 16 if counter.startswith('dma_') else 1)
                    if engname == 'sp':
                        for c, n in self.cnt.items():
                            if n > 0:
                                e.wait_ge(self._sem(c, n - 1), self._val(c, n - 1))
                return body
            block.tensor(mk('pe'))
            block.scalar(mk('act'))
            block.vector(mk('dve'))
            block.gpsimd(mk('pool'))
            block.sync(mk('sp'))


TM_FIELDS = [
    ('dt_bias', 4), ('a_log', 4), ('ssd_d', 4), ('ssd_ng', 256),
    ('rw_mu', 768), ('rw_w0', 256), ('rw_a0', 256), ('rw_kk', 256), ('rw_ka', 256), ('rw_rk', 256),
    ('rw_lng', 256), ('rw_lnb', 256),
    ('gl_gb', 128), ('gl_ng', 256),
    ('ml_ib', 4), ('ml_fb', 4), ('ml_ng', 256),
    ('ln1_g', 1024), ('ln1_b', 1024), ('ln2_g', 1024), ('ln2_b', 1024), ('rt_b', 32),
]
TM_OFF = {}
_o = 0
for _n, _w in TM_FIELDS:
    TM_OFF[_n] = (_o, _w)
    _o += _w
TM_W = _o
CM_W = 41


def h4(ap, h=4):
    return ap.rearrange("p (h d) -> p h d", h=h)


def b4(ap, w, h=4):
    return ap.rearrange("p (h o) -> p h o", o=1).to_broadcast([128, h, w])


import os
CONVPS = int(os.environ.get("CONVPS", "0"))


def build(n_layers=DEPTH, debug=False, stop_after=None, ntiles=NT, skip=(), maxops=None):
    nc = bass.Bass("TRN2", target_bir_lowering=False)
    P = Prog(nc)
    P.maxops = maxops
    LD = n_layers
    NED = 1 if stop_after in ('mixers', 'phaseA') else NE

    def din(name, shape, dt=F32):
        return nc.dram_tensor(name, list(shape), dt, kind="ExternalInput").ap()
    x_in = din('x', [T, D])
    p_in = din('p', [LD, T, 256])
    w_in = din('w_in', [LD, D, NCOL])
    w_out = din('w_out', [LD, D, D])
    tmrow = din('tmrow', [LD, 1, TM_W])
    cmcol = din('cmcol', [LD, 128, CM_W])
    lora_up = din('lora_up', [LD, 128, 256])
    gla_up = din('gla_up', [LD, 16, 128])
    router_w = din('router_w', [LD, D, NE])
    w_gu = din('w_gu', [LD, NED, D, 2 * D])
    b_gu = din('b_gu', [LD, NE, 128, 16])
    w_dn = din('w_dn', [LD, NED, D, D])
    b_dn = din('b_dn', [LD, NE, 1, D])
    ple_g = din('ple_g', [LD, D, D])
    ple_p = din('ple_p', [LD, 256, D])
    consts = din('consts', [128, 6, 128])
    consts2 = din('consts2', [128, 324])
    out = nc.dram_tensor('out', [T, D], F32, kind="ExternalOutput").ap()
    xbuf = nc.dram_tensor('xbuf', [T, D], F32).ap()
    x1buf = nc.dram_tensor('x1buf', [T, D], F32).ap()
    xg = nc.dram_tensor('xg', [NE * CAP, D], BF16).ap()
    yg = nc.dram_tensor('yg', [NE * CAP, D], F32).ap()
    dbg = {}
    if debug:
        for nm, w in [('d_ssd', 256), ('d_rwkv', 256), ('d_gla', 256), ('d_mlstm', 256), ('d_x1', 1024),
                      ('d_ffn', 1024), ('d_logits', 32), ('d_dest', 4), ('d_gate', 4)]:
            dbg[nm] = nc.dram_tensor(nm, [T, w], F32, kind="ExternalOutput").ap()

    with ExitStack() as st:
        def sb(name, shape, dt=F32):
            return st.enter_context(nc.sbuf_tensor(name, list(shape), dt))

        def psb(name, shape, dt=F32):
            return st.enter_context(nc.psum_tensor(name, list(shape), dt))

        R0 = sb('R0', [128, 8 * NCOL], BF16)
        R1 = sb('R1', [128, 24576], BF16)
        tmc = sb('tmc', [128, TM_W])
        cmc = sb('cmc', [128, CM_W])
        cst = sb('cst', [128, 6, 128])
        cst2 = sb('cst2', [128, 324])
        identb = sb('identb', [128, 128], BF16)
        lup = sb('lup', [128, 256])
        gup = sb('gup', [16, 128])
        rtw = sb('rtw', [128, 8, NE])
        ones = sb('ones', [128, 128])
        G = [sb('G%d' % i, [128, 1024]) for i in range(7)]
        H = [sb('H%d' % i, [128, 1024], BF16) for i in range(5)]
        xTe = [sb('xTe%d' % i, [128, 8, 129], BF16) for i in range(2)]
        cbuf = [sb('cbuf%d' % i, [128, 131]) for i in range(8)]
        small = sb('small', [128, 128])
        small2 = sb('small2', [128, 32])
        smallS = sb('smallS', [128, 80])
        small2S = sb('small2S', [128, 16])
        Q2 = sb('Q2', [128, 512]); Q3 = sb('Q3', [128, 1024]); Q4 = sb('Q4', [128, 1024])
        J0 = sb('J0', [128, 512], BF16); J1 = sb('J1', [128, 640], BF16); J2 = sb('J2', [128, 512], BF16)
        ssd_S = sb('ssd_S', [128, 256]); ssd_Sb = sb('ssd_Sb', [128, 256], BF16)
        rw_S = [sb('rw_S%d' % i, [128, 64]) for i in range(2)]
        rw_Sb = [sb('rw_Sb%d' % i, [128, 64], BF16) for i in range(2)]
        gl_S = sb('gl_S', [128, 256]); gl_Sb = sb('gl_Sb', [128, 256], BF16)
        ml_S = [sb('ml_S%d' % i, [128, 65]) for i in range(2)]
        ml_Sb = [sb('ml_Sb%d' % i, [128, 65], BF16) for i in range(2)]
        dest_all = sb('dest_all', [128, NT, 4], I32)
        gate_all = sb('gate_all', [128, NT, 4])
        cntb = sb('cntb', [128, NE])
        rsc = sb('rsc', [128, 256])
        idx8 = sb('idx8', [128, 8], U32)
        bgu = [sb('bgu%d' % i, [128, 16]) for i in range(2)]
        PS = [psb('ps%d' % i, [128, 512]) for i in range(8)]
        PSB = PS[7][:].bitcast(BF16)

        ident = cst[:, 0, :]
        TRI = cst[:, 1, :]
        TRIS = cst[:, 2, :]
        BTRI = cst[:, 3, :]
        BTRIS = cst[:, 4, :]
        BLOW = cst[:, 5, :]
        GLMASK = cst2[:, 0:256]
        IOTA32 = cst2[:, 256:288]

        def tmf(name):
            o, w = TM_OFF[name]
            return tmc[:, o:o + w]

        P.dma(cst[:], consts)
        P.dma(cst2[:], consts2)
        P.cp('dve', identb[:], cst[:, 0, :])
        P.memset('dve', ones[:], 1.0)

        W3 = R0[:, 0:8 * NCOL].rearrange("p (k n) -> p k n", k=8)
        WO = R1[:, 0:8192].rearrange("p (k n) -> p k n", k=8)
        PG = R1[:, 8192:16384].rearrange("p (k n) -> p k n", k=8)
        PP = R1[:, 16384:18432].rearrange("p (k n) -> p k n", k=2)

        def layernorm(dst, src, gname, bname, sc, sq=None):
            sq = G[6] if sq is None else sq
            P.rsum('dve', sc[:, 0:1], src)
            P.ts('dve', sc[:, 1:2], sc[:, 0:1], -1.0 / D, ALU.mult)
            P.act(sq[:], src, AF.Square, bias=sc[:, 1:2], accum_out=sc[:, 2:3])
            P.act(sc[:, 3:4], sc[:, 2:3], AF.Ln, bias=LN_EPS, scale=1.0 / D)
            P.act(sc[:, 3:4], sc[:, 3:4], AF.Exp, scale=-0.5)
            P.ts('dve', dst, src, sc[:, 1:2], ALU.add, sc[:, 3:4], ALU.mult)
            P.tt('dve', dst, dst, tmf(gname), ALU.mult)
            P.tt('pool', dst, dst, tmf(bname), ALU.add)

        def groupnorm(y, nh, gain, bias, eps, center, sc, tmp):
            hd = 256 // nh
            y3 = h4(y, nh)
            t3 = h4(tmp, nh)
            if center:
                P.treduce('dve', sc[:, 0:nh], y3)
                P.ts('dve', sc[:, 0:nh], sc[:, 0:nh], -1.0 / hd, ALU.mult)
                P.tt('dve', y3, y3, b4(sc[:, 0:nh], hd, nh), ALU.add)
            P.tt('pool', tmp, y, y, ALU.mult)
            P.treduce('dve', sc[:, 4:4 + nh], t3)
            P.act(sc[:, 4:4 + nh], sc[:, 4:4 + nh], AF.Ln, bias=eps, scale=1.0 / hd)
            P.act(sc[:, 4:4 + nh], sc[:, 4:4 + nh], AF.Exp, scale=-0.5)
            P.tt('dve', y3, y3, b4(sc[:, 4:4 + nh], hd, nh), ALU.mult)
            P.tt('dve', y, y, gain, ALU.mult)
            if bias is not None:
                P.tt('dve', y, y, bias, ALU.add)

        def load_r1(layer):
            for k in range(8):
                P.dmac(WO[:, k, :], w_out[layer, k * 128:(k + 1) * 128, :])
            for k in range(8):
                P.dmac(PG[:, k, :], ple_g[layer, k * 128:(k + 1) * 128, :])
            for k in range(2):
                P.dmac(PP[:, k, :], ple_p[layer, k * 128:(k + 1) * 128, :])

        win_prefetched = False
        for layer in range(n_layers):
            xsrc = x_in if layer == 0 else xbuf
            xdst = out if layer == n_layers - 1 else xbuf
            if not win_prefetched:
                for k in range(8):
                    P.dmac(W3[:, k, :], w_in[layer, k * 128:(k + 1) * 128, :])
            win_prefetched = False
            load_r1(layer)
            P.dma(tmc[:], tmrow[layer].partition_broadcast(128))
            P.dma(cmc[:], cmcol[layer])
            P.dma(lup[:], lora_up[layer])
            P.dma(gup[:], gla_up[layer])
            P.dma(rtw[:], router_w[layer].rearrange("(k p) e -> p k e", p=128))
            P.act(tmf('a_log'), tmf('a_log'), AF.Exp)
            P.ts('dve', tmf('a_log'), tmf('a_log'), -1.0, ALU.mult)
            for s_ in [ssd_S, gl_S] + rw_S + ml_S + [cntb]:
                P.memset('dve', s_[:], 0.0)
            for s_ in [ssd_Sb, gl_Sb] + rw_Sb + ml_Sb:
                P.memset('pool', s_[:], 0.0)
            for cb in cbuf:
                P.memset('pool', cb[:, 0:3], 0.0)
            P.memset('dve', xTe[0][:, :, 0:1], 0.0)

            for c in range(ntiles):
                xt = G[0]
                xe = xTe[c % 2]
                xn = xTe[(c + 1) % 2]
                P.dma(xt[:], xsrc[c * 128:(c + 1) * 128, :])
                for k in range(8):
                    P.tr(PS[k // 4][:, (k % 4) * 128:(k % 4 + 1) * 128], xt[:, k * 128:(k + 1) * 128], ident)
                for hf in range(2):
                    P.cp('act' if hf == 0 else 'dve', xe[:, hf * 4:(hf + 1) * 4, 1:129],
                         PS[hf][:, :].rearrange("p (k n) -> p k n", k=4))
                P.cp('pool', xn[:, :, 0:1], xe[:, :, 128:129])

                def cm_mm(ps_ap, c0, ncols, shifted=False):
                    for k in range(8):
                        rhs = xe[:, k, 0:128] if shifted else xe[:, k, 1:129]
                        P.mm(ps_ap, W3[:, k, c0:c0 + ncols], rhs, start=(k == 0), stop=(k == 7))

                def tm_mm(ps_ap, c0, ncols, shifted=False):
                    for k in range(8):
                        lhsT = xe[:, k, 0:128] if shifted else xe[:, k, 1:129]
                        P.mm(ps_ap, lhsT, W3[:, k, c0:c0 + ncols], start=(k == 0), stop=(k == 7))

                def conv_silu(ci, wcol, bcol, c0, dst):
                    cb = cbuf[ci]
                    ps = (PS[0], PS[1])[ci % 2]
                    cm_mm(ps[:, 0:128], c0, 128)
                    P.cp('act', cb[:, 3:131], ps[:, 0:128])
                    tmp = Q4[:, 384:512]
                    P.ts('dve', tmp, cb[:, 0:128], cmc[:, wcol:wcol + 1], ALU.mult, cmc[:, bcol:bcol + 1], ALU.add)
                    for j in range(1, 4):
                        P.stt('dve', tmp, cb[:, j:j + 128], cmc[:, wcol + j:wcol + j + 1], tmp, ALU.mult, ALU.add)
                    P.act(dst, tmp, AF.Silu)
                    P.cp('pool', cb[:, 0:3], cb[:, 128:131])

                Y = G[1]

                P.begin('s2')
                if 'ssd' not in skip:
                    cmf = Q2
                    for ci in range(4):
                        conv_silu(ci, ci * 4, 16 + ci, SSD0 + 256 + ci * 128, cmf[:, ci * 128:(ci + 1) * 128])
                    P.cp('pool', J0[:, 0:512], cmf[:, 0:512])
                    BT = J0[:, 256:384]
                    CT = J0[:, 384:512]
                    ps = PS[4]
                    for j in range(3):
                        P.tr(ps[:, j * 128:(j + 1) * 128], cmf[:, j * 128:(j + 1) * 128], ident)
                    xs = Q3[:, 0:256]
                    P.cp('act', xs, ps[:, 0:256])
                    Btm = J1[:, 0:128]
                    P.cp('dve', Btm, ps[:, 256:384])
                    ps = PS[5]
                    tm_mm(ps[:, 0:256], SSD0 + 0, 256)
                    tm_mm(ps[:, 256:260], SSD0 + 768, 4)
                    zs = Q3[:, 256:512]
                    P.act(zs, ps[:, 0:256], AF.Silu)
                    sc = smallS
                    dt = sc[:, 0:4]
                    P.tt('dve', dt, ps[:, 256:260], tmf('dt_bias'), ALU.add)
                    P.act(dt, dt, AF.Exp)
                    P.act(dt, dt, AF.Ln, bias=1.0)
                    adt = sc[:, 4:8]
                    P.tt('dve', adt, dt, tmf('a_log'), ALU.mult)
                    ps = PS[0]
                    P.mm(ps[:, 0:4], TRI, adt)
                    P.mm(ps[:, 4:8], ones[:], adt)
                    acum = sc[:, 8:12]
                    P.cp('dve', acum, ps[:, 0:4])
                    ea = sc[:, 12:16]
                    P.act(ea, ps[:, 0:4], AF.Exp)
                    dsx = sc[:, 16:20]
                    P.tt('dve', dsx, ps[:, 4:8], acum, ALU.subtract)
                    P.act(dsx, dsx, AF.Exp)
                    eal = sc[:, 20:24]
                    P.act(eal, ps[:, 4:8], AF.Exp)
                    xdt = J1[:, 128:384]
                    xdt2 = J1[:, 384:640]
                    xdtf = Q3[:, 512:768]
                    P.tt('dve', h4(xdtf), h4(xs), b4(dt, 64), ALU.mult)
                    P.cp('pool', xdt, xdtf)
                    P.tt('dve', h4(xdt2), h4(xdtf), b4(dsx, 64), ALU.mult)
                    ps = PS[0]
                    P.mm(ps[:, 0:128], BT, CT)
                    GTm = Q4[:, 0:128]
                    P.tt('dve', GTm, ps[:, 0:128], TRI, ALU.mult)
                    psy = PS[1]
                    for h in range(4):
                        psl = (PS[4], PS[5])[h % 2]
                        adt_bc = Q4[:, 128:256]
                        P.cp('pool', adt_bc, adt[:, h:h + 1].to_broadcast([128, 128]))
                        P.mm(psl[:, 0:128], adt_bc, TRI)
                        lt = Q4[:, 256:384]
                        P.ts('dve', lt, psl[:, 0:128], acum[:, h:h + 1], ALU.subtract, 0.0, ALU.min)
                        P.act(lt, lt, AF.Exp)
                        MT = J2[:, h * 128:(h + 1) * 128]
                        P.tt('dve', MT, lt, GTm, ALU.mult)
                        P.mm(psy[:, h * 64:(h + 1) * 64], MT, xdt[:, h * 64:(h + 1) * 64])
                    pso = PS[0]
                    P.mm(pso[:, 256:512], CT, ssd_Sb[:])
                    yv = Y[:, 0:256]
                    P.tt('dve', h4(yv), h4(pso[:, 256:512]), b4(ea, 64), ALU.mult)
                    P.tt('dve', yv, yv, psy[:, 0:256], ALU.add)
                    psn = PS[0]
                    P.mm(psn[:, 128:384], Btm, xdt2)
                    P.tt('dve', h4(ssd_S[:]), h4(ssd_S[:]), b4(eal, 64), ALU.mult)
                    P.tt('dve', ssd_S[:], ssd_S[:], psn[:, 128:384], ALU.add)
                    P.cp('pool', ssd_Sb[:], ssd_S[:])
                    t1 = Q4[:, 512:768]
                    P.tt('pool', h4(t1), h4(xs), b4(tmf('ssd_d'), 64), ALU.mult)
                    P.tt('dve', yv, yv, t1, ALU.add)
                    P.tt('dve', yv, yv, zs, ALU.mult)
                    groupnorm(yv, 1, tmf('ssd_ng'), None, 1e-5, False, small2S, Q4[:, 768:1024])

                if 'gla' not in skip:
                    ps = PS[5]
                    tm_mm(ps[:, 0:512], GL0, 512)
                    qkv = Q2
                    P.cp('act', qkv[:, 0:512], ps[:, 0:512])
                    vb = J1[:, 0:256]
                    P.cp('pool', vb, qkv[:, 256:512])
                    ps = PS[0]
                    tm_mm(ps[:, 0:256], GL0 + 528, 256)
                    og = Q3[:, 0:256]
                    P.act(og, ps[:, 0:256], AF.Silu)
                    ps = PS[0]
                    cm_mm(ps[0:16, 0:128], GL0 + 512, 16)
                    gdT = Q3[0:16, 256:384]
                    P.cp('act', gdT, ps[0:16, 0:128])
                    ps = PS[1]
                    P.mm(ps[:, 0:128], gdT, gup[:])
                    la = Q3[:, 384:512]
                    P.tt('dve', la, ps[:, 0:128], tmf('gl_gb'), ALU.add)
                    P.act(la, la, AF.Exp, scale=-1.0)
                    P.act(la, la, AF.Ln, bias=1.0)
                    P.ts('dve', la, la, -1.0 / 16.0, ALU.mult)
                    ps = PS[4]
                    P.mm(ps[:, 0:128], TRI, la)
                    P.mm(ps[:, 128:256], ones[:], la)
                    P.mm(ps[:, 256:257], la, ones[:, 0:1])
                    ebc = Q3[:, 512:640]
                    P.act(ebc, ps[:, 0:128], AF.Exp)
                    enb = Q3[:, 640:768]
                    P.act(enb, ps[:, 0:128], AF.Exp, scale=-1.0)
                    kd = Q3[:, 768:896]
                    bc_sb = Q3[:, 896:1024]
                    P.cp('dve', bc_sb, ps[:, 0:128])
                    P.tt('dve', kd, ps[:, 128:256], bc_sb, ALU.subtract)
                    P.act(kd, kd, AF.Exp)
                    ebl = smallS[:, 32:33]
                    P.act(ebl, ps[:, 256:257], AF.Exp)
                    qd = Q4[:, 0:128]
                    P.stt('dve', qd, qkv[:, 0:128], 32 ** -0.5, ebc, ALU.mult, ALU.mult)
                    ki = Q4[:, 128:256]
                    P.tt('dve', ki, qkv[:, 128:256], enb, ALU.mult)
                    kdb = J1[:, 256:384]
                    P.tt('dve', kdb, qkv[:, 128:256], kd, ALU.mult)
                    ps = PS[0]
                    P.tr(ps[:, 0:128], qd, ident)
                    P.tr(ps[:, 128:256], ki, ident)
                    qdT = J1[:, 384:512]
                    P.cp('act', qdT, ps[:, 0:128])
                    pso = PS[1]
                    P.mm(pso[:, 0:256], qdT, gl_Sb[:], start=True, stop=False)
                    for h in range(4):
                        kim = J1[:, 512:640]
                        P.ts('dve', kim, ps[:, 128:256], cst2[:, 320 + h:321 + h], ALU.mult)
                        psa = (PS[4], PS[5])[h % 2]
                        P.mm(psa[:, 384:512], kim, qdT)
                        at = J2[:, h * 128:(h + 1) * 128]
                        P.tt('dve', at, psa[:, 384:512], TRI, ALU.mult)
                        P.mm(pso[:, h * 64:(h + 1) * 64], at, vb[:, h * 64:(h + 1) * 64], start=False, stop=(h == 3))
                    yv = Y[:, 512:768]
                    P.cp('act', yv, pso[:, 0:256])
                    psn = PS[0]
                    P.mm(psn[:, 256:512], kdb, vb)
                    P.ts('dve', gl_S[:], gl_S[:], ebl, ALU.mult)
                    t1 = Q4[:, 256:512]
                    P.tt('dve', t1, psn[:, 256:512], GLMASK, ALU.mult)
                    P.tt('dve', gl_S[:], gl_S[:], t1, ALU.add)
                    P.cp('pool', gl_Sb[:], gl_S[:])
                    groupnorm(yv, 4, tmf('gl_ng'), None, 1e-5, False, small2S, Q4[:, 768:1024])
                    P.tt('dve', yv, yv, og, ALU.mult)

                P.begin('s1')
                if 'rwkv' not in skip:
                    rkv = G[2]
                    for half in range(2):
                        c0 = RW0 + half * 384
                        tm_mm(PS[2][:, 0:384], c0, 384)
                        tm_mm(PS[3][:, 0:384], c0, 384, shifted=True)
                        cur = G[4][:, 0:384]
                        P.cp('act', cur, PS[2][:, 0:384])
                        dl = G[4][:, 384:768]
                        P.tt('dve', dl, PS[3][:, 0:384], cur, ALU.subtract)
                        P.tt('pool', dl, dl, tmf('rw_mu')[:, half * 384:(half + 1) * 384], ALU.mult)
                        P.tt('dve', rkv[:, half * 384:(half + 1) * 384], cur, dl, ALU.add)
                    r_ = rkv[:, 0:256]
                    k_ = rkv[:, 256:512]
                    v_ = rkv[:, 512:768]
                    cm_mm(PS[6][:, 0:128], RW0 + 768, 128)
                    cm_mm(PS[6][:, 128:256], RW0 + 768, 128, shifted=True)
                    lo = G[3][:, 0:128]
                    P.cp('act', lo, PS[6][:, 0:128])
                    dl = G[3][:, 128:256]
                    P.tt('dve', dl, PS[6][:, 128:256], lo, ALU.subtract)
                    P.stt('dve', lo, dl, cmc[:, 40:41], lo, ALU.mult, ALU.add)
                    P.act(lo[0:32, :], lo[0:32, :], AF.Tanh)
                    P.act(lo[64:128, :], lo[64:128, :], AF.Sigmoid)
                    ps = PS[7]
                    P.mm(ps[:, 0:256], lo[0:32, :], lup[0:32, :])
                    ps2 = PS[6]
                    P.mm(ps2[:, 0:256], lo[32:64, :], lup[32:64, :])
                    P.mm(ps2[:, 256:512], lo[64:128, :], lup[64:128, :])
                    gg = G[3][:, 256:512]
                    P.cp('act', gg, ps2[:, 256:512])
                    lw = G[3][:, 512:768]
                    P.tt('dve', lw, ps[:, 0:256], tmf('rw_w0'), ALU.add)
                    P.act(lw, lw, AF.Exp, scale=-1.0)
                    P.act(lw, lw, AF.Ln, bias=1.0)
                    P.ts('dve', lw, lw, -1.0, ALU.mult, -0.5, ALU.add)
                    P.act(lw, lw, AF.Exp)
                    P.ts('dve', lw, lw, -1.0, ALU.mult)
                    av = G[3][:, 768:1024]
                    P.tt('dve', av, ps2[:, 0:256], tmf('rw_a0'), ALU.add)
                    P.act(av, av, AF.Sigmoid)
                    kk = G[4][:, 0:256]
                    P.tt('dve', kk, k_, tmf('rw_kk'), ALU.mult)
                    sq = G[4][:, 256:512]
                    P.tt('pool', sq, kk, kk, ALU.mult)
                    nrm = small[:, 80:84]
                    P.treduce('dve', nrm, h4(sq))
                    P.ts('dve', nrm, nrm, 1e-24, ALU.max)
                    P.act(nrm, nrm, AF.Ln)
                    P.act(nrm, nrm, AF.Exp, scale=-0.5)
                    P.tt('dve', h4(kk), h4(kk), b4(nrm, 64), ALU.mult)
                    km = G[4][:, 256:512]
                    P.ts('dve', km, av, -1.0, ALU.add)
                    P.tt('dve', km, km, tmf('rw_ka'), ALU.mult)
                    P.ts('dve', km, km, 1.0, ALU.add)
                    P.tt('dve', km, km, k_, ALU.mult)
                    bt_ = G[4][:, 512:768]
                    P.tt('pool', bt_, r_, km, ALU.mult)
                    P.tt('pool', bt_, bt_, tmf('rw_rk'), ALU.mult)
                    bon = small[:, 84:88]
                    P.treduce('dve', bon, h4(bt_))
                    ps = PS[7]
                    P.mm(ps[:, 0:256], BTRI, lw)
                    Wt = G[4][:, 512:768]
                    P.act(Wt, ps[:, 0:256], AF.Exp)
                    Wi = G[4][:, 768:1024]
                    P.act(Wi, ps[:, 0:256], AF.Exp, scale=-1.0)
                    Wp = G[5][:, 0:256]
                    cwsb = G[5][:, 256:512]
                    P.cp('dve', cwsb, ps[:, 0:256])
                    P.tt('dve', Wp, cwsb, lw, ALU.subtract)
                    P.act(Wp, Wp, AF.Exp)
                    ps = PS[2]
                    for hp in range(2):
                        for j in range(2):
                            P.mm(ps[:, hp * 2 + j:hp * 2 + j + 1], lw[j * 64:(j + 1) * 64, hp * 128:(hp + 1) * 128],
                                 ones[j * 64:(j + 1) * 64, 0:1])
                    WL = small[:, 88:92]
                    P.act(WL, ps[:, 0:4], AF.Exp)
                    rt = G[5][:, 256:512]
                    P.tt('dve', rt, r_, Wt, ALU.mult)
                    at_ = G[5][:, 512:768]
                    P.stt('dve', at_, kk, -1.0, Wp, ALU.mult, ALU.mult)
                    btl = G[5][:, 768:1024]
                    P.tt('dve', btl, kk, av, ALU.mult)
                    P.tt('dve', btl, btl, Wi, ALU.mult)
                    kt = G[4][:, 0:256]
                    P.tt('dve', kt, km, Wi, ALU.mult)
                    btb = H[1][:, 0:256]
                    ktb = H[1][:, 256:512]
                    vbb = H[1][:, 512:768]
                    P.cp('pool', btb, btl)
                    P.cp('pool', ktb, kt)
                    P.cp('pool', vbb, v_)
                    cmT = {}
                    for ai, (nm, arr) in enumerate([('r', rt), ('a', at_), ('b', btl), ('k', kt)]):
                        ps = PS[2 + ai % 2]
                        P.tr(ps[:, 0:128], arr[:, 0:128], ident)
                        P.tr(ps[:, 128:256], arr[:, 128:256], ident)
                        dstT = G[6][:, ai * 256:(ai + 1) * 256]
                        P.cp('act' if ai % 2 == 0 else 'dve', dstT, ps[:, 0:256])
                        cmT[nm] = dstT
                    o_rw = Y[:, 256:512]
                    def rw_head(h, Fs, Bs, bk1, bk2):
                        hp, hq = h // 2, (h % 2) * 64
                        sl = slice(hq, hq + 64)
                        rT = cmT['r'][sl, hp * 128:(hp + 1) * 128]
                        aT = cmT['a'][sl, hp * 128:(hp + 1) * 128]
                        bT = cmT['b'][sl, hp * 128:(hp + 1) * 128]
                        kT = cmT['k'][sl, hp * 128:(hp + 1) * 128]
                        psA = bk1
                        P.mm(psA[:, 0:128], bT, aT)
                        P.mm(psA[:, 128:256], aT, bT)
                        P.mm(psA[:, 256:384], kT, aT)
                        psB = bk2
                        P.mm(psB[:, 0:128], bT, rT)
                        P.mm(psB[:, 128:256], kT, rT)
                        Pm = Fs[:, 0:128]
                        PTm = Fs[:, 128:256]
                        TT = Fs[:, 256:384]
                        P.tt('dve', Pm, psA[:, 0:128], BTRIS, ALU.mult)
                        P.tt('dve', PTm, psA[:, 128:256], BLOW, ALU.mult)
                        AakT = Bs[:, 0:128]
                        P.tt('dve', AakT, psA[:, 256:384], BTRIS, ALU.mult)
                        ArbT = Bs[:, 128:256]
                        P.tt('dve', ArbT, psB[:, 0:128], BTRI, ALU.mult)
                        ArkT = Bs[:, 256:384]
                        P.tt('dve', ArkT, psB[:, 128:256], BTRI, ALU.mult)
                        P.tt('dve', TT, Pm, ident, ALU.add)
                        for step in range(5):
                            psq = bk1
                            P.mm(psq[:, 0:128], PTm, Pm)
                            P.mm(psq[:, 128:256], Pm, PTm)
                            P.cp('dve', Pm, psq[:, 0:128])
                            P.cp('act', PTm, psq[:, 128:256])
                            psq2 = bk2
                            P.mm(psq2[:, 256:384], PTm, TT)
                            P.tt('dve', TT, TT, psq2[:, 256:384], ALU.add)
                        TTb = Bs[:, 384:512]
                        P.cp('pool', TTb, TT)
                        aTb = Bs[:, 512:640]
                        rTb = Bs[:, 640:768]
                        P.cp('pool', aTb[sl, :], aT)
                        P.cp('pool', rTb[sl, :], rT)
                        for j in range(2):
                            js = slice(j * 64, (j + 1) * 64)
                            psx = bk2
                            vh = vbb[js, h * 64:(h + 1) * 64]
                            P.mm(psx[js, 0:64], aTb[sl, js], rw_Sb[hp][sl, :], start=True, stop=False)
                            P.mm(psx[js, 0:64], AakT[js, js], vh, start=False, stop=True)
                            X1 = Bs[:, 768:832]
                            P.cp('act', X1[js, :], psx[js, 0:64])
                            P.mm(psx[js, 64:128], TTb[js, js], X1[js, :])
                            Ub = Bs[:, 832:896]
                            P.cp('act', Ub[js, :], psx[js, 64:128])
                            P.mm(psx[js, 128:192], rTb[sl, js], rw_Sb[hp][sl, :], start=True, stop=False)
                            P.mm(psx[js, 128:192], ArbT[js, js], Ub[js, :], start=False, stop=False)
                            P.mm(psx[js, 128:192], ArkT[js, js], vh, start=False, stop=True)
                            P.cp('dve', o_rw[js, h * 64:(h + 1) * 64], psx[js, 128:192])
                            P.mm(psx[sl, 192:256], btb[js, h * 64:(h + 1) * 64], Ub[js, :], start=True, stop=False)
                            P.mm(psx[sl, 192:256], ktb[js, h * 64:(h + 1) * 64], vh, start=False, stop=True)
                            P.tt('dve', rw_S[hp][sl, :], rw_S[hp][sl, :], psx[sl, 192:256], ALU.add)
                            P.ts('dve', rw_S[hp][sl, :], rw_S[hp][sl, :], WL[sl, hp * 2 + j:hp * 2 + j + 1], ALU.mult)
                            P.cp('pool', rw_Sb[hp][sl, :], rw_S[hp][sl, :])
                P.merge()
                P.begin('s2')
                if 'mlstm' not in skip:
                    cmf = Q2
                    for ci in range(4):
                        conv_silu(4 + ci, 20 + ci * 4, 36 + ci, ML0 + ci * 128, cmf[:, ci * 128:(ci + 1) * 128])
                    P.cp('pool', J0[:, 0:512], cmf[:, 0:512])
                    ps = PS[4]
                    P.tr(ps[:, 0:128], cmf[:, 256:384], ident)
                    P.tr(ps[:, 128:256], cmf[:, 384:512], ident)
                    ktm = Q3[:, 0:256]
                    P.cp('act', ktm, ps[:, 0:256])
                    ps = PS[5]
                    tm_mm(ps[:, 0:264], ML0 + 512, 264)
                    vaug = J1[:, 0:260].rearrange("p (h d) -> p h d", h=4)
                    P.cp('act', vaug[:, :, 0:64], h4(ps[:, 0:256]))
                    P.memset('pool', vaug[:, :, 64:65], 1.0)
                    ig = smallS[:, 48:52]
                    P.tt('dve', ig, ps[:, 256:260], tmf('ml_ib'), ALU.add)
                    lf = smallS[:, 52:56]
                    P.tt('dve', lf, ps[:, 260:264], tmf('ml_fb'), ALU.add)
                    P.act(lf, lf, AF.Exp, scale=-1.0)
                    P.act(lf, lf, AF.Ln, bias=1.0)
                    P.ts('dve', lf, lf, -1.0, ALU.mult)
                    ps = PS[0]
                    tm_mm(ps[:, 0:256], ML0 + 776, 256)
                    ogs = Q3[:, 256:512]
                    P.act(ogs, ps[:, 0:256], AF.Sigmoid)
                    ps = PS[0]
                    P.mm(ps[:, 0:4], TRI, lf)
                    P.mm(ps[:, 4:8], ones[:], lf)
                    bb = smallS[:, 56:60]
                    P.cp('dve', bb, ps[:, 0:4])
                    eb = smallS[:, 64:68]
                    P.act(eb, ps[:, 0:4], AF.Exp)
                    wst = smallS[:, 68:72]
                    P.tt('dve', wst, ps[:, 4:8], bb, ALU.subtract)
                    P.tt('dve', wst, wst, ig, ALU.add)
                    P.act(wst, wst, AF.Exp)
                    ebl4 = smallS[:, 72:76]
                    P.act(ebl4, ps[:, 4:8], AF.Exp)
                    pso = PS[1]
                    for h in range(4):
                        hp, hq = h // 2, (h % 2) * 64
                        qT_h = J0[hq:hq + 64, hp * 128:(hp + 1) * 128]
                        kT_h = J0[hq:hq + 64, 256 + hp * 128:256 + (hp + 1) * 128]
                        psl = (PS[4], PS[5])[h % 2]
                        lfb = Q4[:, 128:256]
                        P.cp('pool', lfb, lf[:, h:h + 1].to_broadcast([128, 128]))
                        P.mm(psl[:, 0:128], lfb, TRI)
                        dm = Q4[:, 256:384]
                        P.ts('dve', dm, psl[:, 0:128], bb[:, h:h + 1], ALU.subtract, 0.0, ALU.min)
                        P.act(dm, dm, AF.Exp, bias=ig[:, h:h + 1])
                        P.tt('pool', dm, dm, TRI, ALU.mult)
                        P.mm(psl[:, 128:256], kT_h, qT_h)
                        sT = J2[:, h * 128:(h + 1) * 128]
                        P.stt('dve', sT, psl[:, 128:256], 0.125, dm, ALU.mult, ALU.mult)
                        P.mm(pso[:, h * 65:(h + 1) * 65], sT, vaug[:, h, :])
                        P.mm(PS[0][:, h * 65:(h + 1) * 65], qT_h, ml_Sb[hp][hq:hq + 64, :])
                    numf = Q4[:, 512:772]
                    num = h4(numf)
                    P.tt('dve', num, h4(PS[0][:, 0:260]), b4(eb, 65), ALU.mult)
                    P.stt('dve', numf, numf, 0.125, pso[:, 0:260], ALU.mult, ALU.add)
                    den = smallS[:, 76:80]
                    den3 = den.rearrange("p (h o) -> p h o", o=1)
                    P.stt('dve', den3, num[:, :, 64:65], -1.0, num[:, :, 64:65], ALU.mult, ALU.max)
                    P.ts('dve', den, den, 1.0, ALU.max)
                    P.recip(den, den)
                    yv = Y[:, 768:1024]
                    P.tt('dve', h4(yv), num[:, :, 0:64], b4(den, 64), ALU.mult)
                    P.tt('dve', yv, yv, ogs, ALU.mult)
                    kw = J1[:, 260:516]
                    P.tt('dve', h4(kw), h4(ktm), b4(wst, 64), ALU.mult)
                    psn = PS[0]
                    for h in range(4):
                        hp, hq = h // 2, (h % 2) * 64
                        P.mm(psn[hq:hq + 64, 256 + hp * 65:256 + (hp + 1) * 65], kw[:, h * 64:(h + 1) * 64], vaug[:, h, :])
                    for hp in range(2):
                        for hh in range(2):
                            h = hp * 2 + hh
                            hq = hh * 64
                            P.ts('dve', ml_S[hp][hq:hq + 64, :], ml_S[hp][hq:hq + 64, :], ebl4[hq:hq + 64, h:h + 1], ALU.mult)
                        P.tt('dve', ml_S[hp][:], ml_S[hp][:], psn[:, 256 + hp * 65:256 + (hp + 1) * 65], ALU.add)
                        P.cp('pool', ml_Sb[hp][:], ml_S[hp][:])
                    groupnorm(yv, 4, tmf('ml_ng'), None, 1e-5, True, small2S, Q4[:, 768:1024])

                if 'rwkv' not in skip:
                    P.begin('ha')
                    rw_head(0, G[5], H[3], PS[2], PS[6])
                    P.begin('hb')
                    rw_head(2, G[4], H[4], PS[3], PS[7])
                P.merge()
                if 'rwkv' not in skip:
                    P.begin('ha')
                    rw_head(1, G[5], H[3], PS[2], PS[6])
                    P.begin('hb')
                    rw_head(3, G[4], H[4], PS[3], PS[7])
                P.merge()
                if 'rwkv' not in skip:
                    groupnorm(o_rw, 4, tmf('rw_lng'), tmf('rw_lnb'), 64e-5, True, small2, G[4][:, 768:1024])
                    t1 = G[4][:, 512:768]
                    P.tt('dve', h4(t1), h4(v_), b4(bon, 64), ALU.mult)
                    P.tt('dve', o_rw, o_rw, t1, ALU.add)
                    P.tt('dve', o_rw, o_rw, gg, ALU.mult)
                if debug and layer == 0:
                    for nm, c0 in [('d_ssd', 0), ('d_rwkv', 256), ('d_gla', 512), ('d_mlstm', 768)]:
                        P.dma(dbg[nm][c * 128:(c + 1) * 128, :], Y[:, c0:c0 + 256], comm=True)
                if stop_after == 'mixers':
                    continue

                YT = H[2].rearrange("p (k n) -> p k n", k=8)
                for k in range(8):
                    P.tr(PS[2 + k // 4][:, (k % 4) * 128:(k % 4 + 1) * 128], Y[:, k * 128:(k + 1) * 128], ident)
                for hf in range(2):
                    P.cp('act' if hf == 0 else 'dve', YT[:, hf * 4:(hf + 1) * 4, :],
                         PS[2 + hf][:, :].rearrange("p (k n) -> p k n", k=4))
                x1 = G[2]
                for hf in range(2):
                    ps = PS[4 + hf]
                    for k in range(8):
                        P.mm(ps[:, :], YT[:, k, :], WO[:, k, hf * 512:(hf + 1) * 512], start=(k == 0), stop=(k == 7))
                    P.stt('dve', x1[:, hf * 512:(hf + 1) * 512], xt[:, hf * 512:(hf + 1) * 512], ALPHA, ps[:, :], ALU.mult, ALU.add)
                layernorm(x1[:], x1[:], 'ln1_g', 'ln1_b', small2)
                P.dma(x1buf[c * 128:(c + 1) * 128, :], x1[:], comm=True)
                if debug and layer == 0:
                    P.dma(dbg['d_x1'][c * 128:(c + 1) * 128, :], x1[:], comm=True)
                x1b = H[0]
                P.cp('pool', x1b[:], x1[:])
                x1T = G[3].rearrange("p (k n) -> p k n", k=8)
                for k in range(8):
                    P.tr(PS[2 + k // 4][:, (k % 4) * 128:(k % 4 + 1) * 128], x1[:, k * 128:(k + 1) * 128], ident)
                for hf in range(2):
                    P.cp('act' if hf == 0 else 'dve', x1T[:, hf * 4:(hf + 1) * 4, :],
                         PS[2 + hf][:, :].rearrange("p (k n) -> p k n", k=4))
                ps = PS[6]
                for k in range(8):
                    P.mm(ps[:, 0:32], x1T[:, k, :], rtw[:, k, :], start=(k == 0), stop=(k == 7))
                lg = rsc[:, 0:32]
                P.tt('dve', lg, ps[:, 0:32], tmf('rt_b'), ALU.add)
                mx8 = rsc[:, 32:40]
                P.op('dve', lambda e, mx8=mx8, lg=lg: e.max(out=mx8, in_=lg), reads=K(lg), writes=K(mx8))
                P.op('dve', lambda e, mx8=mx8, lg=lg: e.max_index(out=idx8[:], in_max=mx8, in_values=lg),
                     reads=K(lg, mx8), writes=K(idx8))
                msk = rsc[:, 40:72]
                P.ts('dve', msk, lg, mx8[:, 3:4], ALU.is_ge)
                nmx = rsc[:, 72:73]
                P.ts('dve', nmx, mx8[:, 0:1], -1.0, ALU.mult)
                ex = rsc[:, 76:108]
                P.act(ex, lg, AF.Exp, bias=nmx)
                P.tt('dve', ex, ex, msk, ALU.mult)
                ssum = rsc[:, 73:74]
                P.rsum('dve', ssum, ex)
                P.recip(ssum, ssum)
                P.ts('dve', ex, ex, ssum, ALU.mult)
                ps = PS[5]
                P.mm(ps[:, 0:32], TRIS, msk)
                P.mm(ps[:, 32:64], ones[:], msk)
                pos = rsc[:, 108:140]
                P.tt('dve', pos, ps[:, 0:32], cntb[:], ALU.add)
                P.tt('dve', cntb[:], cntb[:], ps[:, 32:64], ALU.add)
                idxf = rsc[:, 140:144]
                P.cp('dve', idxf, idx8[:, 0:4])
                destf = rsc[:, 144:148]
                oh3 = Q3[:, 0:128].rearrange("p (k e) -> p k e", k=4)
                tmp3 = Q3[:, 128:256].rearrange("p (k e) -> p k e", k=4)
                bce = lambda a: a.rearrange("p (o e) -> p o e", o=1).to_broadcast([128, 4, NE])
                P.tt('dve', oh3, bce(IOTA32), b4(idxf, NE), ALU.is_equal)
                P.tt('dve', tmp3, oh3, bce(pos), ALU.mult)
                P.treduce('dve', destf, tmp3)
                P.tt('dve', tmp3, oh3, bce(ex), ALU.mult)
                P.treduce('dve', gate_all[:, c, :], tmp3)
                ovf = rsc[:, 148:152]
                P.ts('dve', ovf, destf, float(CAP), ALU.is_ge, float(NE * CAP), ALU.mult)
                P.stt('dve', destf, idxf, float(CAP), destf, ALU.mult, ALU.add)
                P.tt('dve', destf, destf, ovf, ALU.add)
                P.cp('dve', dest_all[:, c, :], destf)
                for k4 in range(4):
                    P.scatter(xg, dest_all[:, c, k4:k4 + 1], x1b[:], NE * CAP - 1)
                if debug and layer == 0:
                    P.dma(dbg['d_logits'][c * 128:(c + 1) * 128, :], lg, comm=True)
                    P.dma(dbg['d_dest'][c * 128:(c + 1) * 128, :], destf, comm=True)
                    P.dma(dbg['d_gate'][c * 128:(c + 1) * 128, :], gate_all[:, c, :], comm=True)
            if stop_after in ('mixers', 'phaseA'):
                continue

            EB = [R1, R0]

            def eb_views(i):
                gu = EB[i][:, 0:16384].rearrange("p (k n) -> p k n", k=8)
                dn = EB[i][:, 16384:24576].rearrange("p (k n) -> p k n", k=8)
                return gu, dn

            def load_expert(e):
                gu, dn = eb_views(e % 2)
                for k in range(8):
                    P.dmac(gu[:, k, :], w_gu[layer, e, k * 128:(k + 1) * 128, :])
                for k in range(8):
                    P.dmac(dn[:, k, :], w_dn[layer, e, k * 128:(k + 1) * 128, :])
                P.dma(bgu[e % 2][:], b_gu[layer, e])

            bdn = G[6]
            groups = [(e, grp) for e in range(NE) for grp in range(CAP // 256)]

            def views(gi):
                xgT = G[(gi % 2) * 2][:].bitcast(BF16).rearrange("p (k n) -> p k n", k=8)
                actT = G[(gi % 2) * 2 + 1][:].bitcast(BF16).rearrange("p (k n) -> p k n", k=8)
                return xgT, actT

            def prep(gi):
                e, grp = groups[gi]
                xgT, _ = views(gi)
                base = e * CAP + grp * 256
                for stl in range(2):
                    xr = H[stl]
                    P.dma(xr[:], xg[base + stl * 128:base + (stl + 1) * 128, :])
                    for k in range(8):
                        P.tr(PSB[:, k * 128:(k + 1) * 128], xr[:, k * 128:(k + 1) * 128], identb[:])
                    P.cp('act', xgT[:, :, stl * 128:(stl + 1) * 128],
                         PSB[:, :].rearrange("p (k n) -> p k n", k=8))

            def hphase(gi):
                e, grp = groups[gi]
                gu, dn = eb_views(e % 2)
                xgT, actT = views(gi)
                for fc in range(8):
                    psg = PS[(fc % 2) * 2]
                    psl = PS[(fc % 2) * 2 + 1]
                    for k in range(8):
                        P.mm(psg[:, 0:256], gu[:, k, fc * 128:(fc + 1) * 128], xgT[:, k, :], start=(k == 0), stop=(k == 7))
                    for k in range(8):
                        P.mm(psl[:, 0:256], gu[:, k, 1024 + fc * 128:1024 + (fc + 1) * 128], xgT[:, k, :], start=(k == 0), stop=(k == 7))
                    tgl = (G[4], Q4)[fc % 2]
                    tg = tgl[:, 0:256]
                    tl = tgl[:, 256:512]
                    bg = bgu[e % 2]
                    P.ts('dve', tg, psg[:, 0:256], bg[:, fc:fc + 1], ALU.add, 7.0, ALU.min)
                    P.ts('dve', tl, psl[:, 0:256], bg[:, 8 + fc:9 + fc], ALU.add, 7.0, ALU.min)
                    sg = Q2[:, (fc % 2) * 256:(fc % 2) * 256 + 256]
                    P.act(sg, tg, AF.Sigmoid, scale=1.702)
                    P.ts('dve', tl, tl, -7.0, ALU.max, 1.0, ALU.add)
                    P.tt('dve', tg, tg, sg, ALU.mult)
                    P.tt('dve', actT[:, fc, :], tl, tg, ALU.mult)

            def down(gi):
                e, grp = groups[gi]
                gu, dn = eb_views(e % 2)
                _, actT = views(gi)
                base = e * CAP + grp * 256
                if grp == 0:
                    P.dma(bdn[:], b_dn[layer, e].partition_broadcast(128))
                for stl in range(2):
                    yb = (G[5], Q3)[stl]
                    for hf in range(2):
                        ps = PS[4 + hf]
                        for k in range(8):
                            P.mm(ps[:, :], actT[:, k, stl * 128:(stl + 1) * 128], dn[:, k, hf * 512:(hf + 1) * 512],
                                 start=(k == 0), stop=(k == 7))
                        P.tt('dve', yb[:, hf * 512:(hf + 1) * 512], ps[:, :], bdn[:, hf * 512:(hf + 1) * 512], ALU.add)
                    P.dma(yg[base + stl * 128:base + (stl + 1) * 128, :], yb[:], comm=True)

            load_expert(0)
            prep(0)
            for gi in range(len(groups)):
                e, grp = groups[gi]
                if grp == 0 and e + 1 < NE:
                    load_expert(e + 1)
                if grp == 0 and e == NE - 1:
                    load_r1(layer)
                hphase(gi)
                if gi + 1 < len(groups):
                    prep(gi + 1)
                down(gi)
            if stop_after == 'phaseB':
                continue

            if layer + 1 < n_layers:
                for k in range(8):
                    P.dmac(W3[:, k, :], w_in[layer + 1, k * 128:(k + 1) * 128, :])
                win_prefetched = True
            for c in range(ntiles):
                x1 = G[2]
                P.dma(x1[:], x1buf[c * 128:(c + 1) * 128, :])
                hh = G[0]
                P.ts('dve', hh[:], x1[:], ALPHA, ALU.mult)
                for k4 in range(4):
                    gt = G[3 + k4]
                    P.memset('pool', gt[:], 0.0)
                    P.gather(gt[:], yg, dest_all[:, c, k4:k4 + 1], NE * CAP - 1)
                for k4 in range(4):
                    gt = G[3 + k4]
                    P.stt('dve', hh[:], gt[:], gate_all[:, c, k4:k4 + 1], hh[:], ALU.mult, ALU.add)
                if debug and layer == 0:
                    ff = Q4
                    P.stt('dve', ff[:], x1[:], -ALPHA, hh[:], ALU.mult, ALU.add)
                    P.dma(dbg['d_ffn'][c * 128:(c + 1) * 128, :], ff[:], comm=True)
                hT = H[2].rearrange("p (k n) -> p k n", k=8)
                for k in range(8):
                    P.tr(PS[k // 4][:, (k % 4) * 128:(k % 4 + 1) * 128], hh[:, k * 128:(k + 1) * 128], ident)
                for hf in range(2):
                    P.cp('act' if hf == 0 else 'dve', hT[:, hf * 4:(hf + 1) * 4, :],
                         PS[hf][:, :].rearrange("p (k n) -> p k n", k=4))
                pt = Q3[:, 0:256]
                P.dma(pt, p_in[layer, c * 128:(c + 1) * 128, :])
                P.tr(PS[2][:, 0:128], pt[:, 0:128], ident)
                P.tr(PS[2][:, 128:256], pt[:, 128:256], ident)
                pT = H[3][:, 0:256].rearrange("p (k n) -> p k n", k=2)
                P.cp('act', pT, PS[2][:, 0:256].rearrange("p (k n) -> p k n", k=2))
                sig = G[1]
                for hf in range(2):
                    ps = PS[3 + hf]
                    for k in range(8):
                        P.mm(ps[:, :], hT[:, k, :], PG[:, k, hf * 512:(hf + 1) * 512], start=(k == 0), stop=(k == 7))
                    P.act(sig[:, hf * 512:(hf + 1) * 512], ps[:, :], AF.Sigmoid)
                    ps2 = PS[5 + hf]
                    for k in range(2):
                        P.mm(ps2[:, :], pT[:, k, :], PP[:, k, hf * 512:(hf + 1) * 512], start=(k == 0), stop=(k == 1))
                    P.tt('dve', sig[:, hf * 512:(hf + 1) * 512], sig[:, hf * 512:(hf + 1) * 512], ps2[:, :], ALU.mult)
                P.tt('dve', hh[:], hh[:], sig[:], ALU.add)
                layernorm(hh[:], hh[:], 'ln2_g', 'ln2_b', small2, sq=Q4)
                P.dma(xdst[c * 128:(c + 1) * 128, :], hh[:], comm=True)
        print('total ops recorded', P.nops)
        P.emit()
    return nc


def _consts():
    s = np.arange(128)[:, None]
    t = np.arange(128)[None, :]
    blk = (s // 64) == (t // 64)
    c = np.zeros((128, 6, 128), np.float32)
    c[:, 0] = np.eye(128)
    c[:, 1] = (s <= t)
    c[:, 2] = (s < t)
    c[:, 3] = blk & (s <= t)
    c[:, 4] = blk & (s < t)
    c[:, 5] = blk & (t < s)
    c2 = np.zeros((128, 324), np.float32)
    c2[:, 0:256] = (np.arange(128)[:, None] // 32) == (np.arange(256)[None, :] // 64)
    c2[:, 256:288] = np.arange(32)[None, :]
    c2[:, 320:324] = (np.arange(128)[:, None] // 32) == np.arange(4)[None, :]
    return c, c2


def prep_inputs(inp):
    f = lambda k: np.asarray(inp[k], dtype=np.float32)
    L = DEPTH
    tm = np.zeros((L, 1, TM_W), np.float32)
    src = {
        'dt_bias': f('ssd_dt_bias'), 'a_log': f('ssd_a_log'), 'ssd_d': f('ssd_d'), 'ssd_ng': f('ssd_norm_g'),
        'rw_mu': f('rwkv_mu')[:, 0:768], 'rw_w0': f('rwkv_w0'), 'rw_a0': f('rwkv_a0'), 'rw_kk': f('rwkv_k_k'),
        'rw_ka': f('rwkv_k_a'), 'rw_rk': f('rwkv_r_k').reshape(L, 256), 'rw_lng': f('rwkv_ln_g'), 'rw_lnb': f('rwkv_ln_b'),
        'gl_gb': f('gla_gate_b'), 'gl_ng': f('gla_norm_g'),
        'ml_ib': f('mlstm_i_b'), 'ml_fb': f('mlstm_f_b'), 'ml_ng': f('mlstm_norm_g'),
        'ln1_g': f('ln1_g'), 'ln1_b': f('ln1_b'), 'ln2_g': f('ln2_g'), 'ln2_b': f('ln2_b'), 'rt_b': f('router_b'),
    }
    for nm, (o, w) in TM_OFF.items():
        tm[:, 0, o:o + w] = src[nm]
    cm = np.zeros((L, 128, CM_W), np.float32)
    scw = f('ssd_conv_w'); scb = f('ssd_conv_b'); mcw = f('mlstm_conv_w'); mcb = f('mlstm_conv_b')
    for ci in range(4):
        for j in range(4):
            cm[:, :, ci * 4 + j] = scw[:, j, ci * 128:(ci + 1) * 128]
            cm[:, :, 20 + ci * 4 + j] = mcw[:, j, ci * 128:(ci + 1) * 128]
        cm[:, :, 16 + ci] = scb[:, ci * 128:(ci + 1) * 128]
        cm[:, :, 36 + ci] = mcb[:, ci * 128:(ci + 1) * 128]
    cm[:, :, 40] = f('rwkv_mu')[:, 768:896]
    lora = np.concatenate([f('rwkv_w_up'), f('rwkv_a_up'), f('rwkv_g_up')], axis=1)
    bgu = np.ascontiguousarray(f('exp_b_gu').reshape(L, NE, 16, 128).transpose(0, 1, 3, 2))
    bdn = f('exp_b_down').reshape(L, NE, 1, D)
    c, c2 = _consts()
    shared = {
        'w_in': f('w_in'), 'w_out': f('w_out'), 'tmrow': tm, 'cmcol': cm, 'lora_up': np.ascontiguousarray(lora),
        'gla_up': f('gla_gate_up'), 'router_w': f('router_w'), 'w_gu': f('exp_w_gu'), 'b_gu': bgu,
        'w_dn': f('exp_w_down'), 'b_dn': bdn, 'ple_g': f('ple_gate_w'), 'ple_p': f('ple_proj'),
        'consts': c, 'consts2': c2,
    }
    return shared


_NC_CACHE = {}


def kernel(**inputs):
    shared = prep_inputs(inputs)
    x = np.asarray(inputs['x'], dtype=np.float32)
    p = np.asarray(inputs['p'], dtype=np.float32)
    if 'nc' not in _NC_CACHE:
        _NC_CACHE['nc'] = build()
    nc = _NC_CACHE['nc']
    in_maps = []
    for core in range(8):
        b = core % 4
        m = dict(shared)
        m['x'] = np.ascontiguousarray(x[b])
        m['p'] = np.ascontiguousarray(p[:, b])
        in_maps.append(m)
    res = run_bass_kernel_spmd(nc, in_maps, core_ids=list(range(8)))
    outs = [res.results[b]['out'] for b in range(4)]
    return np.stack(outs, axis=0).astype(np.float32)
```
